# Optimizing a Trainium2 kernel written in Bass

```python
import math
import jax
import jax.numpy as jnp
from jax import lax
import numpy as np

D_MODEL = 1024
BATCH = 4
SEQ = 4096
DEPTH = 2

N_A_LAYERS = DEPTH // 2
N_B_LAYERS = DEPTH - N_A_LAYERS

RWKV_HEAD = 64
RWKV_HEADS = D_MODEL // RWKV_HEAD
DECAY_LORA = 64
AAA_LORA = 64
GATE_LORA = 128
RWKV_GN_EPS = 64e-5

DIFF_QK_DIM = 64
DIFF_V_DIM = 2 * DIFF_QK_DIM
DIFF_HEADS = D_MODEL // DIFF_V_DIM
Q_BLOCK = 128
SUBLN_EPS = 1e-5

N_GROUPS = 4
EXPERTS_PER_GROUP = 8
N_EXPERTS = N_GROUPS * EXPERTS_PER_GROUP
TOP_K = 2
EXPERT_FF = 512
MOE_BLOCK = 128

RMS_EPS = 1e-6

kernel_name = 'hybrid_rwkv7_diffattn_hmoe_yoco'


def rmsnorm(x, g, eps=RMS_EPS):
    xf = x.astype(jnp.float32)
    y = xf * lax.rsqrt(jnp.mean(xf * xf, axis=-1, keepdims=True) + eps)
    return (y * g.astype(jnp.float32)).astype(x.dtype)


def modulate(h, shift, scale):
    return h * (1 + scale[:, None, :]) + shift[:, None, :]


def rwkv7_time_mix(h, mu, w_rkv, w0, w1, w2, a0, a1, a2, g1, g2, k_k, k_a, r_k, gn_g, gn_b, w_o):
    B, T, D = h.shape
    H, N = RWKV_HEADS, RWKV_HEAD
    xx = jnp.pad(h, ((0, 0), (1, 0), (0, 0)))[:, :-1] - h
    xs = h[None] + xx[None] * mu[:, None, None, :]
    r, k, v = jnp.einsum('ibtd,ide->ibte', xs[:3], w_rkv)
    xw, xa, xg = xs[3], xs[4], xs[5]
    w_log = -jax.nn.softplus(-(w0 + jnp.tanh(xw @ w1) @ w2)) - 0.5
    decay = jnp.exp(-jnp.exp(w_log.astype(jnp.float32)))
    a = jax.nn.sigmoid(a0 + (xa @ a1) @ a2)
    g = jax.nn.sigmoid(xg @ g1) @ g2
    heads = lambda t: t.reshape(B, T, H, N).astype(jnp.float32)
    kk = heads(k * k_k)
    kk = kk / jnp.maximum(jnp.linalg.norm(kk, axis=-1, keepdims=True), 1e-12)
    k = k * (1 + (a - 1) * k_a)
    r_h, k_h, v_h, a_h = heads(r), heads(k), heads(v), heads(a)
    w_h = decay.reshape(B, T, H, N)
    a_vec = -kk
    b_vec = kk * a_h

    def step(S, inp):
        r_t, w_t, k_t, v_t, a_t, b_t = inp
        sa = jnp.einsum('bhij,bhj->bhi', S, a_t)
        S = S * w_t[:, :, None, :] + sa[..., None] * b_t[:, :, None, :] + v_t[..., None] * k_t[:, :, None, :]
        return S, jnp.einsum('bhij,bhj->bhi', S, r_t)

    tm = lambda t: jnp.moveaxis(t, 1, 0)
    S0 = jnp.zeros((B, H, N, N), jnp.float32)
    _, y = lax.scan(step, S0, (tm(r_h), tm(w_h), tm(k_h), tm(v_h), tm(a_vec), tm(b_vec)))
    y = jnp.moveaxis(y, 0, 1)
    mean = jnp.mean(y, axis=-1, keepdims=True)
    var = jnp.mean(jnp.square(y - mean), axis=-1, keepdims=True)
    y = ((y - mean) * lax.rsqrt(var + RWKV_GN_EPS)).reshape(B, T, D) * gn_g + gn_b
    bonus = jnp.sum(r_h * k_h * r_k, axis=-1, keepdims=True) * v_h
    y = y + bonus.reshape(B, T, D)
    return (y.astype(h.dtype) * g) @ w_o


def diff_attention(h, k_sh, v_sh, w_q, lq1, lk1, lq2, lk2, subln_g, w_o, lambda_init):
    B, T, D = h.shape
    H = DIFF_HEADS
    q = (h @ w_q).reshape(B, T, H, 2, DIFF_QK_DIM)
    lam = (jnp.exp(jnp.sum(lq1 * lk1).astype(jnp.float32))
           - jnp.exp(jnp.sum(lq2 * lk2).astype(jnp.float32)) + lambda_init)
    nb = T // Q_BLOCK
    qb = jnp.moveaxis(q.reshape(B, nb, Q_BLOCK, H, 2, DIFF_QK_DIM), 1, 0)
    k_pos = jnp.arange(T)
    scale = DIFF_QK_DIM ** -0.5

    def attend(args):
        q_i, i = args
        s = jnp.einsum('bqhcd,bkhcd->bhcqk', q_i, k_sh).astype(jnp.float32) * scale
        q_pos = i * Q_BLOCK + jnp.arange(Q_BLOCK)
        mask = k_pos[None, :] <= q_pos[:, None]
        p = jax.nn.softmax(jnp.where(mask, s, -jnp.inf), axis=-1)
        attn = p[:, :, 0] - lam * p[:, :, 1]
        return jnp.einsum('bhqk,bkhe->bqhe', attn.astype(v_sh.dtype), v_sh)

    o = lax.map(attend, (qb, jnp.arange(nb)))
    o = jnp.moveaxis(o, 0, 1).reshape(B, T, H, DIFF_V_DIM)
    o = rmsnorm(o, subln_g, eps=SUBLN_EPS) * (1 - lambda_init)
    return o.reshape(B, T, D) @ w_o


def hier_moe(h, w_rg, w_re, w_gate, w_up, w_down):
    B, T, D = h.shape
    n_tok = B * T
    x = h.reshape(n_tok, D)
    grp_logits = (x @ w_rg).astype(jnp.float32)
    grp_prob = jax.nn.softmax(grp_logits, axis=-1)
    grp_idx = jnp.argmax(grp_logits, axis=-1).astype(jnp.int32)
    grp_gate = jnp.max(grp_prob, axis=-1, keepdims=True)
    exp_logits = (x @ w_re).astype(jnp.float32).reshape(n_tok, N_GROUPS, EXPERTS_PER_GROUP)
    exp_logits = exp_logits[jnp.arange(n_tok), grp_idx]
    top_val, top_idx = lax.top_k(exp_logits, TOP_K)
    gate = grp_gate * jax.nn.softmax(top_val, axis=-1)
    eid = grp_idx[:, None] * EXPERTS_PER_GROUP + top_idx.astype(jnp.int32)
    n_asg = n_tok * TOP_K
    flat_e = eid.reshape(-1)
    flat_tok = jnp.repeat(jnp.arange(n_tok, dtype=jnp.int32), TOP_K)
    flat_w = gate.reshape(-1)
    order = jnp.argsort(flat_e)
    se = flat_e[order]
    counts = jax.ops.segment_sum(jnp.ones_like(flat_e), flat_e, num_segments=N_EXPERTS)
    padded = (counts + MOE_BLOCK - 1) // MOE_BLOCK * MOE_BLOCK
    pad_end = jnp.cumsum(padded)
    pad_start = pad_end - padded
    start = jnp.cumsum(counts) - counts
    dest = pad_start[se] + jnp.arange(n_asg, dtype=jnp.int32) - start[se]
    n_rows = (n_asg + N_EXPERTS * MOE_BLOCK + MOE_BLOCK - 1) // MOE_BLOCK * MOE_BLOCK
    n_blocks = n_rows // MOE_BLOCK
    row_tok = jnp.zeros((n_rows,), jnp.int32).at[dest].set(flat_tok[order])
    row_w = jnp.zeros((n_rows,), jnp.float32).at[dest].set(flat_w[order])
    blk_start = jnp.arange(n_blocks, dtype=jnp.int32) * MOE_BLOCK
    blk_e = jnp.minimum(jnp.searchsorted(pad_end, blk_start, side='right'), N_EXPERTS - 1)
    xb = x[row_tok].reshape(n_blocks, MOE_BLOCK, D)

    def expert_block(args):
        xi, e = args
        hdn = jax.nn.silu(xi @ w_gate[e]) * (xi @ w_up[e])
        return hdn @ w_down[e]

    yb = lax.map(expert_block, (xb, blk_e)).reshape(n_rows, D)
    out = jnp.zeros((n_tok, D), jnp.float32).at[row_tok].add(yb.astype(jnp.float32) * row_w[:, None])
    return out.astype(h.dtype).reshape(B, T, D)


def setup_inputs(seed: int = 0) -> dict:
    key = jax.random.key(seed)
    ks = iter(jax.random.split(key, 64))
    D = D_MODEL
    NA, NB, L = N_A_LAYERS, N_B_LAYERS, DEPTH

    def nrm(shape, scale):
        return jax.random.normal(next(ks), shape, jnp.float32) * scale

    def gain(shape):
        return 1.0 + nrm(shape, 0.02)

    def unif(shape, lo, hi):
        return jax.random.uniform(next(ks), shape, jnp.float32, lo, hi)

    return {
        'x': nrm((BATCH, SEQ, D), 1.0),
        'c': nrm((BATCH, D), 1.0),
        'ada_w': nrm((L, D, 6 * D), 0.5 * D ** -0.5),
        'ada_b': nrm((L, 6 * D), 0.02),
        'norm_mix_g': gain((L, D)),
        'norm_ffn_g': gain((L, D)),
        'rw_mu': unif((NA, 6, D), 0.0, 1.0),
        'rw_w_rkv': nrm((NA, 3, D, D), D ** -0.5),
        'rw_w0': unif((NA, D), -6.0, -1.0),
        'rw_w1': nrm((NA, D, DECAY_LORA), D ** -0.5),
        'rw_w2': nrm((NA, DECAY_LORA, D), 0.1 * DECAY_LORA ** -0.5),
        'rw_a0': nrm((NA, D), 0.1),
        'rw_a1': nrm((NA, D, AAA_LORA), D ** -0.5),
        'rw_a2': nrm((NA, AAA_LORA, D), 0.1 * AAA_LORA ** -0.5),
        'rw_g1': nrm((NA, D, GATE_LORA), D ** -0.5),
        'rw_g2': nrm((NA, GATE_LORA, D), GATE_LORA ** -0.5),
        'rw_k_k': 0.85 + nrm((NA, D), 0.05),
        'rw_k_a': 1.0 + nrm((NA, D), 0.05),
        'rw_r_k': nrm((NA, RWKV_HEADS, RWKV_HEAD), 0.1),
        'rw_gn_g': gain((NA, D)),
        'rw_gn_b': nrm((NA, D), 0.02),
        'rw_w_o': nrm((NA, D, D), D ** -0.5),
        'ada_kv_w': nrm((D, 2 * D), 0.5 * D ** -0.5),
        'ada_kv_b': nrm((2 * D,), 0.02),
        'norm_kv_g': gain((D,)),
        'w_kv': nrm((D, 2 * D), D ** -0.5),
        'df_w_q': nrm((NB, D, D), D ** -0.5),
        'df_lq1': nrm((NB, DIFF_QK_DIM), 0.1),
        'df_lk1': nrm((NB, DIFF_QK_DIM), 0.1),
        'df_lq2': nrm((NB, DIFF_QK_DIM), 0.1),
        'df_lk2': nrm((NB, DIFF_QK_DIM), 0.1),
        'df_subln_g': gain((NB, DIFF_V_DIM)),
        'df_w_o': nrm((NB, D, D), D ** -0.5),
        'moe_w_rg': nrm((L, D, N_GROUPS), D ** -0.5),
        'moe_w_re': nrm((L, D, N_EXPERTS), D ** -0.5),
        'moe_w_gate': nrm((L, N_EXPERTS, D, EXPERT_FF), D ** -0.5),
        'moe_w_up': nrm((L, N_EXPERTS, D, EXPERT_FF), D ** -0.5),
        'moe_w_down': nrm((L, N_EXPERTS, EXPERT_FF, D), EXPERT_FF ** -0.5),
        'final_g': gain((D,)),
    }


def reference(x, c, ada_w, ada_b, norm_mix_g, norm_ffn_g,
              rw_mu, rw_w_rkv, rw_w0, rw_w1, rw_w2, rw_a0, rw_a1, rw_a2, rw_g1, rw_g2,
              rw_k_k, rw_k_a, rw_r_k, rw_gn_g, rw_gn_b, rw_w_o,
              ada_kv_w, ada_kv_b, norm_kv_g, w_kv,
              df_w_q, df_lq1, df_lk1, df_lq2, df_lk2, df_subln_g, df_w_o,
              moe_w_rg, moe_w_re, moe_w_gate, moe_w_up, moe_w_down, final_g):
    B, T, D = x.shape
    c_act = jax.nn.silu(c)
    k_sh = None
    v_sh = None
    for l in range(DEPTH):
        mod = c_act @ ada_w[l] + ada_b[l]
        sh_m, sc_m, g_m, sh_f, sc_f, g_f = jnp.split(mod, 6, axis=-1)
        if l < N_A_LAYERS:
            i = l
            h = modulate(rmsnorm(x, norm_mix_g[l]), sh_m, sc_m)
            y = rwkv7_time_mix(h, rw_mu[i], rw_w_rkv[i], rw_w0[i], rw_w1[i], rw_w2[i],
                               rw_a0[i], rw_a1[i], rw_a2[i], rw_g1[i], rw_g2[i],
                               rw_k_k[i], rw_k_a[i], rw_r_k[i], rw_gn_g[i], rw_gn_b[i], rw_w_o[i])
        else:
            if l == N_A_LAYERS:
                sh_kv, sc_kv = jnp.split(c_act @ ada_kv_w + ada_kv_b, 2, axis=-1)
                hk = modulate(rmsnorm(x, norm_kv_g), sh_kv, sc_kv)
                kv = hk @ w_kv
                k_sh = kv[..., :D].reshape(B, T, DIFF_HEADS, 2, DIFF_QK_DIM)
                v_sh = kv[..., D:].reshape(B, T, DIFF_HEADS, DIFF_V_DIM)
            j = l - N_A_LAYERS
            h = modulate(rmsnorm(x, norm_mix_g[l]), sh_m, sc_m)
            lambda_init = 0.8 - 0.6 * math.exp(-0.3 * l)
            y = diff_attention(h, k_sh, v_sh, df_w_q[j], df_lq1[j], df_lk1[j], df_lq2[j], df_lk2[j],
                               df_subln_g[j], df_w_o[j], lambda_init)
        x = x + g_m[:, None, :] * y
        h = modulate(rmsnorm(x, norm_ffn_g[l]), sh_f, sc_f)
        x = x + g_f[:, None, :] * hier_moe(h, moe_w_rg[l], moe_w_re[l], moe_w_gate[l],
                                           moe_w_up[l], moe_w_down[l])
    return rmsnorm(x, final_g)
```

```python
import math
import os
from contextlib import ExitStack

import numpy as np
import concourse.bass as bass
import concourse.mybir as mybir
from concourse.bass_utils import run_bass_kernel_spmd

F32 = mybir.dt.float32
BF16 = mybir.dt.bfloat16
I32 = mybir.dt.int32
AF = mybir.ActivationFunctionType
ALU = mybir.AluOpType
AX = mybir.AxisListType

D = 1024
NH = 16
HN = 64
NEXP = 32
EFF = 512
C0 = -math.exp(-0.5)


class Buf:
    __slots__ = ("name", "w", "r")

    def __init__(self, name=""):
        self.name = name
        self.w = None
        self.r = {}


class KB:
    def __init__(self, nc, n_dma_sems=(16, 12, 6)):
        self.nc = nc
        self.eng = {"pe": nc.tensor, "act": nc.scalar, "dve": nc.vector, "pool": nc.gpsimd, "sp": nc.sync}
        self.sems = {}
        self.cnt = {}
        self._ctx = []
        for e in ("pe", "act", "dve", "pool"):
            self._mksem("c_" + e)
        self.dma_pool = {}
        for q, n in zip(("sp", "pool", "act"), n_dma_sems):
            keys = []
            for i in range(n):
                k = f"d_{q}{i}"
                self._mksem(k)
                keys.append(k)
            self.dma_pool[q] = [keys, 0]
        self.seen = {e: {} for e in self.eng}
        self.n_inst = 0
        self.n_wait = 0

    def _mksem(self, key):
        g = self.nc.semaphore(key)
        s = g.__enter__()
        self._ctx.append(g)
        self.sems[key] = s
        self.cnt[key] = 0

    def close(self):
        for g in reversed(self._ctx):
            g.__exit__(None, None, None)

    def _wait(self, e, ticket):
        if ticket is None:
            return
        key, val = ticket
        if self.seen[e].get(key, 0) >= val:
            return
        self.eng[e].wait_ge(self.sems[key], val)
        self.seen[e][key] = val
        self.n_wait += 1

    def _mark(self, ticket, reads, writes):
        k, v = ticket
        for b in reads:
            if b.r.get(k, 0) < v:
                b.r[k] = v
        for b in writes:
            b.w = ticket
            b.r = {}

    def op(self, e, fn, reads=(), writes=()):
        own = "c_" + e
        for b in reads:
            self._wait(e, b.w)
        for b in writes:
            if b.w is not None and (b.w[0] != own or e != "pe"):
                self._wait(e, b.w)
            for k, v in b.r.items():
                if k != own or e != "pe":
                    self._wait(e, (k, v))
        inst = fn(self.eng[e])
        self.cnt[own] += 1
        inst.then_inc(self.sems[own], 1)
        self._mark((own, self.cnt[own]), reads, writes)
        self.n_inst += 1
        return inst

    def dma(self, q, out, in_, reads=(), writes=(), **kw):
        keys, idx = self.dma_pool[q]
        key = keys[idx % len(keys)]
        self.dma_pool[q][1] = idx + 1
        if self.cnt[key] > 0:
            self._wait(q, (key, self.cnt[key]))
        for b in reads:
            self._wait(q, b.w)
        for b in writes:
            self._wait(q, b.w)
            for k, v in b.r.items():
                self._wait(q, (k, v))
        inst = self.eng[q].dma_start(out=out, in_=in_, **kw)
        self.cnt[key] += 16
        inst.then_inc(self.sems[key], 16)
        self._mark((key, self.cnt[key]), reads, writes)
        self.n_inst += 1
        return inst

    def barrier(self):
        for e in self.eng:
            for key, val in self.cnt.items():
                if val > 0:
                    self._wait(e, (key, val))

    def finish(self, e, bufs):
        for b in bufs:
            self._wait(e, b.w)


class T_:
    def __init__(self, t, name):
        self.t = t
        self.b = Buf(name)

    def __getitem__(self, k):
        return self.t[k]

    def v(self, k):
        return View(self.t[k], self.b)


class _APWrap:
    def __init__(self, ap):
        self.ap = ap

    def __getitem__(self, k):
        return self.ap[k]


class View:
    def __init__(self, ap, b):
        self.ap = ap
        self.b = b


def _ap(x):
    return x.ap if isinstance(x, View) else x.t[:]


VC_C, VC_NMIX0, VC_NFFN0, VC_NMIX1, VC_NFFN1, VC_NKV = 0, 1, 2, 3, 4, 5
VC_MU = 6
VC_ADAB0 = 12
VC_ADAB1 = 18
VC_KVB = 24
N_VC = 26
VR_KK, VR_KA, VR_RK, VR_GNG, VR_GNB, VR_W0, VR_A0, VR_FING = 0, 1, 2, 3, 4, 5, 6, 7
VR_GM0, VR_GF0, VR_GM1, VR_GF1 = 8, 9, 10, 11
VR_SUBLN = 12
N_VR = 13


def build_program(T, dbg=0, phases=("p0", "p1", "p2", "p3", "p4", "p5", "moe")):
    NT = T // 128
    nc = bass.Bass("TRN2", target_bir_lowering=False)
    kb = KB(nc)
    es = ExitStack()

    in_names = []

    def din(name, shape, dt=F32, need=True):
        if not need:
            return None
        in_names.append(name)
        return nc.dram_tensor(name, list(shape), dt, kind="ExternalInput").ap()

    has = lambda ph: ph in phases

    dbg_outs = []

    def dscr(name, shape, dt=F32, tap=False):
        kind = "ExternalOutput" if (dbg and tap) else "Internal"
        if dbg and tap:
            dbg_outs.append(name)
        return nc.dram_tensor(name, list(shape), dt, kind=kind).ap()

    x_in = din("x", [T, D])
    vec_col = din("vec_col", [128, N_VC, 8])
    vec_row = din("vec_row", [N_VR, D])
    ada_w = din("ada_w", [2, D, 6 * D])
    ada_kv_w = din("ada_kv_w", [D, 2 * D])
    w_rkv = din("rw_w_rkv", [3, D, D], need=has("p1"))
    w1a_in = din("rw_w1a", [D, 128], need=has("p1"))
    g1_in = din("rw_g1", [D, 128], need=has("p1"))
    w2a2_in = din("rw_w2a2", [128, D], need=has("p1"))
    g2_in = din("rw_g2", [128, D], need=has("p1"))
    rw_wo = din("rw_w_o", [D, D], need=has("p3"))
    w_kv = din("w_kv", [D, 2 * D], need=has("p4"))
    df_wq = din("df_w_q", [D, D], need=has("p5"))
    df_wo = din("df_w_o", [D, D], need=has("p5"))
    moe_wr = din("moe_wr", [2, D, 36], need=has("moe"))
    moe_wg = din("moe_w_gate", [2, NEXP * 128, 4096], need=has("moe"))
    moe_wu = din("moe_w_up", [2, NEXP * 128, 4096], need=has("moe"))
    moe_wd = din("moe_w_down", [2, NEXP * 128, 4096], need=has("moe"))
    sel_in = din("sel", [128, 4], need=has("p5"))
    cmask_in = din("cmask", [2, 128, 128], need=has("p5"))
    lam_in = din("lam", [128, 4], need=has("p5"))
    out = nc.dram_tensor("out", [T // 2, D], F32, kind="ExternalOutput").ap()

    XF = [dscr(f"xf{q}", [NT, 128, D], BF16, tap=(dbg == 1)) for q in range(4)]
    TKV = [dscr(f"tk{q}", [T, D], BF16, tap=(dbg == 1)) for q in range(3)]
    GSC = dscr("gsc", [T, D], F32, tap=(dbg == 1))
    BON = dscr("bon", [T, D], F32, tap=(dbg == 1))
    GAMD = dscr("gamd", [128, 8, 2 * NT], F32, tap=(dbg == 1))
    YSC = dscr("ysc", [T, D], F32, tap=(dbg == 2))

    def sb(name, shape, dt=F32):
        t = es.enter_context(nc.sbuf_tensor(name, list(shape), dt))
        return T_(t, name)

    def ps(name, shape, dt=F32):
        t = es.enter_context(nc.psum_tensor(name, list(shape), dt))
        return T_(t, name)

    ident = sb("ident", [128, 128])
    identb = sb("identb", [128, 128], BF16)
    ones = sb("ones", [128, 128])
    onesb = sb("onesb", [128, 128], BF16)
    vcol = sb("vcol", [128, N_VC, 8])
    modc = sb("modc", [128, 12, 8])
    AB = sb("AB", [128, 12, 8])
    cact = sb("cact", [128, 8])

    kb.op("pool", lambda e: e.memset(ident[:], 0.0), writes=[ident.b])
    kb.op("pool", lambda e: e.affine_select(out=ident[:], in_=ident[:], pattern=[[-1, 128]], compare_op=ALU.not_equal,
                                            fill=1.0, base=0, channel_multiplier=1), reads=[ident.b], writes=[ident.b])
    kb.op("pool", lambda e: e.tensor_copy(out=identb[:], in_=ident[:]), reads=[ident.b], writes=[identb.b])
    kb.op("pool", lambda e: e.memset(ones[:], 1.0), writes=[ones.b])
    kb.op("pool", lambda e: e.memset(onesb[:], 1.0), writes=[onesb.b])
    kb.dma("sp", vcol[:], vec_col[:, :, :], writes=[vcol.b])
    kb.op("act", lambda e: e.activation(out=cact[:], in_=vcol[:, VC_C, :], func=AF.Silu), reads=[vcol.b], writes=[cact.b])

    def bc_row(dst, row):
        kb.dma("sp", dst[:], row.partition_broadcast(128), writes=[dst.b])

    gmb = [sb(f"gmb{i}", [128, D]) for i in range(4)]
    with ExitStack() as es0:
        wst = [T_(es0.enter_context(nc.sbuf_tensor(f"wst{i}", [128, 8, D], F32)), f"wst{i}") for i in range(2)]
        cbc = T_(es0.enter_context(nc.sbuf_tensor("cbc", [128, 8, 128], F32)), "cbc")
        pcol = T_(es0.enter_context(nc.psum_tensor("pcol", [128, 8], F32)), "pcol")
        prow = T_(es0.enter_context(nc.psum_tensor("prow", [128, D], F32)), "prow")
        brow = T_(es0.enter_context(nc.sbuf_tensor("brow", [128, D], F32)), "brow")
        for m in range(8):
            kb.op("dve", lambda e, m=m: e.tensor_scalar(out=cbc[:, m, :], in0=ones[:], scalar1=cact[:, m:m + 1], scalar2=None,
                                                        op0=ALU.mult), reads=[ones.b, cact.b], writes=[cbc.b])
        jobs = []
        for l in range(2):
            base = VC_ADAB0 if l == 0 else VC_ADAB1
            jobs += [(ada_w[l], 0 * D, "col", 4 * l + 0, base + 0), (ada_w[l], 1 * D, "col", 4 * l + 1, base + 1),
                     (ada_w[l], 3 * D, "col", 4 * l + 2, base + 3), (ada_w[l], 4 * D, "col", 4 * l + 3, base + 4),
                     (ada_w[l], 2 * D, "row", 2 * l + 0, VR_GM0 + 2 * l), (ada_w[l], 5 * D, "row", 2 * l + 1, VR_GF0 + 2 * l)]
        jobs += [(ada_kv_w, 0, "col", 8, VC_KVB), (ada_kv_w, D, "col", 9, VC_KVB + 1)]
        for ji, (src, off, kind, di, bi) in enumerate(jobs):
            w = wst[ji % 2]
            kb.dma("sp" if ji % 2 == 0 else "pool", w[:], src[:, off:off + D].rearrange("(m p) n -> p m n", p=128), writes=[w.b])
            if kind == "col":
                for fc in range(8):
                    for mk in range(8):
                        kb.op("pe", lambda e, fc=fc, mk=mk, w=w: e.matmul(pcol[:, fc:fc + 1], lhsT=w[:, mk, fc * 128:(fc + 1) * 128],
                                                                           rhs=cact[:, mk:mk + 1], start=(mk == 0), stop=(mk == 7)),
                              reads=[w.b, cact.b], writes=[pcol.b])
                kb.op("dve", lambda e, di=di, bi=bi: e.tensor_tensor(out=modc[:, di, :], in0=pcol[:], in1=vcol[:, bi, :], op=ALU.add),
                      reads=[pcol.b, vcol.b], writes=[modc.b])
            else:
                for hf in range(2):
                    for mk in range(8):
                        kb.op("pe", lambda e, hf=hf, mk=mk, w=w: e.matmul(prow[:, hf * 512:(hf + 1) * 512], lhsT=cbc[:, mk, :],
                                                                           rhs=w[:, mk, hf * 512:(hf + 1) * 512], start=(mk == 0), stop=(mk == 7)),
                              reads=[w.b, cbc.b], writes=[prow.b])
                bc_row(brow, vec_row[bi:bi + 1, :])
                kb.op("dve", lambda e, di=di: e.tensor_tensor(out=gmb[di][:], in0=prow[:], in1=brow[:], op=ALU.add),
                      reads=[prow.b, brow.b], writes=[gmb[di].b])
        for (ai, gi, shi, sci) in [(0, VC_NMIX0, 0, 1), (2, VC_NFFN0, 2, 3), (4, VC_NMIX1, 4, 5), (6, VC_NFFN1, 6, 7), (8, VC_NKV, 8, 9)]:
            kb.op("dve", lambda e, ai=ai, sci=sci: e.tensor_scalar(out=AB[:, ai, :], in0=modc[:, sci, :], scalar1=1.0, scalar2=32.0,
                                                                  op0=ALU.add, op1=ALU.mult), reads=[modc.b], writes=[AB.b])
            kb.op("dve", lambda e, ai=ai, gi=gi: e.tensor_tensor(out=AB[:, ai, :], in0=AB[:, ai, :], in1=vcol[:, gi, :], op=ALU.mult),
                  reads=[AB.b, vcol.b], writes=[AB.b])
            kb.op("dve", lambda e, ai=ai, shi=shi: e.tensor_copy(out=AB[:, ai + 1, :], in_=modc[:, shi, :]), reads=[modc.b], writes=[AB.b])

    def TT(e, o, a, b_, op):
        kb.op(e, lambda en: en.tensor_tensor(out=_ap(o), in0=_ap(a), in1=_ap(b_), op=op), reads=[a.b, b_.b], writes=[o.b])

    def TS(e, o, a, s1, s2, op0, op1=None):
        rd = [a.b] + [z.b for z in (s1, s2) if isinstance(z, (View, T_))]
        f = lambda z: _ap(z) if isinstance(z, (View, T_)) else z
        if op1 is None:
            kb.op(e, lambda en: en.tensor_scalar(out=_ap(o), in0=_ap(a), scalar1=f(s1), scalar2=None, op0=op0), reads=rd, writes=[o.b])
        else:
            kb.op(e, lambda en: en.tensor_scalar(out=_ap(o), in0=_ap(a), scalar1=f(s1), scalar2=f(s2), op0=op0, op1=op1), reads=rd, writes=[o.b])

    def STT(e, o, a, sc, b_, op0, op1):
        rd = [a.b, b_.b] + ([sc.b] if isinstance(sc, (View, T_)) else [])
        f = lambda z: _ap(z) if isinstance(z, (View, T_)) else z
        kb.op(e, lambda en: en.scalar_tensor_tensor(out=_ap(o), in0=_ap(a), scalar=f(sc), in1=_ap(b_), op0=op0, op1=op1), reads=rd, writes=[o.b])

    def ACT(o, a, func, scale=1.0, bias=0.0, accum=None):
        rd = [a.b] + [z.b for z in (scale, bias) if isinstance(z, (View, T_))]
        wr = [o.b] + ([accum.b] if accum is not None else [])
        f = lambda z: _ap(z) if isinstance(z, (View, T_)) else z
        kw = {}
        if accum is not None:
            kw["accum_out"] = _ap(accum)
        kb.op("act", lambda en: en.activation(out=_ap(o), in_=_ap(a), func=func, bias=f(bias), scale=f(scale), **kw), reads=rd, writes=wr)

    def CP(e, o, a):
        if e == "act":
            kb.op(e, lambda en: en.copy(out=_ap(o), in_=_ap(a)), reads=[a.b], writes=[o.b])
        else:
            kb.op(e, lambda en: en.tensor_copy(out=_ap(o), in_=_ap(a)), reads=[a.b], writes=[o.b])

    def RED(e, o, a, op=ALU.add, axis=AX.X):
        kb.op(e, lambda en: en.tensor_reduce(out=_ap(o), in_=_ap(a), axis=axis, op=op), reads=[a.b], writes=[o.b])

    def MM(o, lhsT, rhs, start=True, stop=True):
        kb.op("pe", lambda en: en.matmul(_ap(o), lhsT=_ap(lhsT), rhs=_ap(rhs), start=start, stop=stop), reads=[lhsT.b, rhs.b], writes=[o.b])

    def TR(o, a, idt):
        kb.op("pe", lambda en: en.transpose(out=_ap(o), in_=_ap(a), identity=_ap(idt)), reads=[a.b, idt.b], writes=[o.b])

    def DMA(q, o_ap, i_ap, reads=(), writes=()):
        kb.dma(q, o_ap, i_ap, reads=[r.b for r in reads], writes=[w.b for w in writes])

    class DR:
        def __init__(self, name):
            self.b = Buf(name)

    taps = {}
    if dbg == 9:
        o_ab = nc.dram_tensor("o_ab", [128, 12, 8], F32, kind="ExternalOutput").ap()
        o_gm = nc.dram_tensor("o_gm", [4, 128, D], F32, kind="ExternalOutput").ap()
        bo = Buf("o")
        kb.dma("sp", o_ab[:, :, :], AB[:], reads=[AB.b], writes=[bo])
        for i in range(4):
            kb.dma("sp", o_gm[i], gmb[i][:], reads=[gmb[i].b], writes=[bo])
        kb.finish("sp", [bo])
        kb.close()
        es.close()
        return nc, in_names, ["o_ab", "o_gm"]

    gamT = sb("gamT", [128, 8, 2 * NT])
    dXF = [[DR(f"xf{q}_{t}") for t in range(NT)] for q in range(4)]
    dTK = [[DR(f"tk{q}_{t}") for t in range(NT)] for q in range(3)]
    dG = [DR(f"g{t}") for t in range(NT)]
    dBON = [DR(f"bon{t}") for t in range(NT)]
    rowsl = lambda t: slice(t * 128, (t + 1) * 128)
    kb.barrier()
    if has("p1"):
        with ExitStack() as es1:
            def sb1(name, shape, dt=F32):
                return T_(es1.enter_context(nc.sbuf_tensor(name, list(shape), dt)), name)

            def ps1(name, shape, dt=F32):
                return T_(es1.enter_context(nc.psum_tensor(name, list(shape), dt)), name)

            Wp = [sb1(f"Wp{i}", [128, 8, D], BF16) for i in range(3)]
            W1 = [sb1(f"W1{i}", [128, 8, 128], BF16) for i in range(4)]
            w2a2 = sb1("w2a2", [128, D], BF16)
            g2 = sb1("g2", [128, D], BF16)
            b0row = sb1("b0row", [1, 2, D])
            kk_bc, ka_bc, omka_bc, rk_bc = [sb1(n, [128, D]) for n in ("kk_bc", "ka_bc", "omka_bc", "rk_bc")]
            hT = sb1("hT", [128, 8, 129])
            F = [sb1(f"F{i}", [128, D]) for i in range(16)]
            Hb = [sb1(f"Hb{i}", [128, 8, 128], BF16) for i in range(5)]
            Q = [sb1(f"Q{i}", [128, D], BF16) for i in range(7)]
            XFs = [sb1(f"XFs{i}", [128, D], BF16) for i in range(2)]
            wlal = sb1("wlal", [128, 128], BF16)
            glT = sb1("glT", [128, 128], BF16)
            Ltri = sb1("Ltri", [128, 128])
            Lblk = sb1("Lblk", [128, 128])
            ind2 = sb1("ind2", [128, 2])
            ss = sb1("ss", [128, 1])
            rs = sb1("rs", [128, 1])
            ssh = sb1("ssh", [128, 16])
            rnh = sb1("rnh", [128, 16])
            bsum = sb1("bsum", [128, 16])
            PA, PB, PC = [ps1(n, [128, D]) for n in ("PA", "PB", "PC")]
            PS = ps1("PS", [128, 512])
            PT = ps1("PT", [128, D], BF16)
            cs = lambda m: slice(m * 128, (m + 1) * 128)
            hs = lambda h: slice(h * 512, (h + 1) * 512)

            engs = ["act", "dve", "pool"]
            n = 0
            for i in range(3):
                for m in range(8):
                    st = F[14 + n % 2]
                    DMA("sp" if n % 2 == 0 else "pool", st[:], w_rkv[i, cs(m), :], writes=[st])
                    CP(engs[n % 3], Wp[i].v((slice(None), m, slice(None))), st)
                    n += 1
            st3 = lambda t: View(t.t[:].rearrange("p (m n) -> p m n", n=128), t.b)
            for j, (src, mus) in enumerate([(w1a_in, (3, 4)), (g1_in, (5, 5))]):
                st = F[13]
                DMA("sp", st3(st).ap, src.rearrange("(m p) n -> p m n", p=128), writes=[st])
                CP("dve", W1[2 * j], st3(st))
                for m in range(8):
                    for hh in range(2):
                        TS("pool" if hh else "dve", W1[2 * j + 1].v((slice(None), m, slice(hh * 64, hh * 64 + 64))),
                           View(st3(st).ap[:, m, hh * 64:hh * 64 + 64], st.b), vcol.v((slice(None), VC_MU + mus[hh], slice(m, m + 1))), None, ALU.mult)
            DMA("sp", F[12][:], w2a2_in[:, :], writes=[F[12]])
            CP("act", w2a2, F[12])
            DMA("sp", F[11][:], g2_in[:, :], writes=[F[11]])
            CP("act", g2, F[11])
            DMA("sp", b0row[0:1, 0, :], vec_row[VR_W0:VR_W0 + 1, :], writes=[b0row])
            DMA("sp", b0row[0:1, 1, :], vec_row[VR_A0:VR_A0 + 1, :], writes=[b0row])
            bc_row(kk_bc, vec_row[VR_KK:VR_KK + 1, :])
            bc_row(ka_bc, vec_row[VR_KA:VR_KA + 1, :])
            bc_row(rk_bc, vec_row[VR_RK:VR_RK + 1, :])
            TS("dve", omka_bc, ka_bc, -1.0, 1.0, ALU.mult, ALU.add)
            kb.op("pool", lambda e: e.memset(Lblk[:], 0.0), writes=[Lblk.b])
            kb.op("pool", lambda e: e.memset(Lblk[0:64, 0:64], 1.0), writes=[Lblk.b])
            kb.op("pool", lambda e: e.memset(Lblk[64:128, 64:128], 1.0), writes=[Lblk.b])
            CP("pool", Ltri, Lblk)
            for h0 in (0, 64):
                kb.op("pool", lambda e, h0=h0: e.affine_select(out=Ltri[h0:h0 + 64, h0:h0 + 64], in_=Ltri[h0:h0 + 64, h0:h0 + 64], pattern=[[1, 64]],
                                                              compare_op=ALU.is_ge, fill=0.0, base=0, channel_multiplier=-1),
                      reads=[Ltri.b], writes=[Ltri.b])
            kb.op("pool", lambda e: e.memset(ind2[:], 0.0), writes=[ind2.b])
            kb.op("pool", lambda e: e.memset(ind2[0:64, 0:1], 1.0), writes=[ind2.b])
            kb.op("pool", lambda e: e.memset(ind2[64:128, 1:2], 1.0), writes=[ind2.b])
            kb.op("pool", lambda e: e.memset(hT[:, :, 0:1], 0.0), writes=[hT.b])

            v3 = lambda t: View(t.t[:].rearrange("p (m n) -> p m n", n=128), t.b)
            vh = lambda t: View(t.t[:].rearrange("p (h n) -> p h n", n=64), t.b)
            bch = lambda t: View(t.t[:].unsqueeze(2).broadcast_to([128, 16, 64]), t.b)
            for tt in range(NT):
                DMA("sp", F[0][:], x_in[rowsl(tt), :], writes=[F[0]])
                ACT(F[1], F[0], AF.Square, accum=ss)
                ACT(rs, ss, AF.Ln, bias=1024e-6)
                ACT(rs, rs, AF.Exp, scale=-0.5)
                TS("dve", F[1], F[0], rs, None, ALU.mult)
                for m in range(8):
                    TR(PA.v((slice(None), cs(m))), F[1].v((slice(None), cs(m))), ident)
                for m in range(8):
                    ACT(hT.v((slice(None), m, slice(1, 129))), PA.v((slice(None), cs(m))), AF.Identity,
                        scale=AB.v((slice(None), 0, slice(m, m + 1))), bias=AB.v((slice(None), 1, slice(m, m + 1))))
                hprev = hT.v((slice(None), slice(None), slice(0, 128)))
                hcur = hT.v((slice(None), slice(None), slice(1, 129)))
                TT("dve", v3(F[2]), hprev, hcur, ALU.subtract)
                CP("pool", Hb[0], hcur)
                CP("pool", Hb[1], v3(F[2]))
                n = 0
                for i in range(3):
                    for m in range(8):
                        STT("dve", Hb[2 + i].v((slice(None), m, slice(None))), F[2].v((slice(None), cs(m))),
                            vcol.v((slice(None), VC_MU + i, slice(m, m + 1))), hT.v((slice(None), m, slice(1, 129))), ALU.mult, ALU.add)
                        n += 1
                CP("pool", hT.v((slice(None), slice(None), slice(0, 1))), hT.v((slice(None), slice(None), slice(128, 129))))
                for j in range(2):
                    o = PS.v((slice(None), slice(j * 128, (j + 1) * 128)))
                    for mk in range(8):
                        MM(o, W1[2 * j].v((slice(None), mk, slice(None))), Hb[0].v((slice(None), mk, slice(None))), start=(mk == 0), stop=False)
                        MM(o, W1[2 * j + 1].v((slice(None), mk, slice(None))), Hb[1].v((slice(None), mk, slice(None))), start=False, stop=(mk == 7))
                ACT(wlal.v((slice(0, 64), slice(None))), PS.v((slice(0, 64), slice(0, 128))), AF.Tanh)
                ACT(wlal.v((slice(64, 128), slice(None))), PS.v((slice(64, 128), slice(0, 128))), AF.Identity)
                ACT(glT, PS.v((slice(None), slice(128, 256))), AF.Sigmoid)
                for i, P_ in enumerate((PB, PC, PA)):
                    for h in range(2):
                        for mk in range(8):
                            MM(P_.v((slice(None), hs(h))), Hb[2 + i].v((slice(None), mk, slice(None))), Wp[i].v((slice(None), mk, hs(h))),
                               start=(mk == 0), stop=(mk == 7))
                CP("dve", F[0], PB)
                CP("act", F[1], PC)
                CP("act", F[2], PA)
                for h in range(2):
                    MM(PB.v((slice(None), hs(h))), wlal.v((slice(0, 64), slice(None))), w2a2.v((slice(0, 64), hs(h))), start=True, stop=False)
                    MM(PB.v((slice(None), hs(h))), ones.v((slice(0, 1), slice(None))), b0row.v((slice(0, 1), 0, hs(h))), start=False, stop=True)
                for h in range(2):
                    MM(PC.v((slice(None), hs(h))), wlal.v((slice(64, 128), slice(None))), w2a2.v((slice(64, 128), hs(h))), start=True, stop=False)
                    MM(PC.v((slice(None), hs(h))), ones.v((slice(0, 1), slice(None))), b0row.v((slice(0, 1), 1, hs(h))), start=False, stop=True)
                for h in range(2):
                    MM(PA.v((slice(None), hs(h))), glT, g2.v((slice(None), hs(h))))
                ACT(F[3], PB, AF.Sigmoid)
                ACT(F[4], PC, AF.Sigmoid)
                CP("act", F[5], PA)
                DMA("pool", GSC[rowsl(tt), :], F[5][:], reads=[F[5]], writes=[dG[tt]])
                for h in range(2):
                    MM(PB.v((slice(None), hs(h))), Ltri, F[3].v((slice(None), hs(h))))
                for h in range(2):
                    MM(PC.v((slice(None), hs(h))), Lblk, F[3].v((slice(None), hs(h))))
                for m in range(8):
                    MM(PS.v((slice(None), slice(256 + 2 * m, 258 + 2 * m))), F[3].v((slice(None), cs(m))), ind2)
                ACT(F[11], PB, AF.Exp, scale=C0)
                ACT(F[13], PB, AF.Exp, scale=-C0)
                ACT(F[10], F[3], AF.Exp, scale=-C0)
                ACT(F[14], PC, AF.Exp, scale=C0)
                ACT(gamT.v((slice(None), slice(None), slice(2 * tt, 2 * tt + 2))),
                    View(PS.t[:, 256:272].rearrange("p (m c) -> p m c", c=2), PS.b), AF.Exp, scale=C0)
                TT("pool", F[12], F[11], F[10], ALU.mult)
                TT("pool", F[14], F[14], F[13], ALU.mult)
                TT("dve", F[6], F[1], kk_bc, ALU.mult)
                TT("pool", F[7], F[6], F[6], ALU.mult)
                RED("dve", ssh, vh(F[7]))
                ACT(rnh, ssh, AF.Ln, bias=1e-12)
                ACT(rnh, rnh, AF.Exp, scale=-0.5)
                TT("dve", vh(F[6]), vh(F[6]), bch(rnh), ALU.mult)
                TT("pool", F[7], F[4], ka_bc, ALU.mult)
                TT("pool", F[7], F[7], omka_bc, ALU.add)
                TT("dve", F[8], F[1], F[7], ALU.mult)
                TT("pool", F[9], F[6], F[4], ALU.mult)
                STT("dve", Q[0], F[6], -1.0, F[12], ALU.mult, ALU.mult)
                TT("dve", Q[1], F[0], F[11], ALU.mult)
                TT("pool", Q[2], F[9], F[13], ALU.mult)
                TT("dve", Q[3], F[8], F[13], ALU.mult)
                TT("pool", Q[4], F[9], F[14], ALU.mult)
                TT("dve", Q[5], F[8], F[14], ALU.mult)
                CP("pool", Q[6], F[2])
                DMA("pool", TKV[0][rowsl(tt), :], Q[6][:], reads=[Q[6]], writes=[dTK[0][tt]])
                DMA("pool", TKV[1][rowsl(tt), :], Q[4][:], reads=[Q[4]], writes=[dTK[1][tt]])
                DMA("pool", TKV[2][rowsl(tt), :], Q[5][:], reads=[Q[5]], writes=[dTK[2][tt]])
                for q in range(4):
                    for m in range(8):
                        TR(PT.v((slice(None), cs(m))), Q[q].v((slice(None), cs(m))), identb)
                    CP("act" if q % 2 == 0 else "dve", XFs[q % 2], PT)
                    DMA("sp", XF[q][tt], XFs[q % 2][:], reads=[XFs[q % 2]], writes=[dXF[q][tt]])
                TT("pool", F[7], F[0], F[8], ALU.mult)
                TT("pool", F[7], F[7], rk_bc, ALU.mult)
                RED("dve", bsum, vh(F[7]))
                TT("dve", vh(F[15]), vh(F[2]), bch(bsum), ALU.mult)
                DMA("pool", BON[rowsl(tt), :], F[15][:], reads=[F[15]], writes=[dBON[tt]])
    dGAM = DR("gamd")
    if dbg == 1:
        DMA("sp", GAMD[:, :, :], gamT[:], reads=[gamT], writes=[dGAM])
        allb = [d.b for l_ in dXF + dTK for d in l_] + [d.b for d in dG + dBON] + [dGAM.b]
        kb.finish("sp", allb)
        kb.finish("pool", allb)
        kb.close()
        es.close()
        return nc, in_names, dbg_outs

    NC = T // 64
    dY = [DR(f"y{c}") for c in range(NC)]
    kb.barrier()
    if has("p2"):
        with ExitStack() as es2:
            def sb2(name, shape, dt=F32):
                return T_(es2.enter_context(nc.sbuf_tensor(name, list(shape), dt)), name)

            def ps2(name, shape, dt=F32):
                return T_(es2.enter_context(nc.psum_tensor(name, list(shape), dt)), name)

            XFt = [[sb2(f"XFt{q}_{i}", [64, 16, 64], BF16) for q in range(4)] for i in range(2)]
            TKt = [[sb2(f"TKt{q}_{i}", [64, 16, 64], BF16) for q in range(3)] for i in range(2)]
            m_su, m_iu, m_sl, I16 = [sb2(n, [64, 16, 64]) for n in ("m_su", "m_iu", "m_sl", "I16")]
            Pm = [sb2(f"Pm{i}", [64, 16, 64], BF16) for i in range(2)]
            PTm = [sb2(f"PTm{i}", [64, 16, 64], BF16) for i in range(2)]
            Tm = sb2("Tm", [64, 16, 64], BF16)
            Aak, Arb, Ark = [sb2(n, [64, 16, 64], BF16) for n in ("Aak", "Arb", "Ark")]
            W1T = sb2("W1T", [64, 16, 64], BF16)
            UT = sb2("UT", [64, 16, 64], BF16)
            ST = sb2("ST", [64, 16, 64])
            STb = sb2("STb", [64, 16, 64], BF16)
            gam = sb2("gam", [64, 16, NC])
            Ysb = [sb2(f"Ysb{i}", [64, 16, 64]) for i in range(2)]
            PG = [ps2(f"PG{i}", [64, 16, 64]) for i in range(2)]
            PW = ps2("PW", [64, 16, 64])
            PSt = ps2("PSt", [64, 16, 64])
            A_ = slice(None)

            for (mt, pat, cm, op) in [(m_su, [[0, 16], [1, 64]], -1, ALU.is_gt), (m_iu, [[0, 16], [1, 64]], -1, ALU.is_ge),
                                      (m_sl, [[0, 16], [-1, 64]], 1, ALU.is_gt), (I16, [[0, 16], [1, 64]], -1, ALU.is_equal)]:
                kb.op("pool", lambda e, mt=mt: e.memset(mt[:], 1.0), writes=[mt.b])
                kb.op("pool", lambda e, mt=mt, pat=pat, cm=cm, op=op: e.affine_select(
                    out=mt[:], in_=mt[:], pattern=pat, compare_op=op, fill=0.0, base=0, channel_multiplier=cm),
                    reads=[mt.b], writes=[mt.b])
            kb.op("pool", lambda e: e.memset(ST[:], 0.0), writes=[ST.b])
            kb.op("pool", lambda e: e.memset(STb[:], 0.0), writes=[STb.b])
            DMA("sp", GAMD[:, :, :], gamT[:], reads=[gamT], writes=[dGAM])
            DMA("sp", gam[:].rearrange("j (m p) c -> j m p c", p=2), GAMD.rearrange("(p j) m c -> j m p c", p=2), reads=[dGAM], writes=[gam])

            def loads(c):
                i = c % 2
                tt, ci = c // 2, c % 2
                for q in range(4):
                    src = XF[q][tt].rearrange("(p j) (m t) -> j m p t", p=2, t=128)[:, :, :, ci * 64:(ci + 1) * 64]
                    DMA("sp", XFt[i][q][:].rearrange("j (m p) t -> j m p t", p=2), src, reads=[dXF[q][tt]], writes=[XFt[i][q]])
                for q in range(3):
                    DMA("sp", TKt[i][q][:].rearrange("s h n -> s (h n)"), TKV[q][c * 64:(c + 1) * 64, :], reads=[dTK[q][tt]], writes=[TKt[i][q]])

            def headmm(o, lt, rt, **kw):
                for h in range(16):
                    MM(o.v((A_, h, A_)), lt.v((A_, h, A_)), rt.v((A_, h, A_)), **kw)

            P2STOP = int(os.environ.get("P2STOP", "0"))
            loads(0)
            for c in range(NC):
                if c + 1 < NC:
                    loads(c + 1)
                At_, Rt_, Bt_, Kt_ = XFt[c % 2]
                Vt_, Bh_, Kh_ = TKt[c % 2]
                headmm(PG[0], Bt_, At_)
                headmm(PG[1], At_, Bt_)
                TT("dve", Pm[0], PG[0], m_su, ALU.mult)
                TT("dve", PTm[0], PG[1], m_sl, ALU.mult)
                TT("pool", Tm, Pm[0], I16, ALU.add)
                headmm(PG[0], Kt_, At_)
                headmm(PG[1], Bt_, Rt_)
                TT("dve", Aak, PG[0], m_su, ALU.mult)
                TT("dve", Arb, PG[1], m_iu, ALU.mult)
                headmm(PG[0], Kt_, Rt_)
                TT("dve", Ark, PG[0], m_iu, ALU.mult)
                if P2STOP == 2:
                    continue
                cur = 0
                for lvl in range(1, 6):
                    nxt = 1 - cur
                    headmm(PG[1], Pm[cur], PTm[cur])
                    if lvl < 5:
                        headmm(PG[0], PTm[cur], Pm[cur])
                    CP("act", PTm[nxt], PG[1])
                    if lvl < 5:
                        CP("act", Pm[nxt], PG[0])
                    pg = PG[0] if lvl == 5 else PG[1]
                    headmm(pg, PTm[nxt], Tm)
                    TT("dve", Tm, Tm, pg, ALU.add)
                    cur = nxt
                if P2STOP == 3:
                    continue
                Y_ = Ysb[c % 2]
                for h in range(16):
                    MM(PW.v((A_, h, A_)), At_.v((A_, h, A_)), STb.v((A_, h, A_)), start=True, stop=False)
                    MM(PW.v((A_, h, A_)), Aak.v((A_, h, A_)), Vt_.v((A_, h, A_)), start=False, stop=True)
                CP("act", W1T, PW)
                headmm(PW, Tm, W1T)
                CP("act", UT, PW)
                for h in range(16):
                    MM(PSt.v((A_, h, A_)), Bh_.v((A_, h, A_)), UT.v((A_, h, A_)), start=True, stop=False)
                    MM(PSt.v((A_, h, A_)), Kh_.v((A_, h, A_)), Vt_.v((A_, h, A_)), start=False, stop=True)
                for h in range(16):
                    MM(PW.v((A_, h, A_)), Rt_.v((A_, h, A_)), STb.v((A_, h, A_)), start=True, stop=False)
                    MM(PW.v((A_, h, A_)), Arb.v((A_, h, A_)), UT.v((A_, h, A_)), start=False, stop=False)
                    MM(PW.v((A_, h, A_)), Ark.v((A_, h, A_)), Vt_.v((A_, h, A_)), start=False, stop=True)
                TT("dve", ST, ST, View(gam.t[:, :, c:c + 1].broadcast_to([64, 16, 64]), gam.b), ALU.mult)
                TT("dve", ST, ST, PSt, ALU.add)
                CP("act", Y_, PW)
                CP("dve", STb, ST)
                DMA("pool", YSC[c * 64:(c + 1) * 64, :], Y_[:].rearrange("p h n -> p (h n)"), reads=[Y_], writes=[dY[c]])
    if dbg == 2:
        allb = [d.b for d in dY]
        kb.finish("sp", allb)
        kb.finish("pool", allb)
        kb.close()
        es.close()
        return nc, in_names, dbg_outs

    X1 = dscr("x1s", [T, D], F32, tap=(dbg == 3))
    dX1 = [DR(f"x1_{t}") for t in range(NT)]
    GT = 8 if NT >= 8 else NT
    cs = lambda m: slice(m * 128, (m + 1) * 128)
    hs = lambda h: slice(h * 512, (h + 1) * 512)
    A_ = slice(None)

    class FFN2:
        def __init__(self, es_, tag, NTL):
            def sbx(name, shape, dt=F32):
                return T_(es_.enter_context(nc.sbuf_tensor(name + tag, list(shape), dt)), name)

            def psx(name, shape, dt=F32):
                return T_(es_.enter_context(nc.psum_tensor(name + tag, list(shape), dt)), name)
            self.tag, self.NTL = tag, NTL
            self.BR = 512
            self.SUB = self.BR // 128
            self.NB = (NTL * 256 + self.BR - 1) // self.BR + 32
            NB = self.NB
            self.XA = dscr("xa" + tag, [NTL * 128, D], F32)
            self.HTOK = dscr("htok" + tag, [NTL * 128, D], BF16)
            self.HS = dscr("hs" + tag, [NB * self.BR, D], BF16)
            self.YS = dscr("ys" + tag, [NB * self.BR, D], F32)
            self.dXA = [DR("xa") for _ in range(NTL)]
            self.dHT = [DR("ht") for _ in range(NTL)]
            self.dHS = [DR("hs") for _ in range(2 * NTL)]
            self.dYS = [DR("ys") for _ in range(self.SUB * NB)]
            self.xa = sbx("xa_sb", [128, D])
            self.xn = sbx("xn_f", [128, D])
            self.hTf = sbx("hTf", [128, 8, 128])
            self.htk = sbx("htk", [128, D], BF16)
            self.wr = sbx("wr", [128, 8, 36])
            self.sm = sbx("sm", [128, 96])
            self.OH = sbx("OH", [128, NTL, 2, 32], BF16)
            self.ohs = sbx("ohs", [128, 32], BF16)
            self.rk = sbx("rk", [128, NTL, 2])
            self.wk = sbx("wk", [128, NTL, 2])
            self.dstf = sbx("dstf", [128, NTL, 2])
            self.dsti = sbx("dsti", [128, NTL, 2], I32)
            self.base = sbx("base", [128, 32])
            self.t32 = [sbx(f"t32{i}", [128, 32]) for i in range(3)]
            self.Ls = sbx("Ls", [128, 128], BF16)
            self.bst = sbx("bst", [128, NB])
            self.blke = sbx("blke", [128, NB])
            self.widx = sbx("widx", [128, NB], I32)
            self.pcol = sbx("pcol", [128, 1])
            self.PX = psx("PX", [128, D])
            self.PSg = [psx(f"PSg{i}", [128, 512]) for i in range(2)]
            self.PSu = [psx(f"PSu{i}", [128, 512]) for i in range(2)]
            self.PSh = psx("PSh", [128, 8, 128], BF16)
            self.PSr = psx("PSr", [128, 64])
            self.lcur = None
            kb.op("pool", lambda e: e.memset(self.Ls[:], 1.0), writes=[self.Ls.b])
            kb.op("pool", lambda e: e.affine_select(out=self.Ls[:], in_=self.Ls[:], pattern=[[1, 128]], compare_op=ALU.is_gt, fill=0.0,
                                                    base=0, channel_multiplier=-1), reads=[self.Ls.b], writes=[self.Ls.b])
            kb.op("pool", lambda e: e.memset(self.base[:], 0.0), writes=[self.base.b])
            kb.op("pool", lambda e: e.iota(self.bst[:], pattern=[[self.BR, NB]], base=0, channel_multiplier=0, allow_small_or_imprecise_dtypes=True),
                  writes=[self.bst.b])
            kb.op("pool", lambda e: e.iota(self.pcol[:], pattern=[[0, 1]], base=0, channel_multiplier=1, allow_small_or_imprecise_dtypes=True),
                  writes=[self.pcol.b])

        def prep(self, l, t):
            if self.lcur != l:
                DMA("sp", self.wr[:], moe_wr[l].rearrange("(m p) n -> p m n", p=128), writes=[self.wr])
                self.lcur = l
            xa = self.xa
            DMA("pool", self.XA[rowsl(t), :], xa[:], reads=[xa], writes=[self.dXA[t]])
            sm = self.sm
            c1 = lambda i, w=1: sm.v((A_, slice(i, i + w)))
            ACT(self.xn, xa, AF.Square, accum=c1(0))
            ACT(c1(1), c1(0), AF.Ln, bias=1024e-6)
            ACT(c1(1), c1(1), AF.Exp, scale=-0.5)
            TS("dve", self.xn, xa, c1(1), None, ALU.mult)
            for m in range(8):
                TR(self.PX.v((A_, cs(m))), self.xn.v((A_, cs(m))), ident)
            ai = 2 + 4 * l
            for m in range(8):
                ACT(self.hTf.v((A_, m, A_)), self.PX.v((A_, cs(m))), AF.Identity,
                    scale=AB.v((A_, ai, slice(m, m + 1))), bias=AB.v((A_, ai + 1, slice(m, m + 1))))
            for m in range(8):
                TR(self.PX.v((A_, cs(m))), self.hTf.v((A_, m, A_)), ident)
            CP("act", self.htk, self.PX)
            DMA("pool", self.HTOK[rowsl(t), :], self.htk[:], reads=[self.htk], writes=[self.dHT[t]])
            for m in range(8):
                MM(self.PSr.v((A_, slice(0, 36))), self.hTf.v((A_, m, A_)), self.wr.v((A_, m, A_)), start=(m == 0), stop=(m == 7))
            lg = c1(8, 36)
            CP("act", lg, self.PSr.v((A_, slice(0, 36))))
            g4 = c1(8, 4)
            RED("dve", c1(2), g4, op=ALU.max)
            TS("dve", c1(3), c1(2), -1.0, None, ALU.mult)
            ACT(c1(44, 4), g4, AF.Exp, bias=c1(3), accum=c1(4))
            kb.op("dve", lambda e: e.reciprocal(out=_ap(c1(5)), in_=_ap(c1(4))), reads=[sm.b], writes=[sm.b])
            TS("dve", c1(48, 4), g4, c1(2), None, ALU.is_equal)
            sel = c1(52, 8)
            TS("dve", sel, c1(12, 8), c1(48), None, ALU.mult)
            for g in range(1, 4):
                STT("dve", sel, c1(12 + 8 * g, 8), c1(48 + g), sel, ALU.mult, ALU.add)
            RED("dve", c1(6), sel, op=ALU.max)
            TS("dve", c1(60, 8), sel, c1(6), None, ALU.is_equal)
            STT("dve", c1(68, 8), c1(60, 8), -1e30, sel, ALU.mult, ALU.add)
            RED("dve", c1(7), c1(68, 8), op=ALU.max)
            TS("dve", c1(76, 8), c1(68, 8), c1(7), None, ALU.is_equal)
            TT("dve", c1(84), c1(7), c1(6), ALU.subtract)
            ACT(c1(85), c1(84), AF.Exp)
            TS("dve", c1(86), c1(85), 1.0, None, ALU.add)
            kb.op("dve", lambda e: e.reciprocal(out=_ap(c1(87)), in_=_ap(c1(86))), reads=[sm.b], writes=[sm.b])
            TT("dve", self.wk.v((A_, t, slice(0, 1))), c1(87), c1(5), ALU.mult)
            TT("dve", self.wk.v((A_, t, slice(1, 2))), self.wk.v((A_, t, slice(0, 1))), c1(85), ALU.mult)
            for k, o8 in ((0, 60), (1, 76)):
                for g in range(4):
                    TS("dve", self.OH.v((A_, t, k, slice(8 * g, 8 * g + 8))), c1(o8, 8), c1(48 + g), None, ALU.mult)
            TT("dve", self.ohs, self.OH.v((A_, t, 0, A_)), self.OH.v((A_, t, 1, A_)), ALU.add)
            MM(self.PSr.v((A_, slice(0, 32))), self.Ls, self.ohs)
            TT("dve", self.t32[0], self.PSr.v((A_, slice(0, 32))), self.base, ALU.add)
            for k in range(2):
                TT("dve", self.t32[1], self.OH.v((A_, t, k, A_)), self.t32[0], ALU.mult)
                RED("dve", self.rk.v((A_, t, slice(k, k + 1))), self.t32[1])
            MM(self.PSr.v((A_, slice(32, 64))), onesb, self.ohs)
            TT("dve", self.base, self.PSr.v((A_, slice(32, 64))), self.base, ALU.add)

        def run(self, l, gfb, emit):
            NTL, NB = self.NTL, self.NB
            t32 = self.t32
            with ExitStack() as esq:
                cq = T_(esq.enter_context(nc.sbuf_tensor("cq" + self.tag, [128, 32, NB], F32)), "cq")
                kb.op("dve", lambda e: e.tensor_tensor(out=cq[:], in0=self.bst[:].unsqueeze(1).broadcast_to([128, 32, NB]),
                                                       in1=self.base[:].unsqueeze(2).broadcast_to([128, 32, NB]), op=ALU.is_lt),
                      reads=[self.bst.b, self.base.b], writes=[cq.b])
                RED("dve", t32[0], cq)
            kb.barrier()
            TS("dve", t32[0], t32[0], float(self.BR), None, ALU.mult)
            CP("dve", t32[1], t32[0])
            a, b_ = t32[1], t32[2]
            for sh in (1, 2, 4, 8, 16):
                CP("dve", b_, a)
                TT("dve", b_.v((A_, slice(sh, 32))), a.v((A_, slice(sh, 32))), a.v((A_, slice(0, 32 - sh))), ALU.add)
                a, b_ = b_, a
            pend = a
            pstart = b_
            TT("dve", pstart, pend, t32[0], ALU.subtract)
            with ExitStack() as esr:
                def sbr(name, shape, dt=F32):
                    return T_(esr.enter_context(nc.sbuf_tensor(name + self.tag, list(shape), dt)), name)
                with ExitStack() as esc:
                    cmp_ = T_(esc.enter_context(nc.sbuf_tensor("cmp" + self.tag, [128, NB, 32], F32)), "cmp")
                    kb.op("dve", lambda e: e.tensor_tensor(out=cmp_[:], in0=pend[:].unsqueeze(1).broadcast_to([128, NB, 32]),
                                                           in1=self.bst[:].unsqueeze(2).broadcast_to([128, NB, 32]), op=ALU.is_le),
                          reads=[pend.b, self.bst.b], writes=[cmp_.b])
                    RED("dve", self.blke, cmp_)
                kb.barrier()
                TS("dve", self.blke, self.blke, 31.0, 128.0, ALU.min, ALU.mult)
                TS("dve", self.blke, self.blke, self.pcol, float(l * NEXP * 128), ALU.add, ALU.add)
                CP("dve", self.widx, self.blke)
                with ExitStack() as ess:
                    def sbs(name, shape, dt=F32):
                        return T_(ess.enter_context(nc.sbuf_tensor(name + self.tag, list(shape), dt)), name)
                    hrow = [sbs(f"hrow{i}", [128, D], BF16) for i in range(2)]
                    zt = sbs("zt", [128, D], BF16)
                    kb.op("pool", lambda e: e.memset(zt[:], 0.0), writes=[zt.b])
                    dz = [DR("hsz") for _ in range(4)]
                    ntz = NB * self.SUB
                    bnd = [ntz * qq // 4 for qq in range(5)]
                    for qq in range(4):
                        DMA("sp", self.HS[bnd[qq] * 128:bnd[qq + 1] * 128, :].rearrange("(c p) d -> p c d", p=128),
                            zt[:].unsqueeze(1).broadcast_to([128, bnd[qq + 1] - bnd[qq], D]), reads=[zt], writes=[dz[qq]])
                    zb_ = [d.b for d in dz]
                    for t in range(NTL):
                        for k in range(2):
                            TT("dve", t32[0], self.OH.v((A_, t, k, A_)), pstart, ALU.mult)
                            RED("dve", self.dstf.v((A_, t, slice(k, k + 1))), t32[0])
                        TT("dve", self.dstf.v((A_, t, A_)), self.dstf.v((A_, t, A_)), self.rk.v((A_, t, A_)), ALU.add)
                        CP("dve", self.dsti.v((A_, t, A_)), self.dstf.v((A_, t, A_)))
                        hr = hrow[t % 2]
                        DMA("sp", hr[:], self.HTOK[rowsl(t), :], reads=[self.dHT[t]], writes=[hr])
                        for k in range(2):
                            self._ind("scatter", self.HS, self.dsti.t[:, t, k:k + 1], hr, reads=[hr.b, self.dsti.b] + zb_, writes=[self.dHS[2 * t + k].b])
                kb.barrier()
                stg = [[sbr(f"stg{i}_{j}", [128, 4096]) for j in range(3)] for i in range(2)]
                Wb = [[sbr(f"Wb{i}_{j}", [128, 4096], BF16) for j in range(3)] for i in range(2)]
                hsb = [sbr(f"hsb{i}", [128, D], BF16) for i in range(2)]
                hT = [sbr(f"hTx{i}", [128, 8, 128], BF16) for i in range(2)]
                sg = sbr("sg", [128, 512])
                hb = [sbr(f"hb{i}", [128, 512], BF16) for i in range(2)]
                hT2 = [sbr(f"hT2{i}", [128, 4, 128], BF16) for i in range(2)]
                ysb = [sbr("ysb0", [128, D])] * 2
                allhs = [d.b for d in self.dHS]
                wsrc = [w_.rearrange("l r f -> (l r) f") for w_ in (moe_wg, moe_wu, moe_wd)]

                def wgather(blk):
                    for j in range(3):
                        st = stg[blk % 2][j]
                        self._ind("gather", wsrc[j], self.widx.t[:, blk:blk + 1], st, reads=[self.widx.b], writes=[st.b])

                def wcast(blk):
                    for j in range(3):
                        st = stg[blk % 2][j]
                        for q4 in range(4):
                            sl = slice(q4 * 1024, (q4 + 1) * 1024)
                            CP("act" if (j * 4 + q4) % 2 == 0 else "dve", Wb[blk % 2][j].v((A_, sl)), st.v((A_, sl)))

                SUB, BR = self.SUB, self.BR

                def front(u):
                    blk, sub = divmod(u, SUB)
                    j = u % 2
                    r0 = blk * BR + sub * 128
                    Wg_ = View(Wb[blk % 2][0].t[:].rearrange("p (m f) -> p m f", f=512), Wb[blk % 2][0].b)
                    Wu_ = View(Wb[blk % 2][1].t[:].rearrange("p (m f) -> p m f", f=512), Wb[blk % 2][1].b)
                    kb.dma("sp", hsb[j][:], self.HS[r0:r0 + 128, :], reads=allhs, writes=[hsb[j].b])
                    for m in range(8):
                        TR(self.PSh.v((A_, m, A_)), hsb[j].v((A_, cs(m))), identb)
                    CP("act", hT[j], self.PSh)
                    for mk in range(8):
                        MM(self.PSg[j], hT[j].v((A_, mk, A_)), View(Wg_.ap[:, mk, :], Wg_.b), start=(mk == 0), stop=(mk == 7))
                    for mk in range(8):
                        MM(self.PSu[j], hT[j].v((A_, mk, A_)), View(Wu_.ap[:, mk, :], Wu_.b), start=(mk == 0), stop=(mk == 7))
                    ACT(sg, self.PSg[j], AF.Silu)
                    TT("dve", hb[j], self.PSu[j], sg, ALU.mult)

                def back(u):
                    blk, sub = divmod(u, SUB)
                    j = u % 2
                    r0 = blk * BR + sub * 128
                    Wd_ = View(Wb[blk % 2][2].t[:].rearrange("p (m f) -> p m f", f=D), Wb[blk % 2][2].b)
                    for fk in range(4):
                        TR(self.PSh.v((A_, fk, A_)), hb[j].v((A_, cs(fk))), identb)
                    CP("act", hT2[j], self.PSh.v((A_, slice(0, 4), A_)))
                    for h in range(2):
                        for fk in range(4):
                            MM(self.PX.v((A_, hs(h))), hT2[j].v((A_, fk, A_)), View(Wd_.ap[:, fk, hs(h)], Wd_.b), start=(fk == 0), stop=(fk == 3))
                    CP("dve", ysb[j], self.PX)
                    DMA("sp", self.YS[r0:r0 + 128, :], ysb[j][:], reads=[ysb[j]], writes=[self.dYS[u]])
                wgather(0)
                if NB > 1:
                    wgather(1)
                wcast(0)
                front(0)
                for u in range(SUB * NB):
                    blk, sub = divmod(u, SUB)
                    if sub == 0 and blk + 1 < NB:
                        wcast(blk + 1)
                    if sub == 1 and blk + 2 < NB:
                        wgather(blk + 2)
                    if u + 1 < SUB * NB:
                        front(u + 1)
                    back(u)
                allys = [d.b for d in self.dYS]
                for t in range(NTL):
                    y1, y2 = stg[0][0], stg[0][1]
                    y1v, y2v = y1.v((A_, slice(0, D))), y2.v((A_, slice(0, D)))
                    self._ind("gather", self.YS, self.dsti.t[:, t, 0:1], y1v, reads=allys + [self.dsti.b], writes=[y1.b])
                    self._ind("gather", self.YS, self.dsti.t[:, t, 1:2], y2v, reads=allys + [self.dsti.b], writes=[y2.b])
                    DMA("sp", self.xa[:], self.XA[rowsl(t), :], reads=[self.dXA[t]], writes=[self.xa])
                    TS("dve", y1v, y1v, self.wk.v((A_, t, slice(0, 1))), None, ALU.mult)
                    STT("dve", y1v, y2v, self.wk.v((A_, t, slice(1, 2))), y1v, ALU.mult, ALU.add)
                    TT("dve", y1v, y1v, gfb, ALU.mult)
                    TT("dve", self.xa, self.xa, y1v, ALU.add)
                    emit(t, self.xa)

        def _ind(self, kind, dram, idx_ap, sb_, reads, writes):
            q = "pool"
            keys, i = kb.dma_pool[q]
            key = keys[i % len(keys)]
            kb.dma_pool[q][1] = i + 1
            if kb.cnt[key] > 0:
                kb._wait(q, (key, kb.cnt[key]))
            for b in reads:
                kb._wait(q, b.w)
            for b in writes:
                kb._wait(q, b.w)
                for k_, v_ in b.r.items():
                    kb._wait(q, (k_, v_))
            off = bass.IndirectOffsetOnAxis(ap=idx_ap, axis=0)
            if kind == "gather":
                inst = nc.gpsimd.indirect_dma_start(out=_ap(sb_), out_offset=None, in_=dram, in_offset=off)
            else:
                inst = nc.gpsimd.indirect_dma_start(out=dram, out_offset=off, in_=_ap(sb_), in_offset=None)
            kb.cnt[key] += 16
            inst.then_inc(kb.sems[key], 16)
            kb._mark((key, kb.cnt[key]), reads, writes)
            kb.n_inst += 1


    class FFN:
        def __init__(self, es_, tag=""):
            def sbx(name, shape, dt=F32):
                return T_(es_.enter_context(nc.sbuf_tensor(name + tag, list(shape), dt)), name)

            def psx(name, shape, dt=F32):
                return T_(es_.enter_context(nc.psum_tensor(name + tag, list(shape), dt)), name)
            self.acc = sbx("acc", [128, GT, D])
            self.hTb = sbx("hTb", [128, GT, 8, 128], BF16)
            self.gw = sbx("gw", [128, GT, 32])
            self.hTf = sbx("hTf", [128, 8, 128])
            self.xn = sbx("xn_f", [128, D])
            self.wr = sbx("wr", [128, 8, 36])
            self.sm = sbx("sm", [128, 96])
            self.stg = [sbx(f"stg{i}", [128, 4096]) for i in range(2)]
            self.Wg = [sbx(f"Wgb{i}", [128, 8, 512], BF16) for i in range(2)]
            self.Wu = [sbx(f"Wub{i}", [128, 8, 512], BF16) for i in range(2)]
            self.Wd = [sbx(f"Wdb{i}", [128, 4, D], BF16) for i in range(2)]
            self.sg = [sbx("sg0", [128, 512])] * 2
            self.hb = [sbx(f"hb{i}", [128, 512], BF16) for i in range(2)]
            self.hT2 = [sbx(f"hT2{i}", [128, 4, 128], BF16) for i in range(2)]
            self.PX = psx("PX", [128, D])
            self.PSg = [psx(f"PSg{i}", [128, 512]) for i in range(2)]
            self.PSu = [psx(f"PSu{i}", [128, 512]) for i in range(2)]
            self.PSh = psx("PSh", [128, 8, 128], BF16)
            self.PSr = psx("PSr", [128, 64])
            self.lcur = None

        def prep(self, l, t):
            if self.lcur != l:
                DMA("sp", self.wr[:], moe_wr[l].rearrange("(m p) n -> p m n", p=128), writes=[self.wr])
                self.lcur = l
            xa = self.acc.v((A_, t, A_))
            sm = self.sm
            c1 = lambda i, w=1: sm.v((A_, slice(i, i + w)))
            ACT(self.xn, xa, AF.Square, accum=c1(0))
            ACT(c1(1), c1(0), AF.Ln, bias=1024e-6)
            ACT(c1(1), c1(1), AF.Exp, scale=-0.5)
            TS("dve", self.xn, xa, c1(1), None, ALU.mult)
            for m in range(8):
                TR(self.PX.v((A_, cs(m))), self.xn.v((A_, cs(m))), ident)
            ai = 2 + 4 * l
            for m in range(8):
                ACT(self.hTf.v((A_, m, A_)), self.PX.v((A_, cs(m))), AF.Identity,
                    scale=AB.v((A_, ai, slice(m, m + 1))), bias=AB.v((A_, ai + 1, slice(m, m + 1))))
            CP("pool", self.hTb.v((A_, t, A_, A_)), self.hTf)
            for m in range(8):
                MM(self.PSr.v((A_, slice(0, 36))), self.hTf.v((A_, m, A_)), self.wr.v((A_, m, A_)), start=(m == 0), stop=(m == 7))
            lg = c1(8, 36)
            CP("act", lg, self.PSr.v((A_, slice(0, 36))))
            g4 = c1(8, 4)
            RED("dve", c1(2), g4, op=ALU.max)
            TS("dve", c1(3), c1(2), -1.0, None, ALU.mult)
            ACT(c1(44, 4), g4, AF.Exp, bias=c1(3), accum=c1(4))
            kb.op("dve", lambda e: e.reciprocal(out=_ap(c1(5)), in_=_ap(c1(4))), reads=[sm.b], writes=[sm.b])
            TS("dve", c1(48, 4), g4, c1(2), None, ALU.is_equal)
            sel = c1(52, 8)
            TS("dve", sel, c1(12, 8), c1(48), None, ALU.mult)
            for g in range(1, 4):
                STT("dve", sel, c1(12 + 8 * g, 8), c1(48 + g), sel, ALU.mult, ALU.add)
            RED("dve", c1(6), sel, op=ALU.max)
            TS("dve", c1(60, 8), sel, c1(6), None, ALU.is_equal)
            STT("dve", c1(68, 8), c1(60, 8), -1e30, sel, ALU.mult, ALU.add)
            RED("dve", c1(7), c1(68, 8), op=ALU.max)
            TS("dve", c1(76, 8), c1(68, 8), c1(7), None, ALU.is_equal)
            TT("dve", c1(84), c1(7), c1(6), ALU.subtract)
            ACT(c1(85), c1(84), AF.Exp)
            TS("dve", c1(86), c1(85), 1.0, None, ALU.add)
            kb.op("dve", lambda e: e.reciprocal(out=_ap(c1(87)), in_=_ap(c1(86))), reads=[sm.b], writes=[sm.b])
            TT("dve", c1(87), c1(87), c1(5), ALU.mult)
            TT("dve", c1(88), c1(87), c1(85), ALU.mult)
            TS("dve", c1(60, 8), c1(60, 8), c1(87), None, ALU.mult)
            STT("dve", c1(60, 8), c1(76, 8), c1(88), c1(60, 8), ALU.mult, ALU.add)
            for g in range(4):
                TS("dve", self.gw.v((A_, t, slice(8 * g, 8 * g + 8))), c1(60, 8), c1(48 + g), None, ALU.mult)

        def experts(self, l, ntile, gfb):
            engs = ["act", "dve", "pool"]
            n = 0
            for e in range(NEXP):
                i = e % 2
                for (dst, src) in ((self.Wg[i], moe_wg), (self.Wu[i], moe_wu)):
                    st = self.stg[n % 2]
                    DMA("sp" if n % 2 == 0 else "pool", st[:].rearrange("p (m f) -> p m f", f=512), src[l, e].rearrange("(m p) f -> p m f", p=128), writes=[st])
                    CP(engs[n % 3], dst, View(st.t[:].rearrange("p (m f) -> p m f", f=512), st.b))
                    n += 1
                st = self.stg[n % 2]
                DMA("sp" if n % 2 == 0 else "pool", st[:].rearrange("p (m f) -> p m f", f=D), moe_wd[l, e].rearrange("(m p) f -> p m f", p=128), writes=[st])
                for fk in range(4):
                    TT("pool" if fk % 2 else "dve", self.Wd[i].v((A_, fk, A_)), st.v((A_, slice(fk * D, (fk + 1) * D))), gfb, ALU.mult)
                n += 1
                for t in range(ntile):
                    j = t % 2
                    for mk in range(8):
                        MM(self.PSg[j], self.hTb.v((A_, t, mk, A_)), self.Wg[i].v((A_, mk, A_)), start=(mk == 0), stop=(mk == 7))
                    for mk in range(8):
                        MM(self.PSu[j], self.hTb.v((A_, t, mk, A_)), self.Wu[i].v((A_, mk, A_)), start=(mk == 0), stop=(mk == 7))
                    ACT(self.sg[j], self.PSg[j], AF.Silu)
                    STT("dve", self.hb[j], self.PSu[j], self.gw.v((A_, t, slice(e, e + 1))), self.sg[j], ALU.mult, ALU.mult)
                    for fk in range(4):
                        TR(self.PSh.v((A_, fk, A_)), self.hb[j].v((A_, cs(fk))), identb)
                    CP("act", self.hT2[j], self.PSh.v((A_, slice(0, 4), A_)))
                    for h in range(2):
                        for fk in range(4):
                            MM(self.PX.v((A_, hs(h))), self.hT2[j].v((A_, fk, A_)), self.Wd[i].v((A_, fk, hs(h))), start=(fk == 0), stop=(fk == 3))
                    TT("dve", self.acc.v((A_, t, A_)), self.acc.v((A_, t, A_)), self.PX, ALU.add)

    kb.barrier()
    if has("p3"):
        with ExitStack() as es3:
            ffn = FFN2(es3, "_l0", NT)
            with ExitStack() as es3a:
                def sb3(name, shape, dt=F32):
                    return T_(es3a.enter_context(nc.sbuf_tensor(name, list(shape), dt)), name)
                Wo = sb3("Wo", [128, 8, D], BF16)
                gng, gnb = sb3("gng", [128, D]), sb3("gnb", [128, D])
                G3 = [sb3(f"G3{i}", [128, D]) for i in range(3)] + [T_(_APWrap(ffn.hTf.t[:].rearrange("p m n -> p (m n)")), "hTf_flat"), ffn.xn]
                G3[3].b = ffn.hTf.b
                zb = sb3("zb", [128, D], BF16)
                zT = sb3("zT", [128, 8, 128], BF16)
                st16 = sb3("st16", [128, 48])
                PZ = ffn.PSh
                vh = lambda t: View(t.t[:].rearrange("p (h n) -> p h n", n=64), t.b)
                bch = lambda v_: View(v_.ap.unsqueeze(2).broadcast_to([128, 16, 64]), v_.b)
                s16 = lambda i: st16.v((A_, slice(16 * i, 16 * i + 16)))
                for m in range(8):
                    st = G3[m % 2]
                    DMA("sp", st[:], rw_wo[cs(m), :], writes=[st])
                    CP("act" if m % 2 else "dve", Wo.v((A_, m, A_)), st)
                bc_row(gng, vec_row[VR_GNG:VR_GNG + 1, :])
                bc_row(gnb, vec_row[VR_GNB:VR_GNB + 1, :])
                for tt in range(NT):
                    yv, gv, bv, xv, wk = G3
                    ydeps = [dY[2 * tt], dY[2 * tt + 1]] if has("p2") else []
                    DMA("sp", yv[:], YSC[rowsl(tt), :], reads=ydeps, writes=[yv])
                    DMA("sp", gv[:], GSC[rowsl(tt), :], reads=[dG[tt]], writes=[gv])
                    DMA("sp", bv[:], BON[rowsl(tt), :], reads=[dBON[tt]], writes=[bv])
                    DMA("sp", xv[:], x_in[rowsl(tt), :], writes=[xv])
                    RED("dve", s16(0), vh(yv))
                    TS("dve", s16(0), s16(0), -1.0 / 64, None, ALU.mult)
                    TT("dve", vh(yv), vh(yv), bch(s16(0)), ALU.add)
                    TT("pool", wk, yv, yv, ALU.mult)
                    RED("dve", s16(1), vh(wk))
                    ACT(s16(1), s16(1), AF.Ln, scale=1.0 / 64, bias=64e-5)
                    ACT(s16(1), s16(1), AF.Exp, scale=-0.5)
                    TT("dve", vh(yv), vh(yv), bch(s16(1)), ALU.mult)
                    TT("pool", yv, yv, gng, ALU.mult)
                    TT("pool", yv, yv, gnb, ALU.add)
                    TT("dve", yv, yv, bv, ALU.add)
                    TT("dve", zb, yv, gv, ALU.mult)
                    for m in range(8):
                        TR(PZ.v((A_, m, A_)), zb.v((A_, cs(m))), identb)
                    CP("act", zT, PZ)
                    for h in range(2):
                        for mk in range(8):
                            MM(ffn.PX.v((A_, hs(h))), zT.v((A_, mk, A_)), Wo.v((A_, mk, hs(h))), start=(mk == 0), stop=(mk == 7))
                    TT("dve", wk, ffn.PX, gmb[0], ALU.mult)
                    TT("dve", ffn.xa, wk, xv, ALU.add)
                    ffn.prep(0, tt)
            kb.barrier()

            def emit0(t, xa_):
                DMA("sp", X1[rowsl(t), :], xa_[:], reads=[xa_], writes=[dX1[t]])
            ffn.run(0, gmb[1], emit0)
    if dbg == 3:
        allb = [d.b for d in dX1]
        kb.finish("sp", allb)
        kb.finish("pool", allb)
        kb.close()
        es.close()
        return nc, in_names, dbg_outs

    NO = NT // 2
    KTS = dscr("kts", [16, 64, T], BF16)
    VS = dscr("vs", [T, D], BF16)
    QTS = dscr("qts", [16, 64, NO * 128], BF16)
    XO = dscr("xo", [NO * 128, D], F32)
    OS = dscr("os", [NO * 128, D], BF16)
    dKT = [DR(f"kt{t}") for t in range(NT)]
    dVS = [DR(f"vs{t}") for t in range(NT)]
    dQT = [DR(f"qt{t}") for t in range(NO)]
    dXO = [DR(f"xo{t}") for t in range(NO)]
    dOS = [DR(f"os{t}") for t in range(NO)]
    dOUT = [DR(f"out{t}") for t in range(NO)]
    LAM_INIT = 0.8 - 0.6 * math.exp(-0.3 * 1)
    kb.barrier()
    if has("p5"):
        with ExitStack() as es4:
            def sb4(name, shape, dt=F32):
                return T_(es4.enter_context(nc.sbuf_tensor(name, list(shape), dt)), name)

            def ps4(name, shape, dt=F32):
                return T_(es4.enter_context(nc.psum_tensor(name, list(shape), dt)), name)
            Wkv = sb4("Wkv", [128, 8, 2 * D], BF16)
            Wq = sb4("Wq", [128, 8, D], BF16)
            stg = [sb4(f"stg4{i}", [128, 2 * D]) for i in range(2)]
            xt4 = [sb4(f"xt4{i}", [128, D]) for i in range(3)]
            hk = sb4("hk", [128, 8, 128], BF16)
            kts = sb4("kts_sb", [64, 16, 128], BF16)
            vsb = sb4("vsb", [128, D], BF16)
            sm4 = sb4("sm4", [128, 32])
            selc = sb4("selc", [128, 4])
            cmk = sb4("cmk", [128, 2, 128], BF16)
            cmf = sb4("cmf", [128, 2, 128])
            lamt = sb4("lamt", [128, 8])
            subg = sb4("subg", [128, 128])
            KTh = sb4("KTh", [128, 2, T], BF16)
            Vh = sb4("Vh", [128, NT, 129], BF16)
            QTh = sb4("QTh", [128, 2, NO * 128], BF16)
            mxs = [sb4(f"mx{i}", [128, 16]) for i in range(4)]
            negm65 = sb4("negm65", [128, 4, 65])
            rowbuf = [Buf(f"row{i}") for i in range(4)]
            osq = sb4("osq", [128, 128], BF16)
            pk_slot = [Buf("pk0"), Buf("pk1")]
            psn_slot = [Buf("psn0"), Buf("psn1")]
            pst_slot = [Buf(f"pst{i}") for i in range(4)]
            Pt = [sb4(f"Pt{i}", [128, 2, 128], BF16) for i in range(3)]
            ot = sb4("ot", [128, 128])
            ob = sb4("ob", [128, 128], BF16)
            PXa = ps4("PXa", [128, D])
            PKs = ps4("PKs", [128, D])
            PSt = ps4("PSt4", [128, 4, 128])
            PSo = [ps4(f"PSo{i}", [128, 512]) for i in range(2)]
            PSn = ps4("PSn", [128, 512])
            c4 = lambda i, w=1: sm4.v((A_, slice(i, i + w)))
            PK = View(PKs.t[0:64, :].rearrange("p (s t) -> p s t", t=128), PKs.b)

            n = 0
            for m in range(8):
                st = stg[n % 2]
                DMA("sp" if n % 2 == 0 else "pool", st[:], w_kv[cs(m), :], writes=[st])
                CP(["act", "dve", "pool"][n % 3], Wkv.v((A_, m, A_)), st)
                n += 1
            for m in range(8):
                st = stg[n % 2]
                DMA("sp" if n % 2 == 0 else "pool", st[:, 0:D], df_wq[cs(m), :], writes=[st])
                CP(["act", "dve", "pool"][n % 3], Wq.v((A_, m, A_)), st.v((A_, slice(0, D))))
                n += 1
            DMA("sp", selc[:], sel_in[:, :], writes=[selc])
            DMA("sp", cmf[:], cmask_in.rearrange("c k q -> k c q"), writes=[cmf])
            CP("dve", cmk, cmf)
            DMA("sp", lamt[:, 0:4], lam_in[:, :], writes=[lamt])
            bc_row(subg, vec_row[VR_SUBLN:VR_SUBLN + 1, 0:128])
            TS("dve", subg, subg, 1.0 - LAM_INIT, None, ALU.mult)
            TT("dve", lamt.v((A_, slice(4, 5))), lamt.v((A_, slice(0, 1))), lamt.v((A_, slice(1, 2))), ALU.mult)
            TT("dve", lamt.v((A_, slice(5, 6))), lamt.v((A_, slice(2, 3))), lamt.v((A_, slice(3, 4))), ALU.mult)
            MM(PSn.v((A_, slice(0, 2))), ones, lamt.v((A_, slice(4, 6))))
            ACT(lamt.v((A_, slice(6, 8))), PSn.v((A_, slice(0, 2))), AF.Exp)
            TT("dve", lamt.v((A_, slice(4, 5))), lamt.v((A_, slice(7, 8))), lamt.v((A_, slice(6, 7))), ALU.subtract)
            TS("dve", lamt.v((A_, slice(4, 5))), lamt.v((A_, slice(4, 5))), -LAM_INIT, None, ALU.add)
            neglam = lamt.v((A_, slice(4, 5)))

            def norm_T(xv, ai, dst):
                ACT(xt4[2], xv, AF.Square, accum=c4(0))
                ACT(c4(1), c4(0), AF.Ln, bias=1024e-6)
                ACT(c4(1), c4(1), AF.Exp, scale=-0.5)
                TS("dve", xt4[2], xv, c4(1), None, ALU.mult)
                for m in range(8):
                    TR(PXa.v((A_, cs(m))), xt4[2].v((A_, cs(m))), ident)
                for m in range(8):
                    ACT(dst.v((A_, m, A_)), PXa.v((A_, cs(m))), AF.Identity,
                        scale=AB.v((A_, ai, slice(m, m + 1))), bias=AB.v((A_, ai + 1, slice(m, m + 1))))

            def proj64(W, coff, scale, dram, col0):
                for half in range(2):
                    for s8 in range(8):
                        sl = half * 8 + s8
                        for mk in range(8):
                            MM(View(PK.ap[:, s8, :], PK.b), W.v((A_, mk, slice(coff + sl * 64, coff + sl * 64 + 64))), hk.v((A_, mk, A_)),
                               start=(mk == 0), stop=(mk == 7))
                    ACT(kts.v((A_, slice(half * 8, half * 8 + 8), A_)), PK, AF.Copy, scale=scale)

            for tt in range(NT):
                DMA("sp", xt4[0][:], X1[rowsl(tt), :], reads=[dX1[tt]], writes=[xt4[0]])
                norm_T(xt4[0], 8, hk)
                proj64(Wkv, 0, 1.0, KTS, tt * 128)
                DMA("pool", KTS[:, :, tt * 128:(tt + 1) * 128].rearrange("s d t -> d s t"), kts[:], reads=[kts], writes=[dKT[tt]])
                for h in range(2):
                    for mk in range(8):
                        MM(PXa.v((A_, hs(h))), hk.v((A_, mk, A_)), Wkv.v((A_, mk, slice(D + h * 512, D + (h + 1) * 512))), start=(mk == 0), stop=(mk == 7))
                CP("dve", vsb, PXa)
                DMA("pool", VS[rowsl(tt), :], vsb[:], reads=[vsb], writes=[dVS[tt]])
            for i in range(NO):
                DMA("sp", xt4[0][:], X1[rowsl(2 * i), :], reads=[dX1[2 * i]], writes=[xt4[0]])
                DMA("sp", xt4[1][:], X1[rowsl(2 * i + 1), :], reads=[dX1[2 * i + 1]], writes=[xt4[1]])
                TS("dve", xt4[0], xt4[0], selc.v((A_, slice(0, 1))), None, ALU.mult)
                STT("dve", xt4[0], xt4[1], selc.v((A_, slice(1, 2))), xt4[0], ALU.mult, ALU.add)
                DMA("pool", XO[rowsl(i), :], xt4[0][:], reads=[xt4[0]], writes=[dXO[i]])
                norm_T(xt4[0], 4, hk)
                proj64(Wq, 0, 0.125, QTS, i * 128)
                DMA("pool", QTS[:, :, i * 128:(i + 1) * 128].rearrange("s d t -> d s t"), kts[:], reads=[kts], writes=[dQT[i]])
            kb.barrier()
            kb.op("pool", lambda e: e.memset(Vh[:, :, 128:129], 1.0), writes=[Vh.b])
            kb.op("pool", lambda e: e.memset(QTh[:], 0.0), writes=[QTh.b])
            kb.op("pool", lambda e: e.memset(KTh[:], 0.0), writes=[KTh.b])
            kb.op("pool", lambda e: e.memset(KTh[64:65, :, :], 1.0), writes=[KTh.b])
            kb.op("pool", lambda e: e.memset(negm65[:], 0.0), writes=[negm65.b])
            for hd in range(8):
                for c in range(2):
                    DMA("sp", KTh[0:64, c, :], KTS[2 * hd + c], reads=dKT, writes=[KTh])
                    DMA("sp", QTh[0:64, c, :], QTS[2 * hd + c], reads=dQT, writes=[QTh])
                DMA("sp", Vh[:, :, 0:128], VS[:, hd * 128:(hd + 1) * 128].rearrange("(n p) e -> p n e", p=128), reads=dVS, writes=[Vh])
                pst_views = [View(PSt.t[:, 0:2, :], pst_slot[0]), View(PXa.t[:, 0:256].rearrange("p (c q) -> p c q", c=2), pst_slot[1]),
                             View(PXa.t[:, 512:768].rearrange("p (c q) -> p c q", c=2), pst_slot[2])]

                def pass1(i):
                    par = i % 2
                    nk = 2 * i + 2
                    qs = slice(i * 128, (i + 1) * 128)
                    nch = (nk * 128 + 511) // 512
                    for c in range(2):
                        mxt = mxs[2 * par + c]
                        for kc in range(nch):
                            w = min(512, nk * 128 - kc * 512)
                            pss = View(PKs.t[:, (kc % 2) * 512:(kc % 2) * 512 + w], pk_slot[kc % 2])
                            MM(pss, QTh.v((slice(0, 64), c, qs)), KTh.v((slice(0, 64), c, slice(kc * 512, kc * 512 + w))))
                            RED("dve", mxt.v((A_, slice(kc, kc + 1))), pss, op=ALU.max)
                        a = 8 + 4 * par + 2 * c
                        RED("dve", c4(a), mxt.v((A_, slice(0, nch))), op=ALU.max)
                        TS("dve", negm65.v((A_, 2 * par + c, slice(64, 65))), c4(a), -1.0, None, ALU.mult)
                        psn = View(PSn.t[0:65, 0:128], PSn.b)
                        TR(psn, negm65.v((A_, 2 * par + c, A_)), ident)
                        rb = rowbuf[2 * par + c]
                        kb.op("act", lambda en, c=c, qs=qs: en.copy(out=QTh[64:65, c, qs], in_=PSn[64:65, 0:128]), reads=[PSn.b], writes=[rb])

                def pass2(i):
                    par = i % 2
                    nk = 2 * i + 2
                    qs = slice(i * 128, (i + 1) * 128)

                    def score(kt):
                        pst = pst_views[kt % 3]
                        for c in range(2):
                            kb.op("pe", lambda en, c=c, kt=kt, pst=pst: en.matmul(pst.ap[:, c, :], lhsT=KTh[:, c, kt * 128:(kt + 1) * 128],
                                                                               rhs=QTh[:, c, qs], start=True, stop=True),
                                  reads=[KTh.b, QTh.b, rowbuf[2 * par + c]], writes=[pst.b])
                    for kt in range(min(2, nk)):
                        score(kt)
                    for kt in range(nk):
                        if kt + 2 < nk:
                            score(kt + 2)
                        p_ = Pt[kt % 3]
                        ACT(p_, pst_views[kt % 3], AF.Exp)
                        if kt >= nk - 2:
                            kb.op("dve", lambda e, p_=p_, kt=kt: e.tensor_tensor(
                                out=p_[:], in0=p_[:], in1=cmk[:, kt - (nk - 2):kt - (nk - 2) + 1, :].broadcast_to([128, 2, 128]), op=ALU.mult),
                                reads=[p_.b, cmk.b], writes=[p_.b])
                        for c in range(2):
                            MM(PSo[c].v((A_, slice(0, 129))), p_.v((A_, c, A_)), Vh.v((A_, kt, A_)), start=(kt == 0), stop=(kt == nk - 1))
                    kb.op("dve", lambda e: e.reciprocal(out=_ap(c4(4)), in_=PSo[0][:, 128:129]), reads=[PSo[0].b], writes=[sm4.b])
                    kb.op("dve", lambda e: e.reciprocal(out=_ap(c4(5)), in_=PSo[1][:, 128:129]), reads=[PSo[1].b], writes=[sm4.b])
                    TT("dve", c4(5), c4(5), neglam, ALU.mult)
                    TS("dve", ot, PSo[0].v((A_, slice(0, 128))), c4(4), None, ALU.mult)
                    STT("dve", ot, PSo[1].v((A_, slice(0, 128))), c4(5), ot, ALU.mult, ALU.add)
                    ACT(osq, ot, AF.Square, accum=c4(6))
                    ACT(c4(7), c4(6), AF.Ln, scale=1.0 / 128, bias=1e-5)
                    ACT(c4(7), c4(7), AF.Exp, scale=-0.5)
                    STT("dve", ob, ot, c4(7), subg, ALU.mult, ALU.mult)
                    DMA("pool", OS[rowsl(i), hd * 128:(hd + 1) * 128], ob[:], reads=[ob], writes=[dOS[i]])
                pass1(0)
                for i in range(NO):
                    if i + 1 < NO:
                        pass1(i + 1)
                    pass2(i)
        kb.barrier()
        with ExitStack() as es5:
            def sb5(name, shape, dt=F32):
                return T_(es5.enter_context(nc.sbuf_tensor(name, list(shape), dt)), name)
            ffn = FFN2(es5, "_l1", NO)
            fg = sb5("fg", [128, D])
            s5 = sb5("s5", [128, 4])
            hflat = T_(_APWrap(ffn.hTf.t[:].rearrange("p m n -> p (m n)")), "hTf_flat5")
            hflat.b = ffn.hTf.b
            bc_row(fg, vec_row[VR_FING:VR_FING + 1, :])
            TS("dve", fg, fg, 32.0, None, ALU.mult)
            with ExitStack() as es5a:
                def sb5a(name, shape, dt=F32):
                    return T_(es5a.enter_context(nc.sbuf_tensor(name, list(shape), dt)), name)
                Wo1 = sb5a("Wo1", [128, 8, D], BF16)
                x5 = [sb5a(f"x5{i}", [128, D]) for i in range(2)]
                osb = sb5a("osb", [128, D], BF16)
                oT = sb5a("oT", [128, 8, 128], BF16)
                for m in range(8):
                    st = x5[m % 2]
                    DMA("sp", st[:], df_wo[cs(m), :], writes=[st])
                    CP("act" if m % 2 else "dve", Wo1.v((A_, m, A_)), st)
                for i in range(NO):
                    DMA("sp", osb[:], OS[rowsl(i), :], reads=[dOS[i]], writes=[osb])
                    DMA("sp", x5[0][:], XO[rowsl(i), :], reads=[dXO[i]], writes=[x5[0]])
                    for m in range(8):
                        TR(ffn.PSh.v((A_, m, A_)), osb.v((A_, cs(m))), identb)
                    CP("act", oT, ffn.PSh)
                    for h in range(2):
                        for mk in range(8):
                            MM(ffn.PX.v((A_, hs(h))), oT.v((A_, mk, A_)), Wo1.v((A_, mk, hs(h))), start=(mk == 0), stop=(mk == 7))
                    TT("dve", x5[1], ffn.PX, gmb[2], ALU.mult)
                    TT("dve", ffn.xa, x5[1], x5[0], ALU.add)
                    ffn.prep(1, i)
            kb.barrier()

            def emit1(i, xa_):
                ACT(ffn.xn, xa_, AF.Square, accum=s5.v((A_, slice(0, 1))))
                ACT(s5.v((A_, slice(1, 2))), s5.v((A_, slice(0, 1))), AF.Ln, bias=1024e-6)
                ACT(s5.v((A_, slice(1, 2))), s5.v((A_, slice(1, 2))), AF.Exp, scale=-0.5)
                STT("dve", hflat, xa_, s5.v((A_, slice(1, 2))), fg, ALU.mult, ALU.mult)
                DMA("sp", out[rowsl(i), :], hflat[:], reads=[hflat], writes=[dOUT[i]])
            ffn.run(1, gmb[3], emit1)
    allb = [d.b for d in dOUT]
    kb.finish("sp", allb)
    kb.finish("pool", allb)
    kb.close()
    es.close()
    return nc, in_names, dbg_outs


def _col(v):
    return np.ascontiguousarray(np.asarray(v, np.float32).reshape(8, 128).T)


def make_in_maps(inputs, T=None, n_cores=8):
    p = {k: np.asarray(v) for k, v in inputs.items()}
    Tfull = p["x"].shape[1]
    T = T or Tfull
    shared = {
        "ada_w": np.ascontiguousarray(p["ada_w"]),
        "ada_kv_w": np.ascontiguousarray(p["ada_kv_w"]),
        "rw_w_rkv": np.ascontiguousarray(p["rw_w_rkv"][0]),
        "rw_w1a": np.ascontiguousarray(np.concatenate([p["rw_w1"][0], p["rw_a1"][0]], axis=1)),
        "rw_g1": np.ascontiguousarray(p["rw_g1"][0]),
        "rw_w2a2": np.ascontiguousarray(np.concatenate([p["rw_w2"][0], p["rw_a2"][0]], axis=0)),
        "rw_g2": np.ascontiguousarray(p["rw_g2"][0]),
        "rw_w_o": np.ascontiguousarray(p["rw_w_o"][0]),
        "w_kv": np.ascontiguousarray(p["w_kv"]),
        "df_w_q": np.ascontiguousarray(p["df_w_q"][0]),
        "df_w_o": np.ascontiguousarray(p["df_w_o"][0]),
        "moe_wr": np.ascontiguousarray(np.concatenate([p["moe_w_rg"], p["moe_w_re"]], axis=2)),
        "moe_w_gate": np.ascontiguousarray(p["moe_w_gate"].reshape(2, NEXP, 8, 128, EFF).transpose(0, 1, 3, 2, 4).reshape(2, NEXP * 128, 4096)),
        "moe_w_up": np.ascontiguousarray(p["moe_w_up"].reshape(2, NEXP, 8, 128, EFF).transpose(0, 1, 3, 2, 4).reshape(2, NEXP * 128, 4096)),
        "moe_w_down": np.ascontiguousarray(p["moe_w_down"].reshape(2, NEXP, 4, 128, D).transpose(0, 1, 3, 2, 4).reshape(2, NEXP * 128, 4096)),
    }
    vr = np.zeros((N_VR, D), np.float32)
    vr[VR_KK] = p["rw_k_k"][0]
    vr[VR_KA] = p["rw_k_a"][0]
    vr[VR_RK] = p["rw_r_k"][0].reshape(-1)
    vr[VR_GNG] = p["rw_gn_g"][0]
    vr[VR_GNB] = p["rw_gn_b"][0]
    vr[VR_W0] = p["rw_w0"][0]
    vr[VR_A0] = p["rw_a0"][0]
    vr[VR_FING] = p["final_g"]
    vr[VR_GM0] = p["ada_b"][0, 2 * D:3 * D]
    vr[VR_GF0] = p["ada_b"][0, 5 * D:6 * D]
    vr[VR_GM1] = p["ada_b"][1, 2 * D:3 * D]
    vr[VR_GF1] = p["ada_b"][1, 5 * D:6 * D]
    vr[VR_SUBLN] = np.tile(p["df_subln_g"][0], 8)
    shared["vec_row"] = vr
    lam = np.zeros((128, 4), np.float32)
    lam[:64, 0] = p["df_lq1"][0]
    lam[:64, 1] = p["df_lk1"][0]
    lam[:64, 2] = p["df_lq2"][0]
    lam[:64, 3] = p["df_lk2"][0]
    shared["lam"] = lam
    tri = (np.arange(128)[:, None] <= np.arange(128)[None, :]).astype(np.float32)
    maps = []
    for core in range(n_cores):
        b, s = core // 2, core % 2
        vc = np.zeros((128, N_VC, 8), np.float32)
        vc[:, VC_C] = _col(p["c"][b])
        vc[:, VC_NMIX0] = _col(p["norm_mix_g"][0])
        vc[:, VC_NFFN0] = _col(p["norm_ffn_g"][0])
        vc[:, VC_NMIX1] = _col(p["norm_mix_g"][1])
        vc[:, VC_NFFN1] = _col(p["norm_ffn_g"][1])
        vc[:, VC_NKV] = _col(p["norm_kv_g"])
        for i in range(6):
            vc[:, VC_MU + i] = _col(p["rw_mu"][0, i])
            vc[:, VC_ADAB0 + i] = _col(p["ada_b"][0, i * D:(i + 1) * D])
            vc[:, VC_ADAB1 + i] = _col(p["ada_b"][1, i * D:(i + 1) * D])
        vc[:, VC_KVB] = _col(p["ada_kv_b"][:D])
        vc[:, VC_KVB + 1] = _col(p["ada_kv_b"][D:])
        sel = np.zeros((128, 4), np.float32)
        sel[:, 0] = 1.0 if s == 0 else 0.0
        sel[:, 1] = 1.0 if s == 1 else 0.0
        cm = np.stack([tri if s == 0 else np.ones_like(tri), np.zeros_like(tri) if s == 0 else tri]).astype(np.float32)
        m = dict(shared)
        m.update({"x": np.ascontiguousarray(p["x"][b, :T]), "vec_col": vc, "sel": sel, "cmask": cm})
        maps.append(m)
    return maps


_CACHE = {}


def kernel(**inputs):
    T = int(np.asarray(inputs["x"]).shape[1])
    if T not in _CACHE:
        _CACHE[T] = build_program(T)[0]
    nc = _CACHE[T]
    maps = make_in_maps(inputs)
    res = run_bass_kernel_spmd(nc, maps, core_ids=list(range(8)))
    B = np.asarray(inputs["x"]).shape[0]
    outp = np.zeros((B, T, D), np.float32)
    for core in range(8):
        b, s = core // 2, core % 2
        o = np.asarray(res.results[core]["out"]).reshape(T // 256, 128, D)
        outp[b].reshape(T // 128, 128, D)[s::2] = o
    return outp
```

```python
import math
import os
from contextlib import ExitStack

import numpy as np
import concourse.bass as bass
import concourse.mybir as mybir
from concourse.bass_utils import run_bass_kernel_spmd

F32 = mybir.dt.float32
BF16 = mybir.dt.bfloat16
I32 = mybir.dt.int32
AF = mybir.ActivationFunctionType
ALU = mybir.AluOpType
AX = mybir.AxisListType

D = 1024
NH = 16
HN = 64
NEXP = 32
EFF = 512
C0 = -math.exp(-0.5)


class Buf:
    __slots__ = ("name", "w", "r")

    def __init__(self, name=""):
        self.name = name
        self.w = None
        self.r = {}


class KB:
    def __init__(self, nc, n_dma_sems=(16, 12, 6)):
        self.nc = nc
        self.eng = {"pe": nc.tensor, "act": nc.scalar, "dve": nc.vector, "pool": nc.gpsimd, "sp": nc.sync}
        self.sems = {}
        self.cnt = {}
        self._ctx = []
        for e in ("pe", "act", "dve", "pool"):
            self._mksem("c_" + e)
        self.dma_pool = {}
        for q, n in zip(("sp", "pool", "act"), n_dma_sems):
            keys = []
            for i in range(n):
                k = f"d_{q}{i}"
                self._mksem(k)
                keys.append(k)
            self.dma_pool[q] = [keys, 0]
        self.seen = {e: {} for e in self.eng}
        self.n_inst = 0
        self.n_wait = 0

    def _mksem(self, key):
        g = self.nc.semaphore(key)
        s = g.__enter__()
        self._ctx.append(g)
        self.sems[key] = s
        self.cnt[key] = 0

    def close(self):
        for g in reversed(self._ctx):
            g.__exit__(None, None, None)

    def _wait(self, e, ticket):
        if ticket is None:
            return
        key, val = ticket
        if self.seen[e].get(key, 0) >= val:
            return
        self.eng[e].wait_ge(self.sems[key], val)
        self.seen[e][key] = val
        self.n_wait += 1

    def _mark(self, ticket, reads, writes):
        k, v = ticket
        for b in reads:
            if b.r.get(k, 0) < v:
                b.r[k] = v
        for b in writes:
            b.w = ticket
            b.r = {}

    def op(self, e, fn, reads=(), writes=()):
        own = "c_" + e
        for b in reads:
            self._wait(e, b.w)
        for b in writes:
            if b.w is not None and (b.w[0] != own or e != "pe"):
                self._wait(e, b.w)
            for k, v in b.r.items():
                if k != own or e != "pe":
                    self._wait(e, (k, v))
        inst = fn(self.eng[e])
        self.cnt[own] += 1
        inst.then_inc(self.sems[own], 1)
        self._mark((own, self.cnt[own]), reads, writes)
        self.n_inst += 1
        return inst

    def dma(self, q, out, in_, reads=(), writes=(), **kw):
        keys, idx = self.dma_pool[q]
        key = keys[idx % len(keys)]
        self.dma_pool[q][1] = idx + 1
        if self.cnt[key] > 0:
            self._wait(q, (key, self.cnt[key]))
        for b in reads:
            self._wait(q, b.w)
        for b in writes:
            self._wait(q, b.w)
            for k, v in b.r.items():
                self._wait(q, (k, v))
        inst = self.eng[q].dma_start(out=out, in_=in_, **kw)
        self.cnt[key] += 16
        inst.then_inc(self.sems[key], 16)
        self._mark((key, self.cnt[key]), reads, writes)
        self.n_inst += 1
        return inst

    def barrier(self):
        for e in self.eng:
            for key, val in self.cnt.items():
                if val > 0:
                    self._wait(e, (key, val))

    def finish(self, e, bufs):
        for b in bufs:
            self._wait(e, b.w)


class T_:
    def __init__(self, t, name):
        self.t = t
        self.b = Buf(name)

    def __getitem__(self, k):
        return self.t[k]

    def v(self, k):
        return View(self.t[k], self.b)


class _APWrap:
    def __init__(self, ap):
        self.ap = ap

    def __getitem__(self, k):
        return self.ap[k]


class View:
    def __init__(self, ap, b):
        self.ap = ap
        self.b = b


def _ap(x):
    return x.ap if isinstance(x, View) else x.t[:]


VC_C, VC_NMIX0, VC_NFFN0, VC_NMIX1, VC_NFFN1, VC_NKV = 0, 1, 2, 3, 4, 5
VC_MU = 6
VC_ADAB0 = 12
VC_ADAB1 = 18
VC_KVB = 24
N_VC = 26
VR_KK, VR_KA, VR_RK, VR_GNG, VR_GNB, VR_W0, VR_A0, VR_FING = 0, 1, 2, 3, 4, 5, 6, 7
VR_GM0, VR_GF0, VR_GM1, VR_GF1 = 8, 9, 10, 11
VR_SUBLN = 12
N_VR = 13


def build_program(T, dbg=0, phases=("p0", "p1", "p2", "p3", "p4", "p5", "moe")):
    NT = T // 128
    nc = bass.Bass("TRN2", target_bir_lowering=False)
    kb = KB(nc)
    es = ExitStack()

    in_names = []

    def din(name, shape, dt=F32, need=True):
        if not need:
            return None
        in_names.append(name)
        return nc.dram_tensor(name, list(shape), dt, kind="ExternalInput").ap()

    has = lambda ph: ph in phases

    dbg_outs = []

    def dscr(name, shape, dt=F32, tap=False):
        kind = "ExternalOutput" if (dbg and tap) else "Internal"
        if dbg and tap:
            dbg_outs.append(name)
        return nc.dram_tensor(name, list(shape), dt, kind=kind).ap()

    x_in = din("x", [T, D])
    vec_col = din("vec_col", [128, N_VC, 8])
    vec_row = din("vec_row", [N_VR, D])
    ada_w = din("ada_w", [2, D, 6 * D])
    ada_kv_w = din("ada_kv_w", [D, 2 * D])
    w_rkv = din("rw_w_rkv", [3, D, D], need=has("p1"))
    w1a_in = din("rw_w1a", [D, 128], need=has("p1"))
    g1_in = din("rw_g1", [D, 128], need=has("p1"))
    w2a2_in = din("rw_w2a2", [128, D], need=has("p1"))
    g2_in = din("rw_g2", [128, D], need=has("p1"))
    rw_wo = din("rw_w_o", [D, D], need=has("p3"))
    w_kv = din("w_kv", [D, 2 * D], need=has("p4"))
    df_wq = din("df_w_q", [D, D], need=has("p5"))
    df_wo = din("df_w_o", [D, D], need=has("p5"))
    moe_wr = din("moe_wr", [2, D, 36], need=has("moe"))
    moe_wg = din("moe_w_gate", [2, NEXP * 128, 4096], need=has("moe"))
    moe_wu = din("moe_w_up", [2, NEXP * 128, 4096], need=has("moe"))
    moe_wd = din("moe_w_down", [2, NEXP * 128, 4096], need=has("moe"))
    sel_in = din("sel", [128, 4], need=has("p5"))
    cmask_in = din("cmask", [2, 128, 128], need=has("p5"))
    lam_in = din("lam", [128, 4], need=has("p5"))
    out = nc.dram_tensor("out", [T // 2, D], F32, kind="ExternalOutput").ap()

    XF = [dscr(f"xf{q}", [NT, 128, D], BF16, tap=(dbg == 1)) for q in range(4)]
    TKV = [dscr(f"tk{q}", [T, D], BF16, tap=(dbg == 1)) for q in range(3)]
    GSC = dscr("gsc", [T, D], F32, tap=(dbg == 1))
    BON = dscr("bon", [T, D], F32, tap=(dbg == 1))
    GAMD = dscr("gamd", [128, 8, 2 * NT], F32, tap=(dbg == 1))
    YSC = dscr("ysc", [T, D], F32, tap=(dbg == 2))

    def sb(name, shape, dt=F32):
        t = es.enter_context(nc.sbuf_tensor(name, list(shape), dt))
        return T_(t, name)

    def ps(name, shape, dt=F32):
        t = es.enter_context(nc.psum_tensor(name, list(shape), dt))
        return T_(t, name)

    ident = sb("ident", [128, 128])
    identb = sb("identb", [128, 128], BF16)
    ones = sb("ones", [128, 128])
    onesb = sb("onesb", [128, 128], BF16)
    vcol = sb("vcol", [128, N_VC, 8])
    modc = sb("modc", [128, 12, 8])
    AB = sb("AB", [128, 12, 8])
    cact = sb("cact", [128, 8])

    kb.op("pool", lambda e: e.memset(ident[:], 0.0), writes=[ident.b])
    kb.op("pool", lambda e: e.affine_select(out=ident[:], in_=ident[:], pattern=[[-1, 128]], compare_op=ALU.not_equal,
                                            fill=1.0, base=0, channel_multiplier=1), reads=[ident.b], writes=[ident.b])
    kb.op("pool", lambda e: e.tensor_copy(out=identb[:], in_=ident[:]), reads=[ident.b], writes=[identb.b])
    kb.op("pool", lambda e: e.memset(ones[:], 1.0), writes=[ones.b])
    kb.op("pool", lambda e: e.memset(onesb[:], 1.0), writes=[onesb.b])
    kb.dma("sp", vcol[:], vec_col[:, :, :], writes=[vcol.b])
    kb.op("act", lambda e: e.activation(out=cact[:], in_=vcol[:, VC_C, :], func=AF.Silu), reads=[vcol.b], writes=[cact.b])

    def bc_row(dst, row):
        kb.dma("sp", dst[:], row.partition_broadcast(128), writes=[dst.b])

    gmb = [sb(f"gmb{i}", [128, D]) for i in range(4)]
    with ExitStack() as es0:
        wst = [T_(es0.enter_context(nc.sbuf_tensor(f"wst{i}", [128, 8, D], F32)), f"wst{i}") for i in range(2)]
        cbc = T_(es0.enter_context(nc.sbuf_tensor("cbc", [128, 8, 128], F32)), "cbc")
        pcol = T_(es0.enter_context(nc.psum_tensor("pcol", [128, 8], F32)), "pcol")
        prow = T_(es0.enter_context(nc.psum_tensor("prow", [128, D], F32)), "prow")
        brow = T_(es0.enter_context(nc.sbuf_tensor("brow", [128, D], F32)), "brow")
        for m in range(8):
            kb.op("dve", lambda e, m=m: e.tensor_scalar(out=cbc[:, m, :], in0=ones[:], scalar1=cact[:, m:m + 1], scalar2=None,
                                                        op0=ALU.mult), reads=[ones.b, cact.b], writes=[cbc.b])
        jobs = []
        for l in range(2):
            base = VC_ADAB0 if l == 0 else VC_ADAB1
            jobs += [(ada_w[l], 0 * D, "col", 4 * l + 0, base + 0), (ada_w[l], 1 * D, "col", 4 * l + 1, base + 1),
                     (ada_w[l], 3 * D, "col", 4 * l + 2, base + 3), (ada_w[l], 4 * D, "col", 4 * l + 3, base + 4),
                     (ada_w[l], 2 * D, "row", 2 * l + 0, VR_GM0 + 2 * l), (ada_w[l], 5 * D, "row", 2 * l + 1, VR_GF0 + 2 * l)]
        jobs += [(ada_kv_w, 0, "col", 8, VC_KVB), (ada_kv_w, D, "col", 9, VC_KVB + 1)]
        for ji, (src, off, kind, di, bi) in enumerate(jobs):
            w = wst[ji % 2]
            kb.dma("sp" if ji % 2 == 0 else "pool", w[:], src[:, off:off + D].rearrange("(m p) n -> p m n", p=128), writes=[w.b])
            if kind == "col":
                for fc in range(8):
                    for mk in range(8):
                        kb.op("pe", lambda e, fc=fc, mk=mk, w=w: e.matmul(pcol[:, fc:fc + 1], lhsT=w[:, mk, fc * 128:(fc + 1) * 128],
                                                                           rhs=cact[:, mk:mk + 1], start=(mk == 0), stop=(mk == 7)),
                              reads=[w.b, cact.b], writes=[pcol.b])
                kb.op("dve", lambda e, di=di, bi=bi: e.tensor_tensor(out=modc[:, di, :], in0=pcol[:], in1=vcol[:, bi, :], op=ALU.add),
                      reads=[pcol.b, vcol.b], writes=[modc.b])
            else:
                for hf in range(2):
                    for mk in range(8):
                        kb.op("pe", lambda e, hf=hf, mk=mk, w=w: e.matmul(prow[:, hf * 512:(hf + 1) * 512], lhsT=cbc[:, mk, :],
                                                                           rhs=w[:, mk, hf * 512:(hf + 1) * 512], start=(mk == 0), stop=(mk == 7)),
                              reads=[w.b, cbc.b], writes=[prow.b])
                bc_row(brow, vec_row[bi:bi + 1, :])
                kb.op("dve", lambda e, di=di: e.tensor_tensor(out=gmb[di][:], in0=prow[:], in1=brow[:], op=ALU.add),
                      reads=[prow.b, brow.b], writes=[gmb[di].b])
        for (ai, gi, shi, sci) in [(0, VC_NMIX0, 0, 1), (2, VC_NFFN0, 2, 3), (4, VC_NMIX1, 4, 5), (6, VC_NFFN1, 6, 7), (8, VC_NKV, 8, 9)]:
            kb.op("dve", lambda e, ai=ai, sci=sci: e.tensor_scalar(out=AB[:, ai, :], in0=modc[:, sci, :], scalar1=1.0, scalar2=32.0,
                                                                  op0=ALU.add, op1=ALU.mult), reads=[modc.b], writes=[AB.b])
            kb.op("dve", lambda e, ai=ai, gi=gi: e.tensor_tensor(out=AB[:, ai, :], in0=AB[:, ai, :], in1=vcol[:, gi, :], op=ALU.mult),
                  reads=[AB.b, vcol.b], writes=[AB.b])
            kb.op("dve", lambda e, ai=ai, shi=shi: e.tensor_copy(out=AB[:, ai + 1, :], in_=modc[:, shi, :]), reads=[modc.b], writes=[AB.b])

    def TT(e, o, a, b_, op):
        kb.op(e, lambda en: en.tensor_tensor(out=_ap(o), in0=_ap(a), in1=_ap(b_), op=op), reads=[a.b, b_.b], writes=[o.b])

    def TS(e, o, a, s1, s2, op0, op1=None):
        rd = [a.b] + [z.b for z in (s1, s2) if isinstance(z, (View, T_))]
        f = lambda z: _ap(z) if isinstance(z, (View, T_)) else z
        if op1 is None:
            kb.op(e, lambda en: en.tensor_scalar(out=_ap(o), in0=_ap(a), scalar1=f(s1), scalar2=None, op0=op0), reads=rd, writes=[o.b])
        else:
            kb.op(e, lambda en: en.tensor_scalar(out=_ap(o), in0=_ap(a), scalar1=f(s1), scalar2=f(s2), op0=op0, op1=op1), reads=rd, writes=[o.b])

    def STT(e, o, a, sc, b_, op0, op1):
        rd = [a.b, b_.b] + ([sc.b] if isinstance(sc, (View, T_)) else [])
        f = lambda z: _ap(z) if isinstance(z, (View, T_)) else z
        kb.op(e, lambda en: en.scalar_tensor_tensor(out=_ap(o), in0=_ap(a), scalar=f(sc), in1=_ap(b_), op0=op0, op1=op1), reads=rd, writes=[o.b])

    def ACT(o, a, func, scale=1.0, bias=0.0, accum=None):
        rd = [a.b] + [z.b for z in (scale, bias) if isinstance(z, (View, T_))]
        wr = [o.b] + ([accum.b] if accum is not None else [])
        f = lambda z: _ap(z) if isinstance(z, (View, T_)) else z
        kw = {}
        if accum is not None:
            kw["accum_out"] = _ap(accum)
        kb.op("act", lambda en: en.activation(out=_ap(o), in_=_ap(a), func=func, bias=f(bias), scale=f(scale), **kw), reads=rd, writes=wr)

    def CP(e, o, a):
        if e == "act":
            kb.op(e, lambda en: en.copy(out=_ap(o), in_=_ap(a)), reads=[a.b], writes=[o.b])
        else:
            kb.op(e, lambda en: en.tensor_copy(out=_ap(o), in_=_ap(a)), reads=[a.b], writes=[o.b])

    def RED(e, o, a, op=ALU.add, axis=AX.X):
        kb.op(e, lambda en: en.tensor_reduce(out=_ap(o), in_=_ap(a), axis=axis, op=op), reads=[a.b], writes=[o.b])

    def MM(o, lhsT, rhs, start=True, stop=True):
        kb.op("pe", lambda en: en.matmul(_ap(o), lhsT=_ap(lhsT), rhs=_ap(rhs), start=start, stop=stop), reads=[lhsT.b, rhs.b], writes=[o.b])

    def TR(o, a, idt):
        kb.op("pe", lambda en: en.transpose(out=_ap(o), in_=_ap(a), identity=_ap(idt)), reads=[a.b, idt.b], writes=[o.b])

    def DMA(q, o_ap, i_ap, reads=(), writes=()):
        kb.dma(q, o_ap, i_ap, reads=[r.b for r in reads], writes=[w.b for w in writes])

    class DR:
        def __init__(self, name):
            self.b = Buf(name)

    taps = {}
    if dbg == 9:
        o_ab = nc.dram_tensor("o_ab", [128, 12, 8], F32, kind="ExternalOutput").ap()
        o_gm = nc.dram_tensor("o_gm", [4, 128, D], F32, kind="ExternalOutput").ap()
        bo = Buf("o")
        kb.dma("sp", o_ab[:, :, :], AB[:], reads=[AB.b], writes=[bo])
        for i in range(4):
            kb.dma("sp", o_gm[i], gmb[i][:], reads=[gmb[i].b], writes=[bo])
        kb.finish("sp", [bo])
        kb.close()
        es.close()
        return nc, in_names, ["o_ab", "o_gm"]

    gamT = sb("gamT", [128, 8, 2 * NT])
    dXF = [[DR(f"xf{q}_{t}") for t in range(NT)] for q in range(4)]
    dTK = [[DR(f"tk{q}_{t}") for t in range(NT)] for q in range(3)]
    dG = [DR(f"g{t}") for t in range(NT)]
    dBON = [DR(f"bon{t}") for t in range(NT)]
    rowsl = lambda t: slice(t * 128, (t + 1) * 128)
    kb.barrier()
    if has("p1"):
        with ExitStack() as es1:
            def sb1(name, shape, dt=F32):
                return T_(es1.enter_context(nc.sbuf_tensor(name, list(shape), dt)), name)

            def ps1(name, shape, dt=F32):
                return T_(es1.enter_context(nc.psum_tensor(name, list(shape), dt)), name)

            Wp = [sb1(f"Wp{i}", [128, 8, D], BF16) for i in range(3)]
            W1 = [sb1(f"W1{i}", [128, 8, 128], BF16) for i in range(4)]
            w2a2 = sb1("w2a2", [128, D], BF16)
            g2 = sb1("g2", [128, D], BF16)
            b0row = sb1("b0row", [1, 2, D])
            kk_bc, ka_bc, omka_bc, rk_bc = [sb1(n, [128, D]) for n in ("kk_bc", "ka_bc", "omka_bc", "rk_bc")]
            hT = sb1("hT", [128, 8, 129])
            F = [sb1(f"F{i}", [128, D]) for i in range(16)]
            Hb = [sb1(f"Hb{i}", [128, 8, 128], BF16) for i in range(5)]
            Q = [sb1(f"Q{i}", [128, D], BF16) for i in range(7)]
            XFs = [sb1(f"XFs{i}", [128, D], BF16) for i in range(2)]
            wlal = sb1("wlal", [128, 128], BF16)
            glT = sb1("glT", [128, 128], BF16)
            Ltri = sb1("Ltri", [128, 128])
            Lblk = sb1("Lblk", [128, 128])
            ind2 = sb1("ind2", [128, 2])
            ss = sb1("ss", [128, 1])
            rs = sb1("rs", [128, 1])
            ssh = sb1("ssh", [128, 16])
            rnh = sb1("rnh", [128, 16])
            bsum = sb1("bsum", [128, 16])
            PA, PB, PC = [ps1(n, [128, D]) for n in ("PA", "PB", "PC")]
            PS = ps1("PS", [128, 512])
            PT = ps1("PT", [128, D], BF16)
            cs = lambda m: slice(m * 128, (m + 1) * 128)
            hs = lambda h: slice(h * 512, (h + 1) * 512)

            engs = ["act", "dve", "pool"]
            n = 0
            for i in range(3):
                for m in range(8):
                    st = F[14 + n % 2]
                    DMA("sp" if n % 2 == 0 else "pool", st[:], w_rkv[i, cs(m), :], writes=[st])
                    CP(engs[n % 3], Wp[i].v((slice(None), m, slice(None))), st)
                    n += 1
            st3 = lambda t: View(t.t[:].rearrange("p (m n) -> p m n", n=128), t.b)
            for j, (src, mus) in enumerate([(w1a_in, (3, 4)), (g1_in, (5, 5))]):
                st = F[13]
                DMA("sp", st3(st).ap, src.rearrange("(m p) n -> p m n", p=128), writes=[st])
                CP("dve", W1[2 * j], st3(st))
                for m in range(8):
                    for hh in range(2):
                        TS("pool" if hh else "dve", W1[2 * j + 1].v((slice(None), m, slice(hh * 64, hh * 64 + 64))),
                           View(st3(st).ap[:, m, hh * 64:hh * 64 + 64], st.b), vcol.v((slice(None), VC_MU + mus[hh], slice(m, m + 1))), None, ALU.mult)
            DMA("sp", F[12][:], w2a2_in[:, :], writes=[F[12]])
            CP("act", w2a2, F[12])
            DMA("sp", F[11][:], g2_in[:, :], writes=[F[11]])
            CP("act", g2, F[11])
            DMA("sp", b0row[0:1, 0, :], vec_row[VR_W0:VR_W0 + 1, :], writes=[b0row])
            DMA("sp", b0row[0:1, 1, :], vec_row[VR_A0:VR_A0 + 1, :], writes=[b0row])
            bc_row(kk_bc, vec_row[VR_KK:VR_KK + 1, :])
            bc_row(ka_bc, vec_row[VR_KA:VR_KA + 1, :])
            bc_row(rk_bc, vec_row[VR_RK:VR_RK + 1, :])
            TS("dve", omka_bc, ka_bc, -1.0, 1.0, ALU.mult, ALU.add)
            kb.op("pool", lambda e: e.memset(Lblk[:], 0.0), writes=[Lblk.b])
            kb.op("pool", lambda e: e.memset(Lblk[0:64, 0:64], 1.0), writes=[Lblk.b])
            kb.op("pool", lambda e: e.memset(Lblk[64:128, 64:128], 1.0), writes=[Lblk.b])
            CP("pool", Ltri, Lblk)
            for h0 in (0, 64):
                kb.op("pool", lambda e, h0=h0: e.affine_select(out=Ltri[h0:h0 + 64, h0:h0 + 64], in_=Ltri[h0:h0 + 64, h0:h0 + 64], pattern=[[1, 64]],
                                                              compare_op=ALU.is_ge, fill=0.0, base=0, channel_multiplier=-1),
                      reads=[Ltri.b], writes=[Ltri.b])
            kb.op("pool", lambda e: e.memset(ind2[:], 0.0), writes=[ind2.b])
            kb.op("pool", lambda e: e.memset(ind2[0:64, 0:1], 1.0), writes=[ind2.b])
            kb.op("pool", lambda e: e.memset(ind2[64:128, 1:2], 1.0), writes=[ind2.b])
            kb.op("pool", lambda e: e.memset(hT[:, :, 0:1], 0.0), writes=[hT.b])

            v3 = lambda t: View(t.t[:].rearrange("p (m n) -> p m n", n=128), t.b)
            vh = lambda t: View(t.t[:].rearrange("p (h n) -> p h n", n=64), t.b)
            bch = lambda t: View(t.t[:].unsqueeze(2).broadcast_to([128, 16, 64]), t.b)
            for tt in range(NT):
                DMA("sp", F[0][:], x_in[rowsl(tt), :], writes=[F[0]])
                ACT(F[1], F[0], AF.Square, accum=ss)
                ACT(rs, ss, AF.Ln, bias=1024e-6)
                ACT(rs, rs, AF.Exp, scale=-0.5)
                TS("dve", F[1], F[0], rs, None, ALU.mult)
                for m in range(8):
                    TR(PA.v((slice(None), cs(m))), F[1].v((slice(None), cs(m))), ident)
                for m in range(8):
                    ACT(hT.v((slice(None), m, slice(1, 129))), PA.v((slice(None), cs(m))), AF.Identity,
                        scale=AB.v((slice(None), 0, slice(m, m + 1))), bias=AB.v((slice(None), 1, slice(m, m + 1))))
                hprev = hT.v((slice(None), slice(None), slice(0, 128)))
                hcur = hT.v((slice(None), slice(None), slice(1, 129)))
                TT("dve", v3(F[2]), hprev, hcur, ALU.subtract)
                CP("pool", Hb[0], hcur)
                CP("pool", Hb[1], v3(F[2]))
                n = 0
                for i in range(3):
                    for m in range(8):
                        STT("dve", Hb[2 + i].v((slice(None), m, slice(None))), F[2].v((slice(None), cs(m))),
                            vcol.v((slice(None), VC_MU + i, slice(m, m + 1))), hT.v((slice(None), m, slice(1, 129))), ALU.mult, ALU.add)
                        n += 1
                CP("pool", hT.v((slice(None), slice(None), slice(0, 1))), hT.v((slice(None), slice(None), slice(128, 129))))
                for j in range(2):
                    o = PS.v((slice(None), slice(j * 128, (j + 1) * 128)))
                    for mk in range(8):
                        MM(o, W1[2 * j].v((slice(None), mk, slice(None))), Hb[0].v((slice(None), mk, slice(None))), start=(mk == 0), stop=False)
                        MM(o, W1[2 * j + 1].v((slice(None), mk, slice(None))), Hb[1].v((slice(None), mk, slice(None))), start=False, stop=(mk == 7))
                ACT(wlal.v((slice(0, 64), slice(None))), PS.v((slice(0, 64), slice(0, 128))), AF.Tanh)
                ACT(wlal.v((slice(64, 128), slice(None))), PS.v((slice(64, 128), slice(0, 128))), AF.Identity)
                ACT(glT, PS.v((slice(None), slice(128, 256))), AF.Sigmoid)
                for i, P_ in enumerate((PB, PC, PA)):
                    for h in range(2):
                        for mk in range(8):
                            MM(P_.v((slice(None), hs(h))), Hb[2 + i].v((slice(None), mk, slice(None))), Wp[i].v((slice(None), mk, hs(h))),
                               start=(mk == 0), stop=(mk == 7))
                CP("dve", F[0], PB)
                CP("act", F[1], PC)
                CP("act", F[2], PA)
                for h in range(2):
                    MM(PB.v((slice(None), hs(h))), wlal.v((slice(0, 64), slice(None))), w2a2.v((slice(0, 64), hs(h))), start=True, stop=False)
                    MM(PB.v((slice(None), hs(h))), ones.v((slice(0, 1), slice(None))), b0row.v((slice(0, 1), 0, hs(h))), start=False, stop=True)
                for h in range(2):
                    MM(PC.v((slice(None), hs(h))), wlal.v((slice(64, 128), slice(None))), w2a2.v((slice(64, 128), hs(h))), start=True, stop=False)
                    MM(PC.v((slice(None), hs(h))), ones.v((slice(0, 1), slice(None))), b0row.v((slice(0, 1), 1, hs(h))), start=False, stop=True)
                for h in range(2):
                    MM(PA.v((slice(None), hs(h))), glT, g2.v((slice(None), hs(h))))
                ACT(F[3], PB, AF.Sigmoid)
                ACT(F[4], PC, AF.Sigmoid)
                CP("act", F[5], PA)
                DMA("pool", GSC[rowsl(tt), :], F[5][:], reads=[F[5]], writes=[dG[tt]])
                for h in range(2):
                    MM(PB.v((slice(None), hs(h))), Ltri, F[3].v((slice(None), hs(h))))
                for h in range(2):
                    MM(PC.v((slice(None), hs(h))), Lblk, F[3].v((slice(None), hs(h))))
                for m in range(8):
                    MM(PS.v((slice(None), slice(256 + 2 * m, 258 + 2 * m))), F[3].v((slice(None), cs(m))), ind2)
                ACT(F[11], PB, AF.Exp, scale=C0)
                ACT(F[13], PB, AF.Exp, scale=-C0)
                ACT(F[10], F[3], AF.Exp, scale=-C0)
                ACT(F[14], PC, AF.Exp, scale=C0)
                ACT(gamT.v((slice(None), slice(None), slice(2 * tt, 2 * tt + 2))),
                    View(PS.t[:, 256:272].rearrange("p (m c) -> p m c", c=2), PS.b), AF.Exp, scale=C0)
                TT("pool", F[12], F[11], F[10], ALU.mult)
                TT("pool", F[14], F[14], F[13], ALU.mult)
                TT("dve", F[6], F[1], kk_bc, ALU.mult)
                TT("pool", F[7], F[6], F[6], ALU.mult)
                RED("dve", ssh, vh(F[7]))
                ACT(rnh, ssh, AF.Ln, bias=1e-12)
                ACT(rnh, rnh, AF.Exp, scale=-0.5)
                TT("dve", vh(F[6]), vh(F[6]), bch(rnh), ALU.mult)
                TT("pool", F[7], F[4], ka_bc, ALU.mult)
                TT("pool", F[7], F[7], omka_bc, ALU.add)
                TT("dve", F[8], F[1], F[7], ALU.mult)
                TT("pool", F[9], F[6], F[4], ALU.mult)
                STT("dve", Q[0], F[6], -1.0, F[12], ALU.mult, ALU.mult)
                TT("dve", Q[1], F[0], F[11], ALU.mult)
                TT("pool", Q[2], F[9], F[13], ALU.mult)
                TT("dve", Q[3], F[8], F[13], ALU.mult)
                TT("pool", Q[4], F[9], F[14], ALU.mult)
                TT("dve", Q[5], F[8], F[14], ALU.mult)
                CP("pool", Q[6], F[2])
                DMA("pool", TKV[0][rowsl(tt), :], Q[6][:], reads=[Q[6]], writes=[dTK[0][tt]])
                DMA("pool", TKV[1][rowsl(tt), :], Q[4][:], reads=[Q[4]], writes=[dTK[1][tt]])
                DMA("pool", TKV[2][rowsl(tt), :], Q[5][:], reads=[Q[5]], writes=[dTK[2][tt]])
                for q in range(4):
                    for m in range(8):
                        TR(PT.v((slice(None), cs(m))), Q[q].v((slice(None), cs(m))), identb)
                    CP("act" if q % 2 == 0 else "dve", XFs[q % 2], PT)
                    DMA("sp", XF[q][tt], XFs[q % 2][:], reads=[XFs[q % 2]], writes=[dXF[q][tt]])
                TT("pool", F[7], F[0], F[8], ALU.mult)
                TT("pool", F[7], F[7], rk_bc, ALU.mult)
                RED("dve", bsum, vh(F[7]))
                TT("dve", vh(F[15]), vh(F[2]), bch(bsum), ALU.mult)
                DMA("pool", BON[rowsl(tt), :], F[15][:], reads=[F[15]], writes=[dBON[tt]])
    dGAM = DR("gamd")
    if dbg == 1:
        DMA("sp", GAMD[:, :, :], gamT[:], reads=[gamT], writes=[dGAM])
        allb = [d.b for l_ in dXF + dTK for d in l_] + [d.b for d in dG + dBON] + [dGAM.b]
        kb.finish("sp", allb)
        kb.finish("pool", allb)
        kb.close()
        es.close()
        return nc, in_names, dbg_outs

    NC = T // 64
    dY = [DR(f"y{c}") for c in range(NC)]
    kb.barrier()
    if has("p2"):
        with ExitStack() as es2:
            def sb2(name, shape, dt=F32):
                return T_(es2.enter_context(nc.sbuf_tensor(name, list(shape), dt)), name)

            def ps2(name, shape, dt=F32):
                return T_(es2.enter_context(nc.psum_tensor(name, list(shape), dt)), name)

            XFt = [[sb2(f"XFt{q}_{i}", [64, 16, 64], BF16) for q in range(4)] for i in range(2)]
            TKt = [[sb2(f"TKt{q}_{i}", [64, 16, 64], BF16) for q in range(3)] for i in range(2)]
            m_su, m_iu, m_sl, I16 = [sb2(n, [64, 16, 64]) for n in ("m_su", "m_iu", "m_sl", "I16")]
            Pm = [sb2(f"Pm{i}", [64, 16, 64], BF16) for i in range(2)]
            PTm = [sb2(f"PTm{i}", [64, 16, 64], BF16) for i in range(2)]
            Tm = sb2("Tm", [64, 16, 64], BF16)
            Aak, Arb, Ark = [sb2(n, [64, 16, 64], BF16) for n in ("Aak", "Arb", "Ark")]
            W1T = sb2("W1T", [64, 16, 64], BF16)
            UT = sb2("UT", [64, 16, 64], BF16)
            ST = sb2("ST", [64, 16, 64])
            STb = sb2("STb", [64, 16, 64], BF16)
            gam = sb2("gam", [64, 16, NC])
            Ysb = [sb2(f"Ysb{i}", [64, 16, 64]) for i in range(2)]
            PG = [ps2(f"PG{i}", [64, 16, 64]) for i in range(2)]
            PW = ps2("PW", [64, 16, 64])
            PSt = ps2("PSt", [64, 16, 64])
            A_ = slice(None)

            for (mt, pat, cm, op) in [(m_su, [[0, 16], [1, 64]], -1, ALU.is_gt), (m_iu, [[0, 16], [1, 64]], -1, ALU.is_ge),
                                      (m_sl, [[0, 16], [-1, 64]], 1, ALU.is_gt), (I16, [[0, 16], [1, 64]], -1, ALU.is_equal)]:
                kb.op("pool", lambda e, mt=mt: e.memset(mt[:], 1.0), writes=[mt.b])
                kb.op("pool", lambda e, mt=mt, pat=pat, cm=cm, op=op: e.affine_select(
                    out=mt[:], in_=mt[:], pattern=pat, compare_op=op, fill=0.0, base=0, channel_multiplier=cm),
                    reads=[mt.b], writes=[mt.b])
            kb.op("pool", lambda e: e.memset(ST[:], 0.0), writes=[ST.b])
            kb.op("pool", lambda e: e.memset(STb[:], 0.0), writes=[STb.b])
            DMA("sp", GAMD[:, :, :], gamT[:], reads=[gamT], writes=[dGAM])
            DMA("sp", gam[:].rearrange("j (m p) c -> j m p c", p=2), GAMD.rearrange("(p j) m c -> j m p c", p=2), reads=[dGAM], writes=[gam])

            def loads(c):
                i = c % 2
                tt, ci = c // 2, c % 2
                for q in range(4):
                    src = XF[q][tt].rearrange("(p j) (m t) -> j m p t", p=2, t=128)[:, :, :, ci * 64:(ci + 1) * 64]
                    DMA("sp", XFt[i][q][:].rearrange("j (m p) t -> j m p t", p=2), src, reads=[dXF[q][tt]], writes=[XFt[i][q]])
                for q in range(3):
                    DMA("sp", TKt[i][q][:].rearrange("s h n -> s (h n)"), TKV[q][c * 64:(c + 1) * 64, :], reads=[dTK[q][tt]], writes=[TKt[i][q]])

            def headmm(o, lt, rt, **kw):
                for h in range(16):
                    MM(o.v((A_, h, A_)), lt.v((A_, h, A_)), rt.v((A_, h, A_)), **kw)

            P2STOP = int(os.environ.get("P2STOP", "0"))
            loads(0)
            for c in range(NC):
                if c + 1 < NC:
                    loads(c + 1)
                At_, Rt_, Bt_, Kt_ = XFt[c % 2]
                Vt_, Bh_, Kh_ = TKt[c % 2]
                headmm(PG[0], Bt_, At_)
                headmm(PG[1], At_, Bt_)
                TT("dve", Pm[0], PG[0], m_su, ALU.mult)
                TT("dve", PTm[0], PG[1], m_sl, ALU.mult)
                TT("pool", Tm, Pm[0], I16, ALU.add)
                headmm(PG[0], Kt_, At_)
                headmm(PG[1], Bt_, Rt_)
                TT("dve", Aak, PG[0], m_su, ALU.mult)
                TT("dve", Arb, PG[1], m_iu, ALU.mult)
                headmm(PG[0], Kt_, Rt_)
                TT("dve", Ark, PG[0], m_iu, ALU.mult)
                if P2STOP == 2:
                    continue
                cur = 0
                for lvl in range(1, 6):
                    nxt = 1 - cur
                    headmm(PG[1], Pm[cur], PTm[cur])
                    if lvl < 5:
                        headmm(PG[0], PTm[cur], Pm[cur])
                    CP("act", PTm[nxt], PG[1])
                    if lvl < 5:
                        CP("act", Pm[nxt], PG[0])
                    pg = PG[0] if lvl == 5 else PG[1]
                    headmm(pg, PTm[nxt], Tm)
                    TT("dve", Tm, Tm, pg, ALU.add)
                    cur = nxt
                if P2STOP == 3:
                    continue
                Y_ = Ysb[c % 2]
                for h in range(16):
                    MM(PW.v((A_, h, A_)), At_.v((A_, h, A_)), STb.v((A_, h, A_)), start=True, stop=False)
                    MM(PW.v((A_, h, A_)), Aak.v((A_, h, A_)), Vt_.v((A_, h, A_)), start=False, stop=True)
                CP("act", W1T, PW)
                headmm(PW, Tm, W1T)
                CP("act", UT, PW)
                for h in range(16):
                    MM(PSt.v((A_, h, A_)), Bh_.v((A_, h, A_)), UT.v((A_, h, A_)), start=True, stop=False)
                    MM(PSt.v((A_, h, A_)), Kh_.v((A_, h, A_)), Vt_.v((A_, h, A_)), start=False, stop=True)
                for h in range(16):
                    MM(PW.v((A_, h, A_)), Rt_.v((A_, h, A_)), STb.v((A_, h, A_)), start=True, stop=False)
                    MM(PW.v((A_, h, A_)), Arb.v((A_, h, A_)), UT.v((A_, h, A_)), start=False, stop=False)
                    MM(PW.v((A_, h, A_)), Ark.v((A_, h, A_)), Vt_.v((A_, h, A_)), start=False, stop=True)
                TT("dve", ST, ST, View(gam.t[:, :, c:c + 1].broadcast_to([64, 16, 64]), gam.b), ALU.mult)
                TT("dve", ST, ST, PSt, ALU.add)
                CP("act", Y_, PW)
                CP("dve", STb, ST)
                DMA("pool", YSC[c * 64:(c + 1) * 64, :], Y_[:].rearrange("p h n -> p (h n)"), reads=[Y_], writes=[dY[c]])
    if dbg == 2:
        allb = [d.b for d in dY]
        kb.finish("sp", allb)
        kb.finish("pool", allb)
        kb.close()
        es.close()
        return nc, in_names, dbg_outs

    X1 = dscr("x1s", [T, D], F32, tap=(dbg == 3))
    dX1 = [DR(f"x1_{t}") for t in range(NT)]
    GT = 8 if NT >= 8 else NT
    cs = lambda m: slice(m * 128, (m + 1) * 128)
    hs = lambda h: slice(h * 512, (h + 1) * 512)
    A_ = slice(None)

    class FFN2:
        def __init__(self, es_, tag, NTL):
            def sbx(name, shape, dt=F32):
                return T_(es_.enter_context(nc.sbuf_tensor(name + tag, list(shape), dt)), name)

            def psx(name, shape, dt=F32):
                return T_(es_.enter_context(nc.psum_tensor(name + tag, list(shape), dt)), name)
            self.tag, self.NTL = tag, NTL
            self.NB = NTL + 32
            NB = self.NB
            self.XA = dscr("xa" + tag, [NTL * 128, D], F32)
            self.HTOK = dscr("htok" + tag, [NTL * 128, D], BF16)
            self.HS = dscr("hs" + tag, [NB * 256, D], BF16)
            self.YS = dscr("ys" + tag, [NB * 256, D], F32)
            self.dXA = [DR("xa") for _ in range(NTL)]
            self.dHT = [DR("ht") for _ in range(NTL)]
            self.dHS = [DR("hs") for _ in range(2 * NTL)]
            self.dYS = [DR("ys") for _ in range(2 * NB)]
            self.xa = sbx("xa_sb", [128, D])
            self.xn = sbx("xn_f", [128, D])
            self.hTf = sbx("hTf", [128, 8, 128])
            self.htk = sbx("htk", [128, D], BF16)
            self.wr = sbx("wr", [128, 8, 36])
            self.sm = sbx("sm", [128, 96])
            self.OH = sbx("OH", [128, NTL, 2, 32], BF16)
            self.ohs = sbx("ohs", [128, 32], BF16)
            self.rk = sbx("rk", [128, NTL, 2])
            self.wk = sbx("wk", [128, NTL, 2])
            self.dstf = sbx("dstf", [128, NTL, 2])
            self.dsti = sbx("dsti", [128, NTL, 2], I32)
            self.base = sbx("base", [128, 32])
            self.t32 = [sbx(f"t32{i}", [128, 32]) for i in range(3)]
            self.Ls = sbx("Ls", [128, 128], BF16)
            self.bst = sbx("bst", [128, NB])
            self.blke = sbx("blke", [128, NB])
            self.widx = sbx("widx", [128, NB], I32)
            self.pcol = sbx("pcol", [128, 1])
            self.PX = psx("PX", [128, D])
            self.PSg = [psx(f"PSg{i}", [128, 512]) for i in range(2)]
            self.PSu = [psx(f"PSu{i}", [128, 512]) for i in range(2)]
            self.PSh = psx("PSh", [128, 8, 128], BF16)
            self.PSh2 = psx("PSh2", [128, 8, 128], BF16)
            self.PSr = self.PSg[0]
            self.lcur = None
            kb.op("pool", lambda e: e.memset(self.Ls[:], 1.0), writes=[self.Ls.b])
            kb.op("pool", lambda e: e.affine_select(out=self.Ls[:], in_=self.Ls[:], pattern=[[1, 128]], compare_op=ALU.is_gt, fill=0.0,
                                                    base=0, channel_multiplier=-1), reads=[self.Ls.b], writes=[self.Ls.b])
            kb.op("pool", lambda e: e.memset(self.base[:], 0.0), writes=[self.base.b])
            kb.op("pool", lambda e: e.iota(self.bst[:], pattern=[[256, NB]], base=0, channel_multiplier=0, allow_small_or_imprecise_dtypes=True),
                  writes=[self.bst.b])
            kb.op("pool", lambda e: e.iota(self.pcol[:], pattern=[[0, 1]], base=0, channel_multiplier=1, allow_small_or_imprecise_dtypes=True),
                  writes=[self.pcol.b])

        def prep(self, l, t):
            if self.lcur != l:
                DMA("sp", self.wr[:], moe_wr[l].rearrange("(m p) n -> p m n", p=128), writes=[self.wr])
                self.lcur = l
            xa = self.xa
            DMA("pool", self.XA[rowsl(t), :], xa[:], reads=[xa], writes=[self.dXA[t]])
            sm = self.sm
            c1 = lambda i, w=1: sm.v((A_, slice(i, i + w)))
            ACT(self.xn, xa, AF.Square, accum=c1(0))
            ACT(c1(1), c1(0), AF.Ln, bias=1024e-6)
            ACT(c1(1), c1(1), AF.Exp, scale=-0.5)
            TS("dve", self.xn, xa, c1(1), None, ALU.mult)
            for m in range(8):
                TR(self.PX.v((A_, cs(m))), self.xn.v((A_, cs(m))), ident)
            ai = 2 + 4 * l
            for m in range(8):
                ACT(self.hTf.v((A_, m, A_)), self.PX.v((A_, cs(m))), AF.Identity,
                    scale=AB.v((A_, ai, slice(m, m + 1))), bias=AB.v((A_, ai + 1, slice(m, m + 1))))
            for m in range(8):
                TR(self.PX.v((A_, cs(m))), self.hTf.v((A_, m, A_)), ident)
            CP("act", self.htk, self.PX)
            DMA("pool", self.HTOK[rowsl(t), :], self.htk[:], reads=[self.htk], writes=[self.dHT[t]])
            for m in range(8):
                MM(self.PSr.v((A_, slice(0, 36))), self.hTf.v((A_, m, A_)), self.wr.v((A_, m, A_)), start=(m == 0), stop=(m == 7))
            lg = c1(8, 36)
            CP("act", lg, self.PSr.v((A_, slice(0, 36))))
            g4 = c1(8, 4)
            RED("dve", c1(2), g4, op=ALU.max)
            TS("dve", c1(3), c1(2), -1.0, None, ALU.mult)
            ACT(c1(44, 4), g4, AF.Exp, bias=c1(3), accum=c1(4))
            kb.op("dve", lambda e: e.reciprocal(out=_ap(c1(5)), in_=_ap(c1(4))), reads=[sm.b], writes=[sm.b])
            TS("dve", c1(48, 4), g4, c1(2), None, ALU.is_equal)
            sel = c1(52, 8)
            TS("dve", sel, c1(12, 8), c1(48), None, ALU.mult)
            for g in range(1, 4):
                STT("dve", sel, c1(12 + 8 * g, 8), c1(48 + g), sel, ALU.mult, ALU.add)
            RED("dve", c1(6), sel, op=ALU.max)
            TS("dve", c1(60, 8), sel, c1(6), None, ALU.is_equal)
            STT("dve", c1(68, 8), c1(60, 8), -1e30, sel, ALU.mult, ALU.add)
            RED("dve", c1(7), c1(68, 8), op=ALU.max)
            TS("dve", c1(76, 8), c1(68, 8), c1(7), None, ALU.is_equal)
            TT("dve", c1(84), c1(7), c1(6), ALU.subtract)
            ACT(c1(85), c1(84), AF.Exp)
            TS("dve", c1(86), c1(85), 1.0, None, ALU.add)
            kb.op("dve", lambda e: e.reciprocal(out=_ap(c1(87)), in_=_ap(c1(86))), reads=[sm.b], writes=[sm.b])
            TT("dve", self.wk.v((A_, t, slice(0, 1))), c1(87), c1(5), ALU.mult)
            TT("dve", self.wk.v((A_, t, slice(1, 2))), self.wk.v((A_, t, slice(0, 1))), c1(85), ALU.mult)
            for k, o8 in ((0, 60), (1, 76)):
                for g in range(4):
                    TS("dve", self.OH.v((A_, t, k, slice(8 * g, 8 * g + 8))), c1(o8, 8), c1(48 + g), None, ALU.mult)
            TT("dve", self.ohs, self.OH.v((A_, t, 0, A_)), self.OH.v((A_, t, 1, A_)), ALU.add)
            MM(self.PSr.v((A_, slice(0, 32))), self.Ls, self.ohs)
            TT("dve", self.t32[0], self.PSr.v((A_, slice(0, 32))), self.base, ALU.add)
            for k in range(2):
                TT("dve", self.t32[1], self.OH.v((A_, t, k, A_)), self.t32[0], ALU.mult)
                RED("dve", self.rk.v((A_, t, slice(k, k + 1))), self.t32[1])
            MM(self.PSr.v((A_, slice(32, 64))), onesb, self.ohs)
            TT("dve", self.base, self.PSr.v((A_, slice(32, 64))), self.base, ALU.add)

        def run(self, l, gfb, emit):
            NTL, NB = self.NTL, self.NB
            t32 = self.t32
            with ExitStack() as esq:
                cq = T_(esq.enter_context(nc.sbuf_tensor("cq" + self.tag, [128, 32, NB], F32)), "cq")
                kb.op("dve", lambda e: e.tensor_tensor(out=cq[:], in0=self.bst[:].unsqueeze(1).broadcast_to([128, 32, NB]),
                                                       in1=self.base[:].unsqueeze(2).broadcast_to([128, 32, NB]), op=ALU.is_lt),
                      reads=[self.bst.b, self.base.b], writes=[cq.b])
                RED("dve", t32[0], cq)
            kb.barrier()
            TS("dve", t32[0], t32[0], 256.0, None, ALU.mult)
            CP("dve", t32[1], t32[0])
            a, b_ = t32[1], t32[2]
            for sh in (1, 2, 4, 8, 16):
                CP("dve", b_, a)
                TT("dve", b_.v((A_, slice(sh, 32))), a.v((A_, slice(sh, 32))), a.v((A_, slice(0, 32 - sh))), ALU.add)
                a, b_ = b_, a
            pend = a
            pstart = b_
            TT("dve", pstart, pend, t32[0], ALU.subtract)
            with ExitStack() as esr:
                def sbr(name, shape, dt=F32):
                    return T_(esr.enter_context(nc.sbuf_tensor(name + self.tag, list(shape), dt)), name)
                with ExitStack() as esc:
                    cmp_ = T_(esc.enter_context(nc.sbuf_tensor("cmp" + self.tag, [128, NB, 32], F32)), "cmp")
                    kb.op("dve", lambda e: e.tensor_tensor(out=cmp_[:], in0=pend[:].unsqueeze(1).broadcast_to([128, NB, 32]),
                                                           in1=self.bst[:].unsqueeze(2).broadcast_to([128, NB, 32]), op=ALU.is_le),
                          reads=[pend.b, self.bst.b], writes=[cmp_.b])
                    RED("dve", self.blke, cmp_)
                kb.barrier()
                TS("dve", self.blke, self.blke, 31.0, 128.0, ALU.min, ALU.mult)
                TS("dve", self.blke, self.blke, self.pcol, float(l * NEXP * 128), ALU.add, ALU.add)
                CP("dve", self.widx, self.blke)
                with ExitStack() as ess:
                    def sbs(name, shape, dt=F32):
                        return T_(ess.enter_context(nc.sbuf_tensor(name + self.tag, list(shape), dt)), name)
                    hrow = [sbs(f"hrow{i}", [128, D], BF16) for i in range(2)]
                    zt = sbs("zt", [128, D], BF16)
                    kb.op("pool", lambda e: e.memset(zt[:], 0.0), writes=[zt.b])
                    dz = [DR("hsz") for _ in range(4)]
                    ntz = NB * 2
                    bnd = [ntz * qq // 4 for qq in range(5)]
                    for qq in range(4):
                        DMA("sp", self.HS[bnd[qq] * 128:bnd[qq + 1] * 128, :].rearrange("(c p) d -> p c d", p=128),
                            zt[:].unsqueeze(1).broadcast_to([128, bnd[qq + 1] - bnd[qq], D]), reads=[zt], writes=[dz[qq]])
                    zb_ = [d.b for d in dz]
                    for t in range(NTL):
                        for k in range(2):
                            TT("dve", t32[0], self.OH.v((A_, t, k, A_)), pstart, ALU.mult)
                            RED("dve", self.dstf.v((A_, t, slice(k, k + 1))), t32[0])
                        TT("dve", self.dstf.v((A_, t, A_)), self.dstf.v((A_, t, A_)), self.rk.v((A_, t, A_)), ALU.add)
                        CP("dve", self.dsti.v((A_, t, A_)), self.dstf.v((A_, t, A_)))
                        hr = hrow[t % 2]
                        DMA("sp", hr[:], self.HTOK[rowsl(t), :], reads=[self.dHT[t]], writes=[hr])
                        for k in range(2):
                            self._ind("scatter", self.HS, self.dsti.t[:, t, k:k + 1], hr, reads=[hr.b, self.dsti.b] + zb_, writes=[self.dHS[2 * t + k].b])
                kb.barrier()
                stg = [[sbr(f"stg{i}_{j}", [128, 4096]) for j in range(3)] for i in range(2)]
                Wb = [[sbr(f"Wb{i}_{j}", [128, 4096], BF16) for j in range(3)] for i in range(2)]
                hsb = [sbr(f"hsb{i}", [128, D], BF16) for i in range(2)]
                hT = [sbr(f"hTx{i}", [128, 8, 128], BF16) for i in range(2)]
                sg = sbr("sg", [128, 512])
                hb = [sbr(f"hb{i}", [128, 512], BF16) for i in range(2)]
                hT2 = [sbr(f"hT2{i}", [128, 4, 128], BF16) for i in range(2)]
                hfl = T_(_APWrap(self.hTf.t[:].rearrange("p m n -> p (m n)")), "hTf_flatr")
                hfl.b = self.hTf.b
                ysb = [self.xn, hfl]
                if os.environ.get("SBDBG"):
                    print("SBUF free in run", self.tag, nc.sbuf_bytes_remaining)
                allhs = [d.b for d in self.dHS]
                wsrc = [w_.rearrange("l r f -> (l r) f") for w_ in (moe_wg, moe_wu, moe_wd)]

                def wgather(blk):
                    for j in range(3):
                        st = stg[blk % 2][j]
                        self._ind("gather", wsrc[j], self.widx.t[:, blk:blk + 1], st, reads=[self.widx.b], writes=[st.b])

                def wcast(blk):
                    for j in range(3):
                        st = stg[blk % 2][j]
                        for q4 in range(4):
                            sl = slice(q4 * 1024, (q4 + 1) * 1024)
                            CP("act" if (j * 4 + q4) % 2 == 0 else "dve", Wb[blk % 2][j].v((A_, sl)), st.v((A_, sl)))

                def front(u):
                    blk, sub = divmod(u, 2)
                    j = u % 2
                    r0 = blk * 256 + sub * 128
                    Wg_ = View(Wb[blk % 2][0].t[:].rearrange("p (m f) -> p m f", f=512), Wb[blk % 2][0].b)
                    Wu_ = View(Wb[blk % 2][1].t[:].rearrange("p (m f) -> p m f", f=512), Wb[blk % 2][1].b)
                    kb.dma("sp", hsb[j][:], self.HS[r0:r0 + 128, :], reads=allhs, writes=[hsb[j].b])
                    for m in range(8):
                        TR(self.PSh.v((A_, m, A_)), hsb[j].v((A_, cs(m))), identb)
                    CP("act", hT[j], self.PSh)
                    for mk in range(8):
                        MM(self.PSg[j], hT[j].v((A_, mk, A_)), View(Wg_.ap[:, mk, :], Wg_.b), start=(mk == 0), stop=(mk == 7))
                    for mk in range(8):
                        MM(self.PSu[j], hT[j].v((A_, mk, A_)), View(Wu_.ap[:, mk, :], Wu_.b), start=(mk == 0), stop=(mk == 7))
                    ACT(sg, self.PSg[j], AF.Silu)
                    TT("dve", hb[j], self.PSu[j], sg, ALU.mult)

                def back(u):
                    blk, sub = divmod(u, 2)
                    j = u % 2
                    r0 = blk * 256 + sub * 128
                    Wd_ = View(Wb[blk % 2][2].t[:].rearrange("p (m f) -> p m f", f=D), Wb[blk % 2][2].b)
                    for fk in range(4):
                        TR(self.PSh2.v((A_, fk, A_)), hb[j].v((A_, cs(fk))), identb)
                    CP("act", hT2[j], self.PSh2.v((A_, slice(0, 4), A_)))
                    for h in range(2):
                        for fk in range(4):
                            MM(self.PX.v((A_, hs(h))), hT2[j].v((A_, fk, A_)), View(Wd_.ap[:, fk, hs(h)], Wd_.b), start=(fk == 0), stop=(fk == 3))
                    CP("dve", ysb[j], self.PX)
                    DMA("sp", self.YS[r0:r0 + 128, :], ysb[j][:], reads=[ysb[j]], writes=[self.dYS[u]])
                wgather(0)
                if NB > 1:
                    wgather(1)
                wcast(0)
                front(0)
                for u in range(2 * NB):
                    blk, sub = divmod(u, 2)
                    if sub == 0 and blk + 1 < NB:
                        wcast(blk + 1)
                    if sub == 1 and blk + 2 < NB:
                        wgather(blk + 2)
                    if u + 1 < 2 * NB:
                        front(u + 1)
                    back(u)
                allys = [d.b for d in self.dYS]
                for t in range(NTL):
                    y1, y2 = stg[0][0], stg[0][1]
                    y1v, y2v = y1.v((A_, slice(0, D))), y2.v((A_, slice(0, D)))
                    self._ind("gather", self.YS, self.dsti.t[:, t, 0:1], y1v, reads=allys + [self.dsti.b], writes=[y1.b])
                    self._ind("gather", self.YS, self.dsti.t[:, t, 1:2], y2v, reads=allys + [self.dsti.b], writes=[y2.b])
                    DMA("sp", self.xa[:], self.XA[rowsl(t), :], reads=[self.dXA[t]], writes=[self.xa])
                    TS("dve", y1v, y1v, self.wk.v((A_, t, slice(0, 1))), None, ALU.mult)
                    STT("dve", y1v, y2v, self.wk.v((A_, t, slice(1, 2))), y1v, ALU.mult, ALU.add)
                    TT("dve", y1v, y1v, gfb, ALU.mult)
                    TT("dve", self.xa, self.xa, y1v, ALU.add)
                    emit(t, self.xa)

        def _ind(self, kind, dram, idx_ap, sb_, reads, writes):
            q = "pool"
            keys, i = kb.dma_pool[q]
            key = keys[i % len(keys)]
            kb.dma_pool[q][1] = i + 1
            if kb.cnt[key] > 0:
                kb._wait(q, (key, kb.cnt[key]))
            for b in reads:
                kb._wait(q, b.w)
            for b in writes:
                kb._wait(q, b.w)
                for k_, v_ in b.r.items():
                    kb._wait(q, (k_, v_))
            off = bass.IndirectOffsetOnAxis(ap=idx_ap, axis=0)
            if kind == "gather":
                inst = nc.gpsimd.indirect_dma_start(out=_ap(sb_), out_offset=None, in_=dram, in_offset=off)
            else:
                inst = nc.gpsimd.indirect_dma_start(out=dram, out_offset=off, in_=_ap(sb_), in_offset=None)
            kb.cnt[key] += 16
            inst.then_inc(kb.sems[key], 16)
            kb._mark((key, kb.cnt[key]), reads, writes)
            kb.n_inst += 1


    class FFN:
        def __init__(self, es_, tag=""):
            def sbx(name, shape, dt=F32):
                return T_(es_.enter_context(nc.sbuf_tensor(name + tag, list(shape), dt)), name)

            def psx(name, shape, dt=F32):
                return T_(es_.enter_context(nc.psum_tensor(name + tag, list(shape), dt)), name)
            self.acc = sbx("acc", [128, GT, D])
            self.hTb = sbx("hTb", [128, GT, 8, 128], BF16)
            self.gw = sbx("gw", [128, GT, 32])
            self.hTf = sbx("hTf", [128, 8, 128])
            self.xn = sbx("xn_f", [128, D])
            self.wr = sbx("wr", [128, 8, 36])
            self.sm = sbx("sm", [128, 96])
            self.stg = [sbx(f"stg{i}", [128, 4096]) for i in range(2)]
            self.Wg = [sbx(f"Wgb{i}", [128, 8, 512], BF16) for i in range(2)]
            self.Wu = [sbx(f"Wub{i}", [128, 8, 512], BF16) for i in range(2)]
            self.Wd = [sbx(f"Wdb{i}", [128, 4, D], BF16) for i in range(2)]
            self.sg = [sbx("sg0", [128, 512])] * 2
            self.hb = [sbx(f"hb{i}", [128, 512], BF16) for i in range(2)]
            self.hT2 = [sbx(f"hT2{i}", [128, 4, 128], BF16) for i in range(2)]
            self.PX = psx("PX", [128, D])
            self.PSg = [psx(f"PSg{i}", [128, 512]) for i in range(2)]
            self.PSu = [psx(f"PSu{i}", [128, 512]) for i in range(2)]
            self.PSh = psx("PSh", [128, 8, 128], BF16)
            self.PSr = psx("PSr", [128, 64])
            self.lcur = None

        def prep(self, l, t):
            if self.lcur != l:
                DMA("sp", self.wr[:], moe_wr[l].rearrange("(m p) n -> p m n", p=128), writes=[self.wr])
                self.lcur = l
            xa = self.acc.v((A_, t, A_))
            sm = self.sm
            c1 = lambda i, w=1: sm.v((A_, slice(i, i + w)))
            ACT(self.xn, xa, AF.Square, accum=c1(0))
            ACT(c1(1), c1(0), AF.Ln, bias=1024e-6)
            ACT(c1(1), c1(1), AF.Exp, scale=-0.5)
            TS("dve", self.xn, xa, c1(1), None, ALU.mult)
            for m in range(8):
                TR(self.PX.v((A_, cs(m))), self.xn.v((A_, cs(m))), ident)
            ai = 2 + 4 * l
            for m in range(8):
                ACT(self.hTf.v((A_, m, A_)), self.PX.v((A_, cs(m))), AF.Identity,
                    scale=AB.v((A_, ai, slice(m, m + 1))), bias=AB.v((A_, ai + 1, slice(m, m + 1))))
            CP("pool", self.hTb.v((A_, t, A_, A_)), self.hTf)
            for m in range(8):
                MM(self.PSr.v((A_, slice(0, 36))), self.hTf.v((A_, m, A_)), self.wr.v((A_, m, A_)), start=(m == 0), stop=(m == 7))
            lg = c1(8, 36)
            CP("act", lg, self.PSr.v((A_, slice(0, 36))))
            g4 = c1(8, 4)
            RED("dve", c1(2), g4, op=ALU.max)
            TS("dve", c1(3), c1(2), -1.0, None, ALU.mult)
            ACT(c1(44, 4), g4, AF.Exp, bias=c1(3), accum=c1(4))
            kb.op("dve", lambda e: e.reciprocal(out=_ap(c1(5)), in_=_ap(c1(4))), reads=[sm.b], writes=[sm.b])
            TS("dve", c1(48, 4), g4, c1(2), None, ALU.is_equal)
            sel = c1(52, 8)
            TS("dve", sel, c1(12, 8), c1(48), None, ALU.mult)
            for g in range(1, 4):
                STT("dve", sel, c1(12 + 8 * g, 8), c1(48 + g), sel, ALU.mult, ALU.add)
            RED("dve", c1(6), sel, op=ALU.max)
            TS("dve", c1(60, 8), sel, c1(6), None, ALU.is_equal)
            STT("dve", c1(68, 8), c1(60, 8), -1e30, sel, ALU.mult, ALU.add)
            RED("dve", c1(7), c1(68, 8), op=ALU.max)
            TS("dve", c1(76, 8), c1(68, 8), c1(7), None, ALU.is_equal)
            TT("dve", c1(84), c1(7), c1(6), ALU.subtract)
            ACT(c1(85), c1(84), AF.Exp)
            TS("dve", c1(86), c1(85), 1.0, None, ALU.add)
            kb.op("dve", lambda e: e.reciprocal(out=_ap(c1(87)), in_=_ap(c1(86))), reads=[sm.b], writes=[sm.b])
            TT("dve", c1(87), c1(87), c1(5), ALU.mult)
            TT("dve", c1(88), c1(87), c1(85), ALU.mult)
            TS("dve", c1(60, 8), c1(60, 8), c1(87), None, ALU.mult)
            STT("dve", c1(60, 8), c1(76, 8), c1(88), c1(60, 8), ALU.mult, ALU.add)
            for g in range(4):
                TS("dve", self.gw.v((A_, t, slice(8 * g, 8 * g + 8))), c1(60, 8), c1(48 + g), None, ALU.mult)

        def experts(self, l, ntile, gfb):
            engs = ["act", "dve", "pool"]
            n = 0
            for e in range(NEXP):
                i = e % 2
                for (dst, src) in ((self.Wg[i], moe_wg), (self.Wu[i], moe_wu)):
                    st = self.stg[n % 2]
                    DMA("sp" if n % 2 == 0 else "pool", st[:].rearrange("p (m f) -> p m f", f=512), src[l, e].rearrange("(m p) f -> p m f", p=128), writes=[st])
                    CP(engs[n % 3], dst, View(st.t[:].rearrange("p (m f) -> p m f", f=512), st.b))
                    n += 1
                st = self.stg[n % 2]
                DMA("sp" if n % 2 == 0 else "pool", st[:].rearrange("p (m f) -> p m f", f=D), moe_wd[l, e].rearrange("(m p) f -> p m f", p=128), writes=[st])
                for fk in range(4):
                    TT("pool" if fk % 2 else "dve", self.Wd[i].v((A_, fk, A_)), st.v((A_, slice(fk * D, (fk + 1) * D))), gfb, ALU.mult)
                n += 1
                for t in range(ntile):
                    j = t % 2
                    for mk in range(8):
                        MM(self.PSg[j], self.hTb.v((A_, t, mk, A_)), self.Wg[i].v((A_, mk, A_)), start=(mk == 0), stop=(mk == 7))
                    for mk in range(8):
                        MM(self.PSu[j], self.hTb.v((A_, t, mk, A_)), self.Wu[i].v((A_, mk, A_)), start=(mk == 0), stop=(mk == 7))
                    ACT(self.sg[j], self.PSg[j], AF.Silu)
                    STT("dve", self.hb[j], self.PSu[j], self.gw.v((A_, t, slice(e, e + 1))), self.sg[j], ALU.mult, ALU.mult)
                    for fk in range(4):
                        TR(self.PSh.v((A_, fk, A_)), self.hb[j].v((A_, cs(fk))), identb)
                    CP("act", self.hT2[j], self.PSh.v((A_, slice(0, 4), A_)))
                    for h in range(2):
                        for fk in range(4):
                            MM(self.PX.v((A_, hs(h))), self.hT2[j].v((A_, fk, A_)), self.Wd[i].v((A_, fk, hs(h))), start=(fk == 0), stop=(fk == 3))
                    TT("dve", self.acc.v((A_, t, A_)), self.acc.v((A_, t, A_)), self.PX, ALU.add)

    kb.barrier()
    if has("p3"):
        with ExitStack() as es3:
            ffn = FFN2(es3, "_l0", NT)
            with ExitStack() as es3a:
                def sb3(name, shape, dt=F32):
                    return T_(es3a.enter_context(nc.sbuf_tensor(name, list(shape), dt)), name)
                Wo = sb3("Wo", [128, 8, D], BF16)
                gng, gnb = sb3("gng", [128, D]), sb3("gnb", [128, D])
                G3 = [sb3(f"G3{i}", [128, D]) for i in range(3)] + [T_(_APWrap(ffn.hTf.t[:].rearrange("p m n -> p (m n)")), "hTf_flat"), ffn.xn]
                G3[3].b = ffn.hTf.b
                zb = sb3("zb", [128, D], BF16)
                zT = sb3("zT", [128, 8, 128], BF16)
                st16 = sb3("st16", [128, 48])
                PZ = ffn.PSh
                vh = lambda t: View(t.t[:].rearrange("p (h n) -> p h n", n=64), t.b)
                bch = lambda v_: View(v_.ap.unsqueeze(2).broadcast_to([128, 16, 64]), v_.b)
                s16 = lambda i: st16.v((A_, slice(16 * i, 16 * i + 16)))
                for m in range(8):
                    st = G3[m % 2]
                    DMA("sp", st[:], rw_wo[cs(m), :], writes=[st])
                    CP("act" if m % 2 else "dve", Wo.v((A_, m, A_)), st)
                bc_row(gng, vec_row[VR_GNG:VR_GNG + 1, :])
                bc_row(gnb, vec_row[VR_GNB:VR_GNB + 1, :])
                for tt in range(NT):
                    yv, gv, bv, xv, wk = G3
                    ydeps = [dY[2 * tt], dY[2 * tt + 1]] if has("p2") else []
                    DMA("sp", yv[:], YSC[rowsl(tt), :], reads=ydeps, writes=[yv])
                    DMA("sp", gv[:], GSC[rowsl(tt), :], reads=[dG[tt]], writes=[gv])
                    DMA("sp", bv[:], BON[rowsl(tt), :], reads=[dBON[tt]], writes=[bv])
                    DMA("sp", xv[:], x_in[rowsl(tt), :], writes=[xv])
                    RED("dve", s16(0), vh(yv))
                    TS("dve", s16(0), s16(0), -1.0 / 64, None, ALU.mult)
                    TT("dve", vh(yv), vh(yv), bch(s16(0)), ALU.add)
                    TT("pool", wk, yv, yv, ALU.mult)
                    RED("dve", s16(1), vh(wk))
                    ACT(s16(1), s16(1), AF.Ln, scale=1.0 / 64, bias=64e-5)
                    ACT(s16(1), s16(1), AF.Exp, scale=-0.5)
                    TT("dve", vh(yv), vh(yv), bch(s16(1)), ALU.mult)
                    TT("pool", yv, yv, gng, ALU.mult)
                    TT("pool", yv, yv, gnb, ALU.add)
                    TT("dve", yv, yv, bv, ALU.add)
                    TT("dve", zb, yv, gv, ALU.mult)
                    for m in range(8):
                        TR(PZ.v((A_, m, A_)), zb.v((A_, cs(m))), identb)
                    CP("act", zT, PZ)
                    for h in range(2):
                        for mk in range(8):
                            MM(ffn.PX.v((A_, hs(h))), zT.v((A_, mk, A_)), Wo.v((A_, mk, hs(h))), start=(mk == 0), stop=(mk == 7))
                    TT("dve", wk, ffn.PX, gmb[0], ALU.mult)
                    TT("dve", ffn.xa, wk, xv, ALU.add)
                    ffn.prep(0, tt)
            kb.barrier()

            def emit0(t, xa_):
                DMA("sp", X1[rowsl(t), :], xa_[:], reads=[xa_], writes=[dX1[t]])
            ffn.run(0, gmb[1], emit0)
    if dbg == 3:
        allb = [d.b for d in dX1]
        kb.finish("sp", allb)
        kb.finish("pool", allb)
        kb.close()
        es.close()
        return nc, in_names, dbg_outs

    NO = NT // 2
    KTS = dscr("kts", [16, 64, T], BF16)
    VS = dscr("vs", [T, D], BF16)
    QTS = dscr("qts", [16, 64, NO * 128], BF16)
    XO = dscr("xo", [NO * 128, D], F32)
    OS = dscr("os", [NO * 128, D], BF16)
    dKT = [DR(f"kt{t}") for t in range(NT)]
    dVS = [DR(f"vs{t}") for t in range(NT)]
    dQT = [DR(f"qt{t}") for t in range(NO)]
    dXO = [DR(f"xo{t}") for t in range(NO)]
    dOS = [DR(f"os{t}") for t in range(NO)]
    dOUT = [DR(f"out{t}") for t in range(NO)]
    LAM_INIT = 0.8 - 0.6 * math.exp(-0.3 * 1)
    kb.barrier()
    if has("p5"):
        with ExitStack() as es4:
            def sb4(name, shape, dt=F32):
                return T_(es4.enter_context(nc.sbuf_tensor(name, list(shape), dt)), name)

            def ps4(name, shape, dt=F32):
                return T_(es4.enter_context(nc.psum_tensor(name, list(shape), dt)), name)
            Wkv = sb4("Wkv", [128, 8, 2 * D], BF16)
            Wq = sb4("Wq", [128, 8, D], BF16)
            stg = [sb4(f"stg4{i}", [128, 2 * D]) for i in range(2)]
            xt4 = [sb4(f"xt4{i}", [128, D]) for i in range(3)]
            hk = sb4("hk", [128, 8, 128], BF16)
            kts = sb4("kts_sb", [64, 16, 128], BF16)
            vsb = sb4("vsb", [128, D], BF16)
            sm4 = sb4("sm4", [128, 32])
            selc = sb4("selc", [128, 4])
            cmk = sb4("cmk", [128, 2, 128], BF16)
            cmf = sb4("cmf", [128, 2, 128])
            lamt = sb4("lamt", [128, 8])
            subg = sb4("subg", [128, 128])
            KTh = sb4("KTh", [128, 2, T], BF16)
            Vh = sb4("Vh", [128, NT, 129], BF16)
            QTh = sb4("QTh", [128, 2, NO * 128], BF16)
            mxs = [sb4(f"mx{i}", [128, 16]) for i in range(4)]
            negm65 = sb4("negm65", [128, 4, 65])
            rowbuf = [Buf(f"row{i}") for i in range(4)]
            osq = sb4("osq", [128, 128], BF16)
            pk_slot = [Buf("pk0"), Buf("pk1")]
            psn_slot = [Buf("psn0"), Buf("psn1")]
            pst_slot = [Buf(f"pst{i}") for i in range(4)]
            Pt = [sb4(f"Pt{i}", [128, 2, 128], BF16) for i in range(3)]
            ot = sb4("ot", [128, 128])
            ob = sb4("ob", [128, 128], BF16)
            PXa = ps4("PXa", [128, D])
            PKs = ps4("PKs", [128, D])
            PSt = ps4("PSt4", [128, 4, 128])
            PSo = [ps4(f"PSo{i}", [128, 512]) for i in range(2)]
            PSn = ps4("PSn", [128, 512])
            c4 = lambda i, w=1: sm4.v((A_, slice(i, i + w)))
            PK = View(PKs.t[0:64, :].rearrange("p (s t) -> p s t", t=128), PKs.b)

            n = 0
            for m in range(8):
                st = stg[n % 2]
                DMA("sp" if n % 2 == 0 else "pool", st[:], w_kv[cs(m), :], writes=[st])
                CP(["act", "dve", "pool"][n % 3], Wkv.v((A_, m, A_)), st)
                n += 1
            for m in range(8):
                st = stg[n % 2]
                DMA("sp" if n % 2 == 0 else "pool", st[:, 0:D], df_wq[cs(m), :], writes=[st])
                CP(["act", "dve", "pool"][n % 3], Wq.v((A_, m, A_)), st.v((A_, slice(0, D))))
                n += 1
            DMA("sp", selc[:], sel_in[:, :], writes=[selc])
            DMA("sp", cmf[:], cmask_in.rearrange("c k q -> k c q"), writes=[cmf])
            CP("dve", cmk, cmf)
            DMA("sp", lamt[:, 0:4], lam_in[:, :], writes=[lamt])
            bc_row(subg, vec_row[VR_SUBLN:VR_SUBLN + 1, 0:128])
            TS("dve", subg, subg, 1.0 - LAM_INIT, None, ALU.mult)
            TT("dve", lamt.v((A_, slice(4, 5))), lamt.v((A_, slice(0, 1))), lamt.v((A_, slice(1, 2))), ALU.mult)
            TT("dve", lamt.v((A_, slice(5, 6))), lamt.v((A_, slice(2, 3))), lamt.v((A_, slice(3, 4))), ALU.mult)
            MM(PSn.v((A_, slice(0, 2))), ones, lamt.v((A_, slice(4, 6))))
            ACT(lamt.v((A_, slice(6, 8))), PSn.v((A_, slice(0, 2))), AF.Exp)
            TT("dve", lamt.v((A_, slice(4, 5))), lamt.v((A_, slice(7, 8))), lamt.v((A_, slice(6, 7))), ALU.subtract)
            TS("dve", lamt.v((A_, slice(4, 5))), lamt.v((A_, slice(4, 5))), -LAM_INIT, None, ALU.add)
            neglam = lamt.v((A_, slice(4, 5)))

            def norm_T(xv, ai, dst):
                ACT(xt4[2], xv, AF.Square, accum=c4(0))
                ACT(c4(1), c4(0), AF.Ln, bias=1024e-6)
                ACT(c4(1), c4(1), AF.Exp, scale=-0.5)
                TS("dve", xt4[2], xv, c4(1), None, ALU.mult)
                for m in range(8):
                    TR(PXa.v((A_, cs(m))), xt4[2].v((A_, cs(m))), ident)
                for m in range(8):
                    ACT(dst.v((A_, m, A_)), PXa.v((A_, cs(m))), AF.Identity,
                        scale=AB.v((A_, ai, slice(m, m + 1))), bias=AB.v((A_, ai + 1, slice(m, m + 1))))

            def proj64(W, coff, scale, dram, col0):
                for half in range(2):
                    for s8 in range(8):
                        sl = half * 8 + s8
                        for mk in range(8):
                            MM(View(PK.ap[:, s8, :], PK.b), W.v((A_, mk, slice(coff + sl * 64, coff + sl * 64 + 64))), hk.v((A_, mk, A_)),
                               start=(mk == 0), stop=(mk == 7))
                    ACT(kts.v((A_, slice(half * 8, half * 8 + 8), A_)), PK, AF.Copy, scale=scale)

            for tt in range(NT):
                DMA("sp", xt4[0][:], X1[rowsl(tt), :], reads=[dX1[tt]], writes=[xt4[0]])
                norm_T(xt4[0], 8, hk)
                proj64(Wkv, 0, 1.0, KTS, tt * 128)
                DMA("pool", KTS[:, :, tt * 128:(tt + 1) * 128].rearrange("s d t -> d s t"), kts[:], reads=[kts], writes=[dKT[tt]])
                for h in range(2):
                    for mk in range(8):
                        MM(PXa.v((A_, hs(h))), hk.v((A_, mk, A_)), Wkv.v((A_, mk, slice(D + h * 512, D + (h + 1) * 512))), start=(mk == 0), stop=(mk == 7))
                CP("dve", vsb, PXa)
                DMA("pool", VS[rowsl(tt), :], vsb[:], reads=[vsb], writes=[dVS[tt]])
            for i in range(NO):
                DMA("sp", xt4[0][:], X1[rowsl(2 * i), :], reads=[dX1[2 * i]], writes=[xt4[0]])
                DMA("sp", xt4[1][:], X1[rowsl(2 * i + 1), :], reads=[dX1[2 * i + 1]], writes=[xt4[1]])
                TS("dve", xt4[0], xt4[0], selc.v((A_, slice(0, 1))), None, ALU.mult)
                STT("dve", xt4[0], xt4[1], selc.v((A_, slice(1, 2))), xt4[0], ALU.mult, ALU.add)
                DMA("pool", XO[rowsl(i), :], xt4[0][:], reads=[xt4[0]], writes=[dXO[i]])
                norm_T(xt4[0], 4, hk)
                proj64(Wq, 0, 0.125, QTS, i * 128)
                DMA("pool", QTS[:, :, i * 128:(i + 1) * 128].rearrange("s d t -> d s t"), kts[:], reads=[kts], writes=[dQT[i]])
            kb.barrier()
            kb.op("pool", lambda e: e.memset(Vh[:, :, 128:129], 1.0), writes=[Vh.b])
            kb.op("pool", lambda e: e.memset(QTh[:], 0.0), writes=[QTh.b])
            kb.op("pool", lambda e: e.memset(KTh[:], 0.0), writes=[KTh.b])
            kb.op("pool", lambda e: e.memset(KTh[64:65, :, :], 1.0), writes=[KTh.b])
            kb.op("pool", lambda e: e.memset(negm65[:], 0.0), writes=[negm65.b])
            for hd in range(8):
                for c in range(2):
                    DMA("sp", KTh[0:64, c, :], KTS[2 * hd + c], reads=dKT, writes=[KTh])
                    DMA("sp", QTh[0:64, c, :], QTS[2 * hd + c], reads=dQT, writes=[QTh])
                DMA("sp", Vh[:, :, 0:128], VS[:, hd * 128:(hd + 1) * 128].rearrange("(n p) e -> p n e", p=128), reads=dVS, writes=[Vh])
                pst_views = [View(PSt.t[:, 0:2, :], pst_slot[0]), View(PXa.t[:, 0:256].rearrange("p (c q) -> p c q", c=2), pst_slot[1]),
                             View(PXa.t[:, 512:768].rearrange("p (c q) -> p c q", c=2), pst_slot[2])]

                def pass1(i):
                    par = i % 2
                    nk = 2 * i + 2
                    qs = slice(i * 128, (i + 1) * 128)
                    nch = (nk * 128 + 511) // 512
                    for c in range(2):
                        mxt = mxs[2 * par + c]
                        for kc in range(nch):
                            w = min(512, nk * 128 - kc * 512)
                            pss = View(PKs.t[:, (kc % 2) * 512:(kc % 2) * 512 + w], pk_slot[kc % 2])
                            MM(pss, QTh.v((slice(0, 64), c, qs)), KTh.v((slice(0, 64), c, slice(kc * 512, kc * 512 + w))))
                            RED("dve", mxt.v((A_, slice(kc, kc + 1))), pss, op=ALU.max)
                        a = 8 + 4 * par + 2 * c
                        RED("dve", c4(a), mxt.v((A_, slice(0, nch))), op=ALU.max)
                        TS("dve", negm65.v((A_, 2 * par + c, slice(64, 65))), c4(a), -1.0, None, ALU.mult)
                        psn = View(PSn.t[0:65, 0:128], PSn.b)
                        TR(psn, negm65.v((A_, 2 * par + c, A_)), ident)
                        rb = rowbuf[2 * par + c]
                        kb.op("act", lambda en, c=c, qs=qs: en.copy(out=QTh[64:65, c, qs], in_=PSn[64:65, 0:128]), reads=[PSn.b], writes=[rb])

                def pass2(i):
                    par = i % 2
                    nk = 2 * i + 2
                    qs = slice(i * 128, (i + 1) * 128)

                    def score(kt):
                        pst = pst_views[kt % 3]
                        for c in range(2):
                            kb.op("pe", lambda en, c=c, kt=kt, pst=pst: en.matmul(pst.ap[:, c, :], lhsT=KTh[:, c, kt * 128:(kt + 1) * 128],
                                                                               rhs=QTh[:, c, qs], start=True, stop=True),
                                  reads=[KTh.b, QTh.b, rowbuf[2 * par + c]], writes=[pst.b])
                    for kt in range(min(2, nk)):
                        score(kt)
                    for kt in range(nk):
                        if kt + 2 < nk:
                            score(kt + 2)
                        p_ = Pt[kt % 3]
                        ACT(p_, pst_views[kt % 3], AF.Exp)
                        if kt >= nk - 2:
                            kb.op("dve", lambda e, p_=p_, kt=kt: e.tensor_tensor(
                                out=p_[:], in0=p_[:], in1=cmk[:, kt - (nk - 2):kt - (nk - 2) + 1, :].broadcast_to([128, 2, 128]), op=ALU.mult),
                                reads=[p_.b, cmk.b], writes=[p_.b])
                        for c in range(2):
                            MM(PSo[c].v((A_, slice(0, 129))), p_.v((A_, c, A_)), Vh.v((A_, kt, A_)), start=(kt == 0), stop=(kt == nk - 1))
                    kb.op("dve", lambda e: e.reciprocal(out=_ap(c4(4)), in_=PSo[0][:, 128:129]), reads=[PSo[0].b], writes=[sm4.b])
                    kb.op("dve", lambda e: e.reciprocal(out=_ap(c4(5)), in_=PSo[1][:, 128:129]), reads=[PSo[1].b], writes=[sm4.b])
                    TT("dve", c4(5), c4(5), neglam, ALU.mult)
                    TS("dve", ot, PSo[0].v((A_, slice(0, 128))), c4(4), None, ALU.mult)
                    STT("dve", ot, PSo[1].v((A_, slice(0, 128))), c4(5), ot, ALU.mult, ALU.add)
                    ACT(osq, ot, AF.Square, accum=c4(6))
                    ACT(c4(7), c4(6), AF.Ln, scale=1.0 / 128, bias=1e-5)
                    ACT(c4(7), c4(7), AF.Exp, scale=-0.5)
                    STT("dve", ob, ot, c4(7), subg, ALU.mult, ALU.mult)
                    DMA("pool", OS[rowsl(i), hd * 128:(hd + 1) * 128], ob[:], reads=[ob], writes=[dOS[i]])
                pass1(0)
                for i in range(NO):
                    if i + 1 < NO:
                        pass1(i + 1)
                    pass2(i)
        kb.barrier()
        with ExitStack() as es5:
            def sb5(name, shape, dt=F32):
                return T_(es5.enter_context(nc.sbuf_tensor(name, list(shape), dt)), name)
            ffn = FFN2(es5, "_l1", NO)
            fg = sb5("fg", [128, D])
            s5 = sb5("s5", [128, 4])
            hflat = T_(_APWrap(ffn.hTf.t[:].rearrange("p m n -> p (m n)")), "hTf_flat5")
            hflat.b = ffn.hTf.b
            bc_row(fg, vec_row[VR_FING:VR_FING + 1, :])
            TS("dve", fg, fg, 32.0, None, ALU.mult)
            with ExitStack() as es5a:
                def sb5a(name, shape, dt=F32):
                    return T_(es5a.enter_context(nc.sbuf_tensor(name, list(shape), dt)), name)
                Wo1 = sb5a("Wo1", [128, 8, D], BF16)
                x5 = [sb5a(f"x5{i}", [128, D]) for i in range(2)]
                osb = sb5a("osb", [128, D], BF16)
                oT = sb5a("oT", [128, 8, 128], BF16)
                for m in range(8):
                    st = x5[m % 2]
                    DMA("sp", st[:], df_wo[cs(m), :], writes=[st])
                    CP("act" if m % 2 else "dve", Wo1.v((A_, m, A_)), st)
                for i in range(NO):
                    DMA("sp", osb[:], OS[rowsl(i), :], reads=[dOS[i]], writes=[osb])
                    DMA("sp", x5[0][:], XO[rowsl(i), :], reads=[dXO[i]], writes=[x5[0]])
                    for m in range(8):
                        TR(ffn.PSh.v((A_, m, A_)), osb.v((A_, cs(m))), identb)
                    CP("act", oT, ffn.PSh)
                    for h in range(2):
                        for mk in range(8):
                            MM(ffn.PX.v((A_, hs(h))), oT.v((A_, mk, A_)), Wo1.v((A_, mk, hs(h))), start=(mk == 0), stop=(mk == 7))
                    TT("dve", x5[1], ffn.PX, gmb[2], ALU.mult)
                    TT("dve", ffn.xa, x5[1], x5[0], ALU.add)
                    ffn.prep(1, i)
            kb.barrier()

            def emit1(i, xa_):
                ACT(ffn.xn, xa_, AF.Square, accum=s5.v((A_, slice(0, 1))))
                ACT(s5.v((A_, slice(1, 2))), s5.v((A_, slice(0, 1))), AF.Ln, bias=1024e-6)
                ACT(s5.v((A_, slice(1, 2))), s5.v((A_, slice(1, 2))), AF.Exp, scale=-0.5)
                STT("dve", hflat, xa_, s5.v((A_, slice(1, 2))), fg, ALU.mult, ALU.mult)
                DMA("sp", out[rowsl(i), :], hflat[:], reads=[hflat], writes=[dOUT[i]])
            ffn.run(1, gmb[3], emit1)
    allb = [d.b for d in dOUT]
    kb.finish("sp", allb)
    kb.finish("pool", allb)
    kb.close()
    es.close()
    return nc, in_names, dbg_outs


def _col(v):
    return np.ascontiguousarray(np.asarray(v, np.float32).reshape(8, 128).T)


def make_in_maps(inputs, T=None, n_cores=8):
    p = {k: np.asarray(v) for k, v in inputs.items()}
    Tfull = p["x"].shape[1]
    T = T or Tfull
    shared = {
        "ada_w": np.ascontiguousarray(p["ada_w"]),
        "ada_kv_w": np.ascontiguousarray(p["ada_kv_w"]),
        "rw_w_rkv": np.ascontiguousarray(p["rw_w_rkv"][0]),
        "rw_w1a": np.ascontiguousarray(np.concatenate([p["rw_w1"][0], p["rw_a1"][0]], axis=1)),
        "rw_g1": np.ascontiguousarray(p["rw_g1"][0]),
        "rw_w2a2": np.ascontiguousarray(np.concatenate([p["rw_w2"][0], p["rw_a2"][0]], axis=0)),
        "rw_g2": np.ascontiguousarray(p["rw_g2"][0]),
        "rw_w_o": np.ascontiguousarray(p["rw_w_o"][0]),
        "w_kv": np.ascontiguousarray(p["w_kv"]),
        "df_w_q": np.ascontiguousarray(p["df_w_q"][0]),
        "df_w_o": np.ascontiguousarray(p["df_w_o"][0]),
        "moe_wr": np.ascontiguousarray(np.concatenate([p["moe_w_rg"], p["moe_w_re"]], axis=2)),
        "moe_w_gate": np.ascontiguousarray(p["moe_w_gate"].reshape(2, NEXP, 8, 128, EFF).transpose(0, 1, 3, 2, 4).reshape(2, NEXP * 128, 4096)),
        "moe_w_up": np.ascontiguousarray(p["moe_w_up"].reshape(2, NEXP, 8, 128, EFF).transpose(0, 1, 3, 2, 4).reshape(2, NEXP * 128, 4096)),
        "moe_w_down": np.ascontiguousarray(p["moe_w_down"].reshape(2, NEXP, 4, 128, D).transpose(0, 1, 3, 2, 4).reshape(2, NEXP * 128, 4096)),
    }
    vr = np.zeros((N_VR, D), np.float32)
    vr[VR_KK] = p["rw_k_k"][0]
    vr[VR_KA] = p["rw_k_a"][0]
    vr[VR_RK] = p["rw_r_k"][0].reshape(-1)
    vr[VR_GNG] = p["rw_gn_g"][0]
    vr[VR_GNB] = p["rw_gn_b"][0]
    vr[VR_W0] = p["rw_w0"][0]
    vr[VR_A0] = p["rw_a0"][0]
    vr[VR_FING] = p["final_g"]
    vr[VR_GM0] = p["ada_b"][0, 2 * D:3 * D]
    vr[VR_GF0] = p["ada_b"][0, 5 * D:6 * D]
    vr[VR_GM1] = p["ada_b"][1, 2 * D:3 * D]
    vr[VR_GF1] = p["ada_b"][1, 5 * D:6 * D]
    vr[VR_SUBLN] = np.tile(p["df_subln_g"][0], 8)
    shared["vec_row"] = vr
    lam = np.zeros((128, 4), np.float32)
    lam[:64, 0] = p["df_lq1"][0]
    lam[:64, 1] = p["df_lk1"][0]
    lam[:64, 2] = p["df_lq2"][0]
    lam[:64, 3] = p["df_lk2"][0]
    shared["lam"] = lam
    tri = (np.arange(128)[:, None] <= np.arange(128)[None, :]).astype(np.float32)
    maps = []
    for core in range(n_cores):
        b, s = core // 2, core % 2
        vc = np.zeros((128, N_VC, 8), np.float32)
        vc[:, VC_C] = _col(p["c"][b])
        vc[:, VC_NMIX0] = _col(p["norm_mix_g"][0])
        vc[:, VC_NFFN0] = _col(p["norm_ffn_g"][0])
        vc[:, VC_NMIX1] = _col(p["norm_mix_g"][1])
        vc[:, VC_NFFN1] = _col(p["norm_ffn_g"][1])
        vc[:, VC_NKV] = _col(p["norm_kv_g"])
        for i in range(6):
            vc[:, VC_MU + i] = _col(p["rw_mu"][0, i])
            vc[:, VC_ADAB0 + i] = _col(p["ada_b"][0, i * D:(i + 1) * D])
            vc[:, VC_ADAB1 + i] = _col(p["ada_b"][1, i * D:(i + 1) * D])
        vc[:, VC_KVB] = _col(p["ada_kv_b"][:D])
        vc[:, VC_KVB + 1] = _col(p["ada_kv_b"][D:])
        sel = np.zeros((128, 4), np.float32)
        sel[:, 0] = 1.0 if s == 0 else 0.0
        sel[:, 1] = 1.0 if s == 1 else 0.0
        cm = np.stack([tri if s == 0 else np.ones_like(tri), np.zeros_like(tri) if s == 0 else tri]).astype(np.float32)
        m = dict(shared)
        m.update({"x": np.ascontiguousarray(p["x"][b, :T]), "vec_col": vc, "sel": sel, "cmask": cm})
        maps.append(m)
    return maps


_CACHE = {}


def kernel(**inputs):
    T = int(np.asarray(inputs["x"]).shape[1])
    if T not in _CACHE:
        _CACHE[T] = build_program(T)[0]
    nc = _CACHE[T]
    maps = make_in_maps(inputs)
    res = run_bass_kernel_spmd(nc, maps, core_ids=list(range(8)))
    B = np.asarray(inputs["x"]).shape[0]
    outp = np.zeros((B, T, D), np.float32)
    for core in range(8):
        b, s = core // 2, core % 2
        o = np.asarray(res.results[core]["out"]).reshape(T // 256, 128, D)
        outp[b].reshape(T // 128, 128, D)[s::2] = o
    return outp
```

```python
import math
import os
from contextlib import ExitStack

import numpy as np
import concourse.bass as bass
import concourse.mybir as mybir
from concourse.bass_utils import run_bass_kernel_spmd

F32 = mybir.dt.float32
BF16 = mybir.dt.bfloat16
I32 = mybir.dt.int32
AF = mybir.ActivationFunctionType
ALU = mybir.AluOpType
AX = mybir.AxisListType

D = 1024
NH = 16
HN = 64
NEXP = 32
EFF = 512
C0 = -math.exp(-0.5)


class Buf:
    __slots__ = ("name", "w", "r")

    def __init__(self, name=""):
        self.name = name
        self.w = None
        self.r = {}


class KB:
    def __init__(self, nc, n_dma_sems=(16, 12, 6)):
        self.nc = nc
        self.eng = {"pe": nc.tensor, "act": nc.scalar, "dve": nc.vector, "pool": nc.gpsimd, "sp": nc.sync}
        self.sems = {}
        self.cnt = {}
        self._ctx = []
        for e in ("pe", "act", "dve", "pool"):
            self._mksem("c_" + e)
        self.dma_pool = {}
        for q, n in zip(("sp", "pool", "act"), n_dma_sems):
            keys = []
            for i in range(n):
                k = f"d_{q}{i}"
                self._mksem(k)
                keys.append(k)
            self.dma_pool[q] = [keys, 0]
        self.seen = {e: {} for e in self.eng}
        self.n_inst = 0
        self.n_wait = 0

    def _mksem(self, key):
        g = self.nc.semaphore(key)
        s = g.__enter__()
        self._ctx.append(g)
        self.sems[key] = s
        self.cnt[key] = 0

    def close(self):
        for g in reversed(self._ctx):
            g.__exit__(None, None, None)

    def _wait(self, e, ticket):
        if ticket is None:
            return
        key, val = ticket
        if self.seen[e].get(key, 0) >= val:
            return
        self.eng[e].wait_ge(self.sems[key], val)
        self.seen[e][key] = val
        self.n_wait += 1

    def _mark(self, ticket, reads, writes):
        k, v = ticket
        for b in reads:
            if b.r.get(k, 0) < v:
                b.r[k] = v
        for b in writes:
            b.w = ticket
            b.r = {}

    def op(self, e, fn, reads=(), writes=()):
        own = "c_" + e
        for b in reads:
            self._wait(e, b.w)
        for b in writes:
            if b.w is not None and (b.w[0] != own or e != "pe"):
                self._wait(e, b.w)
            for k, v in b.r.items():
                if k != own or e != "pe":
                    self._wait(e, (k, v))
        inst = fn(self.eng[e])
        self.cnt[own] += 1
        inst.then_inc(self.sems[own], 1)
        self._mark((own, self.cnt[own]), reads, writes)
        self.n_inst += 1
        return inst

    def dma(self, q, out, in_, reads=(), writes=(), **kw):
        keys, idx = self.dma_pool[q]
        key = keys[idx % len(keys)]
        self.dma_pool[q][1] = idx + 1
        if self.cnt[key] > 0:
            self._wait(q, (key, self.cnt[key]))
        for b in reads:
            self._wait(q, b.w)
        for b in writes:
            self._wait(q, b.w)
            for k, v in b.r.items():
                self._wait(q, (k, v))
        inst = self.eng[q].dma_start(out=out, in_=in_, **kw)
        self.cnt[key] += 16
        inst.then_inc(self.sems[key], 16)
        self._mark((key, self.cnt[key]), reads, writes)
        self.n_inst += 1
        return inst

    def barrier(self):
        for e in self.eng:
            for key, val in self.cnt.items():
                if val > 0:
                    self._wait(e, (key, val))

    def finish(self, e, bufs):
        for b in bufs:
            self._wait(e, b.w)


class T_:
    def __init__(self, t, name):
        self.t = t
        self.b = Buf(name)

    def __getitem__(self, k):
        return self.t[k]

    def v(self, k):
        return View(self.t[k], self.b)


class _APWrap:
    def __init__(self, ap):
        self.ap = ap

    def __getitem__(self, k):
        return self.ap[k]


class View:
    def __init__(self, ap, b):
        self.ap = ap
        self.b = b


def _ap(x):
    return x.ap if isinstance(x, View) else x.t[:]


VC_C, VC_NMIX0, VC_NFFN0, VC_NMIX1, VC_NFFN1, VC_NKV = 0, 1, 2, 3, 4, 5
VC_MU = 6
VC_ADAB0 = 12
VC_ADAB1 = 18
VC_KVB = 24
N_VC = 26
VR_KK, VR_KA, VR_RK, VR_GNG, VR_GNB, VR_W0, VR_A0, VR_FING = 0, 1, 2, 3, 4, 5, 6, 7
VR_GM0, VR_GF0, VR_GM1, VR_GF1 = 8, 9, 10, 11
VR_SUBLN = 12
N_VR = 13


def build_program(T, dbg=0, phases=("p0", "p1", "p2", "p3", "p4", "p5", "moe")):
    NT = T // 128
    nc = bass.Bass("TRN2", target_bir_lowering=False)
    kb = KB(nc)
    es = ExitStack()

    in_names = []

    def din(name, shape, dt=F32, need=True):
        if not need:
            return None
        in_names.append(name)
        return nc.dram_tensor(name, list(shape), dt, kind="ExternalInput").ap()

    has = lambda ph: ph in phases

    dbg_outs = []

    def dscr(name, shape, dt=F32, tap=False):
        kind = "ExternalOutput" if (dbg and tap) else "Internal"
        if dbg and tap:
            dbg_outs.append(name)
        return nc.dram_tensor(name, list(shape), dt, kind=kind).ap()

    x_in = din("x", [T, D])
    vec_col = din("vec_col", [128, N_VC, 8])
    vec_row = din("vec_row", [N_VR, D])
    ada_w = din("ada_w", [2, D, 6 * D])
    ada_kv_w = din("ada_kv_w", [D, 2 * D])
    w_rkv = din("rw_w_rkv", [3, D, D], need=has("p1"))
    w1a_in = din("rw_w1a", [D, 128], need=has("p1"))
    g1_in = din("rw_g1", [D, 128], need=has("p1"))
    w2a2_in = din("rw_w2a2", [128, D], need=has("p1"))
    g2_in = din("rw_g2", [128, D], need=has("p1"))
    rw_wo = din("rw_w_o", [D, D], need=has("p3"))
    w_kv = din("w_kv", [D, 2 * D], need=has("p4"))
    df_wq = din("df_w_q", [D, D], need=has("p5"))
    df_wo = din("df_w_o", [D, D], need=has("p5"))
    moe_wr = din("moe_wr", [2, D, 36], need=has("moe"))
    moe_wg = din("moe_w_gate", [2, NEXP * 128, 4096], need=has("moe"))
    moe_wu = din("moe_w_up", [2, NEXP * 128, 4096], need=has("moe"))
    moe_wd = din("moe_w_down", [2, NEXP * 128, 4096], need=has("moe"))
    sel_in = din("sel", [128, 4], need=has("p5"))
    cmask_in = din("cmask", [2, 128, 128], need=has("p5"))
    lam_in = din("lam", [128, 4], need=has("p5"))
    out = nc.dram_tensor("out", [T // 2, D], F32, kind="ExternalOutput").ap()

    XF = [dscr(f"xf{q}", [NT, 128, D], BF16, tap=(dbg == 1)) for q in range(4)]
    TKV = [dscr(f"tk{q}", [T, D], BF16, tap=(dbg == 1)) for q in range(3)]
    GSC = dscr("gsc", [T, D], F32, tap=(dbg == 1))
    BON = dscr("bon", [T, D], F32, tap=(dbg == 1))
    GAMD = dscr("gamd", [128, 8, 2 * NT], F32, tap=(dbg == 1))
    YSC = dscr("ysc", [T, D], F32, tap=(dbg == 2))

    def sb(name, shape, dt=F32):
        t = es.enter_context(nc.sbuf_tensor(name, list(shape), dt))
        return T_(t, name)

    def ps(name, shape, dt=F32):
        t = es.enter_context(nc.psum_tensor(name, list(shape), dt))
        return T_(t, name)

    ident = sb("ident", [128, 128])
    identb = sb("identb", [128, 128], BF16)
    ones = sb("ones", [128, 128])
    onesb = sb("onesb", [128, 128], BF16)
    vcol = sb("vcol", [128, N_VC, 8])
    modc = sb("modc", [128, 12, 8])
    AB = sb("AB", [128, 12, 8])
    cact = sb("cact", [128, 8])

    kb.op("pool", lambda e: e.memset(ident[:], 0.0), writes=[ident.b])
    kb.op("pool", lambda e: e.affine_select(out=ident[:], in_=ident[:], pattern=[[-1, 128]], compare_op=ALU.not_equal,
                                            fill=1.0, base=0, channel_multiplier=1), reads=[ident.b], writes=[ident.b])
    kb.op("pool", lambda e: e.tensor_copy(out=identb[:], in_=ident[:]), reads=[ident.b], writes=[identb.b])
    kb.op("pool", lambda e: e.memset(ones[:], 1.0), writes=[ones.b])
    kb.op("pool", lambda e: e.memset(onesb[:], 1.0), writes=[onesb.b])
    kb.dma("sp", vcol[:], vec_col[:, :, :], writes=[vcol.b])
    kb.op("act", lambda e: e.activation(out=cact[:], in_=vcol[:, VC_C, :], func=AF.Silu), reads=[vcol.b], writes=[cact.b])

    def bc_row(dst, row):
        kb.dma("sp", dst[:], row.partition_broadcast(128), writes=[dst.b])

    gmb = [sb(f"gmb{i}", [128, D]) for i in range(4)]
    with ExitStack() as es0:
        wst = [T_(es0.enter_context(nc.sbuf_tensor(f"wst{i}", [128, 8, D], F32)), f"wst{i}") for i in range(2)]
        cbc = T_(es0.enter_context(nc.sbuf_tensor("cbc", [128, 8, 128], F32)), "cbc")
        pcol = T_(es0.enter_context(nc.psum_tensor("pcol", [128, 8], F32)), "pcol")
        prow = T_(es0.enter_context(nc.psum_tensor("prow", [128, D], F32)), "prow")
        brow = T_(es0.enter_context(nc.sbuf_tensor("brow", [128, D], F32)), "brow")
        for m in range(8):
            kb.op("dve", lambda e, m=m: e.tensor_scalar(out=cbc[:, m, :], in0=ones[:], scalar1=cact[:, m:m + 1], scalar2=None,
                                                        op0=ALU.mult), reads=[ones.b, cact.b], writes=[cbc.b])
        jobs = []
        for l in range(2):
            base = VC_ADAB0 if l == 0 else VC_ADAB1
            jobs += [(ada_w[l], 0 * D, "col", 4 * l + 0, base + 0), (ada_w[l], 1 * D, "col", 4 * l + 1, base + 1),
                     (ada_w[l], 3 * D, "col", 4 * l + 2, base + 3), (ada_w[l], 4 * D, "col", 4 * l + 3, base + 4),
                     (ada_w[l], 2 * D, "row", 2 * l + 0, VR_GM0 + 2 * l), (ada_w[l], 5 * D, "row", 2 * l + 1, VR_GF0 + 2 * l)]
        jobs += [(ada_kv_w, 0, "col", 8, VC_KVB), (ada_kv_w, D, "col", 9, VC_KVB + 1)]
        for ji, (src, off, kind, di, bi) in enumerate(jobs):
            w = wst[ji % 2]
            kb.dma("sp" if ji % 2 == 0 else "pool", w[:], src[:, off:off + D].rearrange("(m p) n -> p m n", p=128), writes=[w.b])
            if kind == "col":
                for fc in range(8):
                    for mk in range(8):
                        kb.op("pe", lambda e, fc=fc, mk=mk, w=w: e.matmul(pcol[:, fc:fc + 1], lhsT=w[:, mk, fc * 128:(fc + 1) * 128],
                                                                           rhs=cact[:, mk:mk + 1], start=(mk == 0), stop=(mk == 7)),
                              reads=[w.b, cact.b], writes=[pcol.b])
                kb.op("dve", lambda e, di=di, bi=bi: e.tensor_tensor(out=modc[:, di, :], in0=pcol[:], in1=vcol[:, bi, :], op=ALU.add),
                      reads=[pcol.b, vcol.b], writes=[modc.b])
            else:
                for hf in range(2):
                    for mk in range(8):
                        kb.op("pe", lambda e, hf=hf, mk=mk, w=w: e.matmul(prow[:, hf * 512:(hf + 1) * 512], lhsT=cbc[:, mk, :],
                                                                           rhs=w[:, mk, hf * 512:(hf + 1) * 512], start=(mk == 0), stop=(mk == 7)),
                              reads=[w.b, cbc.b], writes=[prow.b])
                bc_row(brow, vec_row[bi:bi + 1, :])
                kb.op("dve", lambda e, di=di: e.tensor_tensor(out=gmb[di][:], in0=prow[:], in1=brow[:], op=ALU.add),
                      reads=[prow.b, brow.b], writes=[gmb[di].b])
        for (ai, gi, shi, sci) in [(0, VC_NMIX0, 0, 1), (2, VC_NFFN0, 2, 3), (4, VC_NMIX1, 4, 5), (6, VC_NFFN1, 6, 7), (8, VC_NKV, 8, 9)]:
            kb.op("dve", lambda e, ai=ai, sci=sci: e.tensor_scalar(out=AB[:, ai, :], in0=modc[:, sci, :], scalar1=1.0, scalar2=32.0,
                                                                  op0=ALU.add, op1=ALU.mult), reads=[modc.b], writes=[AB.b])
            kb.op("dve", lambda e, ai=ai, gi=gi: e.tensor_tensor(out=AB[:, ai, :], in0=AB[:, ai, :], in1=vcol[:, gi, :], op=ALU.mult),
                  reads=[AB.b, vcol.b], writes=[AB.b])
            kb.op("dve", lambda e, ai=ai, shi=shi: e.tensor_copy(out=AB[:, ai + 1, :], in_=modc[:, shi, :]), reads=[modc.b], writes=[AB.b])

    def TT(e, o, a, b_, op):
        kb.op(e, lambda en: en.tensor_tensor(out=_ap(o), in0=_ap(a), in1=_ap(b_), op=op), reads=[a.b, b_.b], writes=[o.b])

    def TS(e, o, a, s1, s2, op0, op1=None):
        rd = [a.b] + [z.b for z in (s1, s2) if isinstance(z, (View, T_))]
        f = lambda z: _ap(z) if isinstance(z, (View, T_)) else z
        if op1 is None:
            kb.op(e, lambda en: en.tensor_scalar(out=_ap(o), in0=_ap(a), scalar1=f(s1), scalar2=None, op0=op0), reads=rd, writes=[o.b])
        else:
            kb.op(e, lambda en: en.tensor_scalar(out=_ap(o), in0=_ap(a), scalar1=f(s1), scalar2=f(s2), op0=op0, op1=op1), reads=rd, writes=[o.b])

    def STT(e, o, a, sc, b_, op0, op1):
        rd = [a.b, b_.b] + ([sc.b] if isinstance(sc, (View, T_)) else [])
        f = lambda z: _ap(z) if isinstance(z, (View, T_)) else z
        kb.op(e, lambda en: en.scalar_tensor_tensor(out=_ap(o), in0=_ap(a), scalar=f(sc), in1=_ap(b_), op0=op0, op1=op1), reads=rd, writes=[o.b])

    def ACT(o, a, func, scale=1.0, bias=0.0, accum=None):
        rd = [a.b] + [z.b for z in (scale, bias) if isinstance(z, (View, T_))]
        wr = [o.b] + ([accum.b] if accum is not None else [])
        f = lambda z: _ap(z) if isinstance(z, (View, T_)) else z
        kw = {}
        if accum is not None:
            kw["accum_out"] = _ap(accum)
        kb.op("act", lambda en: en.activation(out=_ap(o), in_=_ap(a), func=func, bias=f(bias), scale=f(scale), **kw), reads=rd, writes=wr)

    def CP(e, o, a):
        if e == "act":
            kb.op(e, lambda en: en.copy(out=_ap(o), in_=_ap(a)), reads=[a.b], writes=[o.b])
        else:
            kb.op(e, lambda en: en.tensor_copy(out=_ap(o), in_=_ap(a)), reads=[a.b], writes=[o.b])

    def RED(e, o, a, op=ALU.add, axis=AX.X):
        kb.op(e, lambda en: en.tensor_reduce(out=_ap(o), in_=_ap(a), axis=axis, op=op), reads=[a.b], writes=[o.b])

    def MM(o, lhsT, rhs, start=True, stop=True):
        kb.op("pe", lambda en: en.matmul(_ap(o), lhsT=_ap(lhsT), rhs=_ap(rhs), start=start, stop=stop), reads=[lhsT.b, rhs.b], writes=[o.b])

    def TR(o, a, idt):
        kb.op("pe", lambda en: en.transpose(out=_ap(o), in_=_ap(a), identity=_ap(idt)), reads=[a.b, idt.b], writes=[o.b])

    def DMA(q, o_ap, i_ap, reads=(), writes=()):
        kb.dma(q, o_ap, i_ap, reads=[r.b for r in reads], writes=[w.b for w in writes])

    class DR:
        def __init__(self, name):
            self.b = Buf(name)

    taps = {}
    if dbg == 9:
        o_ab = nc.dram_tensor("o_ab", [128, 12, 8], F32, kind="ExternalOutput").ap()
        o_gm = nc.dram_tensor("o_gm", [4, 128, D], F32, kind="ExternalOutput").ap()
        bo = Buf("o")
        kb.dma("sp", o_ab[:, :, :], AB[:], reads=[AB.b], writes=[bo])
        for i in range(4):
            kb.dma("sp", o_gm[i], gmb[i][:], reads=[gmb[i].b], writes=[bo])
        kb.finish("sp", [bo])
        kb.close()
        es.close()
        return nc, in_names, ["o_ab", "o_gm"]

    gamT = sb("gamT", [128, 8, 2 * NT])
    dXF = [[DR(f"xf{q}_{t}") for t in range(NT)] for q in range(4)]
    dTK = [[DR(f"tk{q}_{t}") for t in range(NT)] for q in range(3)]
    dG = [DR(f"g{t}") for t in range(NT)]
    dBON = [DR(f"bon{t}") for t in range(NT)]
    rowsl = lambda t: slice(t * 128, (t + 1) * 128)
    kb.barrier()
    if has("p1"):
        with ExitStack() as es1:
            def sb1(name, shape, dt=F32):
                return T_(es1.enter_context(nc.sbuf_tensor(name, list(shape), dt)), name)

            def ps1(name, shape, dt=F32):
                return T_(es1.enter_context(nc.psum_tensor(name, list(shape), dt)), name)

            Wp = [sb1(f"Wp{i}", [128, 8, D], BF16) for i in range(3)]
            W1 = [sb1(f"W1{i}", [128, 8, 128], BF16) for i in range(4)]
            w2a2 = sb1("w2a2", [128, D], BF16)
            g2 = sb1("g2", [128, D], BF16)
            b0row = sb1("b0row", [1, 2, D])
            kk_bc, ka_bc, omka_bc, rk_bc = [sb1(n, [128, D]) for n in ("kk_bc", "ka_bc", "omka_bc", "rk_bc")]
            hT = sb1("hT", [128, 8, 129])
            F = [sb1(f"F{i}", [128, D]) for i in range(16)]
            Hb = [sb1(f"Hb{i}", [128, 8, 128], BF16) for i in range(5)]
            Q = [sb1(f"Q{i}", [128, D], BF16) for i in range(7)]
            XFs = [sb1(f"XFs{i}", [128, D], BF16) for i in range(2)]
            wlal = sb1("wlal", [128, 128], BF16)
            glT = sb1("glT", [128, 128], BF16)
            Ltri = sb1("Ltri", [128, 128])
            Lblk = sb1("Lblk", [128, 128])
            ind2 = sb1("ind2", [128, 2])
            ss = sb1("ss", [128, 1])
            rs = sb1("rs", [128, 1])
            ssh = sb1("ssh", [128, 16])
            rnh = sb1("rnh", [128, 16])
            bsum = sb1("bsum", [128, 16])
            PA, PB, PC = [ps1(n, [128, D]) for n in ("PA", "PB", "PC")]
            PS = ps1("PS", [128, 512])
            PT = ps1("PT", [128, D], BF16)
            cs = lambda m: slice(m * 128, (m + 1) * 128)
            hs = lambda h: slice(h * 512, (h + 1) * 512)

            engs = ["act", "dve", "pool"]
            n = 0
            for i in range(3):
                for m in range(8):
                    st = F[14 + n % 2]
                    DMA("sp" if n % 2 == 0 else "pool", st[:], w_rkv[i, cs(m), :], writes=[st])
                    CP(engs[n % 3], Wp[i].v((slice(None), m, slice(None))), st)
                    n += 1
            st3 = lambda t: View(t.t[:].rearrange("p (m n) -> p m n", n=128), t.b)
            for j, (src, mus) in enumerate([(w1a_in, (3, 4)), (g1_in, (5, 5))]):
                st = F[13]
                DMA("sp", st3(st).ap, src.rearrange("(m p) n -> p m n", p=128), writes=[st])
                CP("dve", W1[2 * j], st3(st))
                for m in range(8):
                    for hh in range(2):
                        TS("pool" if hh else "dve", W1[2 * j + 1].v((slice(None), m, slice(hh * 64, hh * 64 + 64))),
                           View(st3(st).ap[:, m, hh * 64:hh * 64 + 64], st.b), vcol.v((slice(None), VC_MU + mus[hh], slice(m, m + 1))), None, ALU.mult)
            DMA("sp", F[12][:], w2a2_in[:, :], writes=[F[12]])
            CP("act", w2a2, F[12])
            DMA("sp", F[11][:], g2_in[:, :], writes=[F[11]])
            CP("act", g2, F[11])
            DMA("sp", b0row[0:1, 0, :], vec_row[VR_W0:VR_W0 + 1, :], writes=[b0row])
            DMA("sp", b0row[0:1, 1, :], vec_row[VR_A0:VR_A0 + 1, :], writes=[b0row])
            bc_row(kk_bc, vec_row[VR_KK:VR_KK + 1, :])
            bc_row(ka_bc, vec_row[VR_KA:VR_KA + 1, :])
            bc_row(rk_bc, vec_row[VR_RK:VR_RK + 1, :])
            TS("dve", omka_bc, ka_bc, -1.0, 1.0, ALU.mult, ALU.add)
            kb.op("pool", lambda e: e.memset(Lblk[:], 0.0), writes=[Lblk.b])
            kb.op("pool", lambda e: e.memset(Lblk[0:64, 0:64], 1.0), writes=[Lblk.b])
            kb.op("pool", lambda e: e.memset(Lblk[64:128, 64:128], 1.0), writes=[Lblk.b])
            CP("pool", Ltri, Lblk)
            for h0 in (0, 64):
                kb.op("pool", lambda e, h0=h0: e.affine_select(out=Ltri[h0:h0 + 64, h0:h0 + 64], in_=Ltri[h0:h0 + 64, h0:h0 + 64], pattern=[[1, 64]],
                                                              compare_op=ALU.is_ge, fill=0.0, base=0, channel_multiplier=-1),
                      reads=[Ltri.b], writes=[Ltri.b])
            kb.op("pool", lambda e: e.memset(ind2[:], 0.0), writes=[ind2.b])
            kb.op("pool", lambda e: e.memset(ind2[0:64, 0:1], 1.0), writes=[ind2.b])
            kb.op("pool", lambda e: e.memset(ind2[64:128, 1:2], 1.0), writes=[ind2.b])
            kb.op("pool", lambda e: e.memset(hT[:, :, 0:1], 0.0), writes=[hT.b])

            v3 = lambda t: View(t.t[:].rearrange("p (m n) -> p m n", n=128), t.b)
            vh = lambda t: View(t.t[:].rearrange("p (h n) -> p h n", n=64), t.b)
            bch = lambda t: View(t.t[:].unsqueeze(2).broadcast_to([128, 16, 64]), t.b)
            for tt in range(NT):
                DMA("sp", F[0][:], x_in[rowsl(tt), :], writes=[F[0]])
                ACT(F[1], F[0], AF.Square, accum=ss)
                ACT(rs, ss, AF.Ln, bias=1024e-6)
                ACT(rs, rs, AF.Exp, scale=-0.5)
                TS("dve", F[1], F[0], rs, None, ALU.mult)
                for m in range(8):
                    TR(PA.v((slice(None), cs(m))), F[1].v((slice(None), cs(m))), ident)
                for m in range(8):
                    ACT(hT.v((slice(None), m, slice(1, 129))), PA.v((slice(None), cs(m))), AF.Identity,
                        scale=AB.v((slice(None), 0, slice(m, m + 1))), bias=AB.v((slice(None), 1, slice(m, m + 1))))
                hprev = hT.v((slice(None), slice(None), slice(0, 128)))
                hcur = hT.v((slice(None), slice(None), slice(1, 129)))
                TT("dve", v3(F[2]), hprev, hcur, ALU.subtract)
                CP("pool", Hb[0], hcur)
                CP("pool", Hb[1], v3(F[2]))
                n = 0
                for i in range(3):
                    for m in range(8):
                        STT("dve", Hb[2 + i].v((slice(None), m, slice(None))), F[2].v((slice(None), cs(m))),
                            vcol.v((slice(None), VC_MU + i, slice(m, m + 1))), hT.v((slice(None), m, slice(1, 129))), ALU.mult, ALU.add)
                        n += 1
                CP("pool", hT.v((slice(None), slice(None), slice(0, 1))), hT.v((slice(None), slice(None), slice(128, 129))))
                for j in range(2):
                    o = PS.v((slice(None), slice(j * 128, (j + 1) * 128)))
                    for mk in range(8):
                        MM(o, W1[2 * j].v((slice(None), mk, slice(None))), Hb[0].v((slice(None), mk, slice(None))), start=(mk == 0), stop=False)
                        MM(o, W1[2 * j + 1].v((slice(None), mk, slice(None))), Hb[1].v((slice(None), mk, slice(None))), start=False, stop=(mk == 7))
                ACT(wlal.v((slice(0, 64), slice(None))), PS.v((slice(0, 64), slice(0, 128))), AF.Tanh)
                ACT(wlal.v((slice(64, 128), slice(None))), PS.v((slice(64, 128), slice(0, 128))), AF.Identity)
                ACT(glT, PS.v((slice(None), slice(128, 256))), AF.Sigmoid)
                for i, P_ in enumerate((PB, PC, PA)):
                    for h in range(2):
                        for mk in range(8):
                            MM(P_.v((slice(None), hs(h))), Hb[2 + i].v((slice(None), mk, slice(None))), Wp[i].v((slice(None), mk, hs(h))),
                               start=(mk == 0), stop=(mk == 7))
                CP("dve", F[0], PB)
                CP("act", F[1], PC)
                CP("act", F[2], PA)
                for h in range(2):
                    MM(PB.v((slice(None), hs(h))), wlal.v((slice(0, 64), slice(None))), w2a2.v((slice(0, 64), hs(h))), start=True, stop=False)
                    MM(PB.v((slice(None), hs(h))), ones.v((slice(0, 1), slice(None))), b0row.v((slice(0, 1), 0, hs(h))), start=False, stop=True)
                for h in range(2):
                    MM(PC.v((slice(None), hs(h))), wlal.v((slice(64, 128), slice(None))), w2a2.v((slice(64, 128), hs(h))), start=True, stop=False)
                    MM(PC.v((slice(None), hs(h))), ones.v((slice(0, 1), slice(None))), b0row.v((slice(0, 1), 1, hs(h))), start=False, stop=True)
                for h in range(2):
                    MM(PA.v((slice(None), hs(h))), glT, g2.v((slice(None), hs(h))))
                ACT(F[3], PB, AF.Sigmoid)
                ACT(F[4], PC, AF.Sigmoid)
                CP("act", F[5], PA)
                DMA("pool", GSC[rowsl(tt), :], F[5][:], reads=[F[5]], writes=[dG[tt]])
                for h in range(2):
                    MM(PB.v((slice(None), hs(h))), Ltri, F[3].v((slice(None), hs(h))))
                for h in range(2):
                    MM(PC.v((slice(None), hs(h))), Lblk, F[3].v((slice(None), hs(h))))
                for m in range(8):
                    MM(PS.v((slice(None), slice(256 + 2 * m, 258 + 2 * m))), F[3].v((slice(None), cs(m))), ind2)
                ACT(F[11], PB, AF.Exp, scale=C0)
                ACT(F[13], PB, AF.Exp, scale=-C0)
                ACT(F[10], F[3], AF.Exp, scale=-C0)
                ACT(F[14], PC, AF.Exp, scale=C0)
                ACT(gamT.v((slice(None), slice(None), slice(2 * tt, 2 * tt + 2))),
                    View(PS.t[:, 256:272].rearrange("p (m c) -> p m c", c=2), PS.b), AF.Exp, scale=C0)
                TT("pool", F[12], F[11], F[10], ALU.mult)
                TT("pool", F[14], F[14], F[13], ALU.mult)
                TT("dve", F[6], F[1], kk_bc, ALU.mult)
                TT("pool", F[7], F[6], F[6], ALU.mult)
                RED("dve", ssh, vh(F[7]))
                ACT(rnh, ssh, AF.Ln, bias=1e-12)
                ACT(rnh, rnh, AF.Exp, scale=-0.5)
                TT("dve", vh(F[6]), vh(F[6]), bch(rnh), ALU.mult)
                TT("pool", F[7], F[4], ka_bc, ALU.mult)
                TT("pool", F[7], F[7], omka_bc, ALU.add)
                TT("dve", F[8], F[1], F[7], ALU.mult)
                TT("pool", F[9], F[6], F[4], ALU.mult)
                STT("dve", Q[0], F[6], -1.0, F[12], ALU.mult, ALU.mult)
                TT("dve", Q[1], F[0], F[11], ALU.mult)
                TT("pool", Q[2], F[9], F[13], ALU.mult)
                TT("dve", Q[3], F[8], F[13], ALU.mult)
                TT("pool", Q[4], F[9], F[14], ALU.mult)
                TT("dve", Q[5], F[8], F[14], ALU.mult)
                CP("pool", Q[6], F[2])
                DMA("pool", TKV[0][rowsl(tt), :], Q[6][:], reads=[Q[6]], writes=[dTK[0][tt]])
                DMA("pool", TKV[1][rowsl(tt), :], Q[4][:], reads=[Q[4]], writes=[dTK[1][tt]])
                DMA("pool", TKV[2][rowsl(tt), :], Q[5][:], reads=[Q[5]], writes=[dTK[2][tt]])
                for q in range(4):
                    for m in range(8):
                        TR(PT.v((slice(None), cs(m))), Q[q].v((slice(None), cs(m))), identb)
                    CP("act" if q % 2 == 0 else "dve", XFs[q % 2], PT)
                    DMA("sp", XF[q][tt], XFs[q % 2][:], reads=[XFs[q % 2]], writes=[dXF[q][tt]])
                TT("pool", F[7], F[0], F[8], ALU.mult)
                TT("pool", F[7], F[7], rk_bc, ALU.mult)
                RED("dve", bsum, vh(F[7]))
                TT("dve", vh(F[15]), vh(F[2]), bch(bsum), ALU.mult)
                DMA("pool", BON[rowsl(tt), :], F[15][:], reads=[F[15]], writes=[dBON[tt]])
    dGAM = DR("gamd")
    if dbg == 1:
        DMA("sp", GAMD[:, :, :], gamT[:], reads=[gamT], writes=[dGAM])
        allb = [d.b for l_ in dXF + dTK for d in l_] + [d.b for d in dG + dBON] + [dGAM.b]
        kb.finish("sp", allb)
        kb.finish("pool", allb)
        kb.close()
        es.close()
        return nc, in_names, dbg_outs

    NC = T // 64
    dY = [DR(f"y{c}") for c in range(NC)]
    kb.barrier()
    if has("p2"):
        with ExitStack() as es2:
            def sb2(name, shape, dt=F32):
                return T_(es2.enter_context(nc.sbuf_tensor(name, list(shape), dt)), name)

            def ps2(name, shape, dt=F32):
                return T_(es2.enter_context(nc.psum_tensor(name, list(shape), dt)), name)

            XFt = [[sb2(f"XFt{q}_{i}", [64, 16, 64], BF16) for q in range(4)] for i in range(2)]
            TKt = [[sb2(f"TKt{q}_{i}", [64, 16, 64], BF16) for q in range(3)] for i in range(2)]
            m_su, m_iu, m_sl, I16 = [sb2(n, [64, 16, 64]) for n in ("m_su", "m_iu", "m_sl", "I16")]
            Pm = [sb2(f"Pm{i}", [64, 16, 64], BF16) for i in range(2)]
            PTm = [sb2(f"PTm{i}", [64, 16, 64], BF16) for i in range(2)]
            Tm = sb2("Tm", [64, 16, 64], BF16)
            Aak, Arb, Ark = [sb2(n, [64, 16, 64], BF16) for n in ("Aak", "Arb", "Ark")]
            W1T = sb2("W1T", [64, 16, 64], BF16)
            UT = sb2("UT", [64, 16, 64], BF16)
            ST = sb2("ST", [64, 16, 64])
            STb = sb2("STb", [64, 16, 64], BF16)
            gam = sb2("gam", [64, 16, NC])
            Ysb = [sb2(f"Ysb{i}", [64, 16, 64]) for i in range(2)]
            PG = [ps2(f"PG{i}", [64, 16, 64]) for i in range(2)]
            PW = ps2("PW", [64, 16, 64])
            PSt = ps2("PSt", [64, 16, 64])
            A_ = slice(None)

            for (mt, pat, cm, op) in [(m_su, [[0, 16], [1, 64]], -1, ALU.is_gt), (m_iu, [[0, 16], [1, 64]], -1, ALU.is_ge),
                                      (m_sl, [[0, 16], [-1, 64]], 1, ALU.is_gt), (I16, [[0, 16], [1, 64]], -1, ALU.is_equal)]:
                kb.op("pool", lambda e, mt=mt: e.memset(mt[:], 1.0), writes=[mt.b])
                kb.op("pool", lambda e, mt=mt, pat=pat, cm=cm, op=op: e.affine_select(
                    out=mt[:], in_=mt[:], pattern=pat, compare_op=op, fill=0.0, base=0, channel_multiplier=cm),
                    reads=[mt.b], writes=[mt.b])
            kb.op("pool", lambda e: e.memset(ST[:], 0.0), writes=[ST.b])
            kb.op("pool", lambda e: e.memset(STb[:], 0.0), writes=[STb.b])
            DMA("sp", GAMD[:, :, :], gamT[:], reads=[gamT], writes=[dGAM])
            DMA("sp", gam[:].rearrange("j (m p) c -> j m p c", p=2), GAMD.rearrange("(p j) m c -> j m p c", p=2), reads=[dGAM], writes=[gam])

            def loads(c):
                i = c % 2
                tt, ci = c // 2, c % 2
                for q in range(4):
                    src = XF[q][tt].rearrange("(p j) (m t) -> j m p t", p=2, t=128)[:, :, :, ci * 64:(ci + 1) * 64]
                    DMA("sp", XFt[i][q][:].rearrange("j (m p) t -> j m p t", p=2), src, reads=[dXF[q][tt]], writes=[XFt[i][q]])
                for q in range(3):
                    DMA("sp", TKt[i][q][:].rearrange("s h n -> s (h n)"), TKV[q][c * 64:(c + 1) * 64, :], reads=[dTK[q][tt]], writes=[TKt[i][q]])

            def headmm(o, lt, rt, **kw):
                for h in range(16):
                    MM(o.v((A_, h, A_)), lt.v((A_, h, A_)), rt.v((A_, h, A_)), **kw)

            P2STOP = int(os.environ.get("P2STOP", "0"))
            loads(0)
            for c in range(NC):
                if c + 1 < NC:
                    loads(c + 1)
                At_, Rt_, Bt_, Kt_ = XFt[c % 2]
                Vt_, Bh_, Kh_ = TKt[c % 2]
                headmm(PG[0], Bt_, At_)
                headmm(PG[1], At_, Bt_)
                TT("dve", Pm[0], PG[0], m_su, ALU.mult)
                TT("dve", PTm[0], PG[1], m_sl, ALU.mult)
                TT("pool", Tm, Pm[0], I16, ALU.add)
                headmm(PG[0], Kt_, At_)
                headmm(PG[1], Bt_, Rt_)
                TT("dve", Aak, PG[0], m_su, ALU.mult)
                TT("dve", Arb, PG[1], m_iu, ALU.mult)
                headmm(PG[0], Kt_, Rt_)
                TT("dve", Ark, PG[0], m_iu, ALU.mult)
                if P2STOP == 2:
                    continue
                cur = 0
                for lvl in range(1, 6):
                    nxt = 1 - cur
                    headmm(PG[1], Pm[cur], PTm[cur])
                    if lvl < 5:
                        headmm(PG[0], PTm[cur], Pm[cur])
                    CP("act", PTm[nxt], PG[1])
                    if lvl < 5:
                        CP("act", Pm[nxt], PG[0])
                    pg = PG[0] if lvl == 5 else PG[1]
                    headmm(pg, PTm[nxt], Tm)
                    TT("dve", Tm, Tm, pg, ALU.add)
                    cur = nxt
                if P2STOP == 3:
                    continue
                Y_ = Ysb[c % 2]
                for h in range(16):
                    MM(PW.v((A_, h, A_)), At_.v((A_, h, A_)), STb.v((A_, h, A_)), start=True, stop=False)
                    MM(PW.v((A_, h, A_)), Aak.v((A_, h, A_)), Vt_.v((A_, h, A_)), start=False, stop=True)
                CP("act", W1T, PW)
                headmm(PW, Tm, W1T)
                CP("act", UT, PW)
                for h in range(16):
                    MM(PSt.v((A_, h, A_)), Bh_.v((A_, h, A_)), UT.v((A_, h, A_)), start=True, stop=False)
                    MM(PSt.v((A_, h, A_)), Kh_.v((A_, h, A_)), Vt_.v((A_, h, A_)), start=False, stop=True)
                for h in range(16):
                    MM(PW.v((A_, h, A_)), Rt_.v((A_, h, A_)), STb.v((A_, h, A_)), start=True, stop=False)
                    MM(PW.v((A_, h, A_)), Arb.v((A_, h, A_)), UT.v((A_, h, A_)), start=False, stop=False)
                    MM(PW.v((A_, h, A_)), Ark.v((A_, h, A_)), Vt_.v((A_, h, A_)), start=False, stop=True)
                TT("dve", ST, ST, View(gam.t[:, :, c:c + 1].broadcast_to([64, 16, 64]), gam.b), ALU.mult)
                TT("dve", ST, ST, PSt, ALU.add)
                CP("act", Y_, PW)
                CP("dve", STb, ST)
                DMA("pool", YSC[c * 64:(c + 1) * 64, :], Y_[:].rearrange("p h n -> p (h n)"), reads=[Y_], writes=[dY[c]])
    if dbg == 2:
        allb = [d.b for d in dY]
        kb.finish("sp", allb)
        kb.finish("pool", allb)
        kb.close()
        es.close()
        return nc, in_names, dbg_outs

    X1 = dscr("x1s", [T, D], F32, tap=(dbg == 3))
    dX1 = [DR(f"x1_{t}") for t in range(NT)]
    GT = 8 if NT >= 8 else NT
    cs = lambda m: slice(m * 128, (m + 1) * 128)
    hs = lambda h: slice(h * 512, (h + 1) * 512)
    A_ = slice(None)

    class FFN2:
        def __init__(self, es_, tag, NTL):
            def sbx(name, shape, dt=F32):
                return T_(es_.enter_context(nc.sbuf_tensor(name + tag, list(shape), dt)), name)

            def psx(name, shape, dt=F32):
                return T_(es_.enter_context(nc.psum_tensor(name + tag, list(shape), dt)), name)
            self.tag, self.NTL = tag, NTL
            self.NB = NTL + 32
            NB = self.NB
            self.XA = dscr("xa" + tag, [NTL * 128, D], F32)
            self.HTOK = dscr("htok" + tag, [NTL * 128, D], BF16)
            self.HS = dscr("hs" + tag, [NB * 256, D], BF16)
            self.YS = dscr("ys" + tag, [NB * 256, D], F32)
            self.dXA = [DR("xa") for _ in range(NTL)]
            self.dHT = [DR("ht") for _ in range(NTL)]
            self.dHS = [DR("hs") for _ in range(2 * NTL)]
            self.dYS = [DR("ys") for _ in range(2 * NB)]
            self.xa = sbx("xa_sb", [128, D])
            self.xn = sbx("xn_f", [128, D])
            self.hTf = sbx("hTf", [128, 8, 128])
            self.htk = sbx("htk", [128, D], BF16)
            self.wr = sbx("wr", [128, 8, 36])
            self.sm = sbx("sm", [128, 96])
            self.OH = sbx("OH", [128, NTL, 2, 32], BF16)
            self.ohs = sbx("ohs", [128, 32], BF16)
            self.rk = sbx("rk", [128, NTL, 2])
            self.wk = sbx("wk", [128, NTL, 2])
            self.dstf = sbx("dstf", [128, NTL, 2])
            self.dsti = sbx("dsti", [128, NTL, 2], I32)
            self.base = sbx("base", [128, 32])
            self.t32 = [sbx(f"t32{i}", [128, 32]) for i in range(3)]
            self.Ls = sbx("Ls", [128, 128], BF16)
            self.bst = sbx("bst", [128, NB])
            self.blke = sbx("blke", [128, NB])
            self.widx = sbx("widx", [128, NB], I32)
            self.pcol = sbx("pcol", [128, 1])
            self.PX = psx("PX", [128, D])
            self.PSg = [psx(f"PSg{i}", [128, 512]) for i in range(2)]
            self.PSu = [psx(f"PSu{i}", [128, 512]) for i in range(2)]
            self.PSh = psx("PSh", [128, 8, 128], BF16)
            self.PSh2 = psx("PSh2", [128, 8, 128], BF16)
            self.PSr = self.PSg[0]
            self.lcur = None
            kb.op("pool", lambda e: e.memset(self.Ls[:], 1.0), writes=[self.Ls.b])
            kb.op("pool", lambda e: e.affine_select(out=self.Ls[:], in_=self.Ls[:], pattern=[[1, 128]], compare_op=ALU.is_gt, fill=0.0,
                                                    base=0, channel_multiplier=-1), reads=[self.Ls.b], writes=[self.Ls.b])
            kb.op("pool", lambda e: e.memset(self.base[:], 0.0), writes=[self.base.b])
            kb.op("pool", lambda e: e.iota(self.bst[:], pattern=[[256, NB]], base=0, channel_multiplier=0, allow_small_or_imprecise_dtypes=True),
                  writes=[self.bst.b])
            kb.op("pool", lambda e: e.iota(self.pcol[:], pattern=[[0, 1]], base=0, channel_multiplier=1, allow_small_or_imprecise_dtypes=True),
                  writes=[self.pcol.b])

        def prep(self, l, t):
            if self.lcur != l:
                DMA("sp", self.wr[:], moe_wr[l].rearrange("(m p) n -> p m n", p=128), writes=[self.wr])
                self.lcur = l
            xa = self.xa
            DMA("pool", self.XA[rowsl(t), :], xa[:], reads=[xa], writes=[self.dXA[t]])
            sm = self.sm
            c1 = lambda i, w=1: sm.v((A_, slice(i, i + w)))
            ACT(self.xn, xa, AF.Square, accum=c1(0))
            ACT(c1(1), c1(0), AF.Ln, bias=1024e-6)
            ACT(c1(1), c1(1), AF.Exp, scale=-0.5)
            TS("dve", self.xn, xa, c1(1), None, ALU.mult)
            for m in range(8):
                TR(self.PX.v((A_, cs(m))), self.xn.v((A_, cs(m))), ident)
            ai = 2 + 4 * l
            for m in range(8):
                ACT(self.hTf.v((A_, m, A_)), self.PX.v((A_, cs(m))), AF.Identity,
                    scale=AB.v((A_, ai, slice(m, m + 1))), bias=AB.v((A_, ai + 1, slice(m, m + 1))))
            for m in range(8):
                TR(self.PX.v((A_, cs(m))), self.hTf.v((A_, m, A_)), ident)
            CP("act", self.htk, self.PX)
            DMA("pool", self.HTOK[rowsl(t), :], self.htk[:], reads=[self.htk], writes=[self.dHT[t]])
            for m in range(8):
                MM(self.PSr.v((A_, slice(0, 36))), self.hTf.v((A_, m, A_)), self.wr.v((A_, m, A_)), start=(m == 0), stop=(m == 7))
            lg = c1(8, 36)
            CP("act", lg, self.PSr.v((A_, slice(0, 36))))
            g4 = c1(8, 4)
            RED("dve", c1(2), g4, op=ALU.max)
            TS("dve", c1(3), c1(2), -1.0, None, ALU.mult)
            ACT(c1(44, 4), g4, AF.Exp, bias=c1(3), accum=c1(4))
            kb.op("dve", lambda e: e.reciprocal(out=_ap(c1(5)), in_=_ap(c1(4))), reads=[sm.b], writes=[sm.b])
            TS("dve", c1(48, 4), g4, c1(2), None, ALU.is_equal)
            sel = c1(52, 8)
            TS("dve", sel, c1(12, 8), c1(48), None, ALU.mult)
            for g in range(1, 4):
                STT("dve", sel, c1(12 + 8 * g, 8), c1(48 + g), sel, ALU.mult, ALU.add)
            RED("dve", c1(6), sel, op=ALU.max)
            TS("dve", c1(60, 8), sel, c1(6), None, ALU.is_equal)
            STT("dve", c1(68, 8), c1(60, 8), -1e30, sel, ALU.mult, ALU.add)
            RED("dve", c1(7), c1(68, 8), op=ALU.max)
            TS("dve", c1(76, 8), c1(68, 8), c1(7), None, ALU.is_equal)
            TT("dve", c1(84), c1(7), c1(6), ALU.subtract)
            ACT(c1(85), c1(84), AF.Exp)
            TS("dve", c1(86), c1(85), 1.0, None, ALU.add)
            kb.op("dve", lambda e: e.reciprocal(out=_ap(c1(87)), in_=_ap(c1(86))), reads=[sm.b], writes=[sm.b])
            TT("dve", self.wk.v((A_, t, slice(0, 1))), c1(87), c1(5), ALU.mult)
            TT("dve", self.wk.v((A_, t, slice(1, 2))), self.wk.v((A_, t, slice(0, 1))), c1(85), ALU.mult)
            for k, o8 in ((0, 60), (1, 76)):
                for g in range(4):
                    TS("dve", self.OH.v((A_, t, k, slice(8 * g, 8 * g + 8))), c1(o8, 8), c1(48 + g), None, ALU.mult)
            TT("dve", self.ohs, self.OH.v((A_, t, 0, A_)), self.OH.v((A_, t, 1, A_)), ALU.add)
            MM(self.PSr.v((A_, slice(0, 32))), self.Ls, self.ohs)
            TT("dve", self.t32[0], self.PSr.v((A_, slice(0, 32))), self.base, ALU.add)
            for k in range(2):
                TT("dve", self.t32[1], self.OH.v((A_, t, k, A_)), self.t32[0], ALU.mult)
                RED("dve", self.rk.v((A_, t, slice(k, k + 1))), self.t32[1])
            MM(self.PSr.v((A_, slice(32, 64))), onesb, self.ohs)
            TT("dve", self.base, self.PSr.v((A_, slice(32, 64))), self.base, ALU.add)

        def run(self, l, gfb, emit):
            NTL, NB = self.NTL, self.NB
            t32 = self.t32
            with ExitStack() as esq:
                cq = T_(esq.enter_context(nc.sbuf_tensor("cq" + self.tag, [128, 32, NB], F32)), "cq")
                kb.op("dve", lambda e: e.tensor_tensor(out=cq[:], in0=self.bst[:].unsqueeze(1).broadcast_to([128, 32, NB]),
                                                       in1=self.base[:].unsqueeze(2).broadcast_to([128, 32, NB]), op=ALU.is_lt),
                      reads=[self.bst.b, self.base.b], writes=[cq.b])
                RED("dve", t32[0], cq)
            kb.barrier()
            TS("dve", t32[0], t32[0], 256.0, None, ALU.mult)
            CP("dve", t32[1], t32[0])
            a, b_ = t32[1], t32[2]
            for sh in (1, 2, 4, 8, 16):
                CP("dve", b_, a)
                TT("dve", b_.v((A_, slice(sh, 32))), a.v((A_, slice(sh, 32))), a.v((A_, slice(0, 32 - sh))), ALU.add)
                a, b_ = b_, a
            pend = a
            pstart = b_
            TT("dve", pstart, pend, t32[0], ALU.subtract)
            with ExitStack() as esr:
                def sbr(name, shape, dt=F32):
                    return T_(esr.enter_context(nc.sbuf_tensor(name + self.tag, list(shape), dt)), name)
                with ExitStack() as esc:
                    cmp_ = T_(esc.enter_context(nc.sbuf_tensor("cmp" + self.tag, [128, NB, 32], F32)), "cmp")
                    kb.op("dve", lambda e: e.tensor_tensor(out=cmp_[:], in0=pend[:].unsqueeze(1).broadcast_to([128, NB, 32]),
                                                           in1=self.bst[:].unsqueeze(2).broadcast_to([128, NB, 32]), op=ALU.is_le),
                          reads=[pend.b, self.bst.b], writes=[cmp_.b])
                    RED("dve", self.blke, cmp_)
                kb.barrier()
                TS("dve", self.blke, self.blke, 31.0, 128.0, ALU.min, ALU.mult)
                TS("dve", self.blke, self.blke, self.pcol, float(l * NEXP * 128), ALU.add, ALU.add)
                CP("dve", self.widx, self.blke)
                with ExitStack() as ess:
                    def sbs(name, shape, dt=F32):
                        return T_(ess.enter_context(nc.sbuf_tensor(name + self.tag, list(shape), dt)), name)
                    hrow = [sbs(f"hrow{i}", [128, D], BF16) for i in range(2)]
                    zt = sbs("zt", [128, D], BF16)
                    kb.op("pool", lambda e: e.memset(zt[:], 0.0), writes=[zt.b])
                    dz = [DR("hsz") for _ in range(4)]
                    ntz = NB * 2
                    bnd = [ntz * qq // 4 for qq in range(5)]
                    for qq in range(4):
                        DMA("sp", self.HS[bnd[qq] * 128:bnd[qq + 1] * 128, :].rearrange("(c p) d -> p c d", p=128),
                            zt[:].unsqueeze(1).broadcast_to([128, bnd[qq + 1] - bnd[qq], D]), reads=[zt], writes=[dz[qq]])
                    zb_ = [d.b for d in dz]
                    for t in range(NTL):
                        for k in range(2):
                            TT("dve", t32[0], self.OH.v((A_, t, k, A_)), pstart, ALU.mult)
                            RED("dve", self.dstf.v((A_, t, slice(k, k + 1))), t32[0])
                        TT("dve", self.dstf.v((A_, t, A_)), self.dstf.v((A_, t, A_)), self.rk.v((A_, t, A_)), ALU.add)
                        CP("dve", self.dsti.v((A_, t, A_)), self.dstf.v((A_, t, A_)))
                        hr = hrow[t % 2]
                        DMA("sp", hr[:], self.HTOK[rowsl(t), :], reads=[self.dHT[t]], writes=[hr])
                        for k in range(2):
                            self._ind("scatter", self.HS, self.dsti.t[:, t, k:k + 1], hr, reads=[hr.b, self.dsti.b] + zb_, writes=[self.dHS[2 * t + k].b])
                kb.barrier()
                stg = [[sbr(f"stg{i}_{j}", [128, 4096]) for j in range(3)] for i in range(2)]
                Wb = [[sbr(f"Wb{i}_{j}", [128, 4096], BF16) for j in range(3)] for i in range(2)]
                hsb = [sbr(f"hsb{i}", [128, D], BF16) for i in range(2)]
                hT = [sbr(f"hTx{i}", [128, 8, 128], BF16) for i in range(2)]
                sg = sbr("sg", [128, 512])
                hb = [sbr(f"hb{i}", [128, 512], BF16) for i in range(2)]
                hT2 = [sbr(f"hT2{i}", [128, 4, 128], BF16) for i in range(2)]
                hfl = T_(_APWrap(self.hTf.t[:].rearrange("p m n -> p (m n)")), "hTf_flatr")
                hfl.b = self.hTf.b
                ysb = [self.xn, hfl]
                if os.environ.get("SBDBG"):
                    print("SBUF free in run", self.tag, nc.sbuf_bytes_remaining)
                allhs = [d.b for d in self.dHS]
                wsrc = [w_.rearrange("l r f -> (l r) f") for w_ in (moe_wg, moe_wu, moe_wd)]

                def wgather(blk):
                    for j in range(3):
                        st = stg[blk % 2][j]
                        self._ind("gather", wsrc[j], self.widx.t[:, blk:blk + 1], st, reads=[self.widx.b], writes=[st.b])

                def wcast(blk):
                    for j in range(3):
                        st = stg[blk % 2][j]
                        for q4 in range(4):
                            sl = slice(q4 * 1024, (q4 + 1) * 1024)
                            CP("act" if (j * 4 + q4) % 2 == 0 else "dve", Wb[blk % 2][j].v((A_, sl)), st.v((A_, sl)))

                def front(u):
                    blk, sub = divmod(u, 2)
                    j = u % 2
                    r0 = blk * 256 + sub * 128
                    Wg_ = View(Wb[blk % 2][0].t[:].rearrange("p (m f) -> p m f", f=512), Wb[blk % 2][0].b)
                    Wu_ = View(Wb[blk % 2][1].t[:].rearrange("p (m f) -> p m f", f=512), Wb[blk % 2][1].b)
                    kb.dma("sp", hsb[j][:], self.HS[r0:r0 + 128, :], reads=allhs, writes=[hsb[j].b])
                    for m in range(8):
                        TR(self.PSh.v((A_, m, A_)), hsb[j].v((A_, cs(m))), identb)
                    CP("act", hT[j], self.PSh)
                    for mk in range(8):
                        MM(self.PSg[j], hT[j].v((A_, mk, A_)), View(Wg_.ap[:, mk, :], Wg_.b), start=(mk == 0), stop=(mk == 7))
                    for mk in range(8):
                        MM(self.PSu[j], hT[j].v((A_, mk, A_)), View(Wu_.ap[:, mk, :], Wu_.b), start=(mk == 0), stop=(mk == 7))
                    ACT(sg, self.PSg[j], AF.Silu)
                    TT("dve", hb[j], self.PSu[j], sg, ALU.mult)

                def back(u):
                    blk, sub = divmod(u, 2)
                    j = u % 2
                    r0 = blk * 256 + sub * 128
                    Wd_ = View(Wb[blk % 2][2].t[:].rearrange("p (m f) -> p m f", f=D), Wb[blk % 2][2].b)
                    for fk in range(4):
                        TR(self.PSh2.v((A_, fk, A_)), hb[j].v((A_, cs(fk))), identb)
                    CP("act", hT2[j], self.PSh2.v((A_, slice(0, 4), A_)))
                    for h in range(2):
                        for fk in range(4):
                            MM(self.PX.v((A_, hs(h))), hT2[j].v((A_, fk, A_)), View(Wd_.ap[:, fk, hs(h)], Wd_.b), start=(fk == 0), stop=(fk == 3))
                    CP("dve", ysb[j], self.PX)
                    DMA("sp", self.YS[r0:r0 + 128, :], ysb[j][:], reads=[ysb[j]], writes=[self.dYS[u]])
                wgather(0)
                if NB > 1:
                    wgather(1)
                wcast(0)
                if NB > 2:
                    wgather(2)
                front(0)
                for u in range(2 * NB):
                    blk, sub = divmod(u, 2)
                    if sub == 0 and blk + 1 < NB:
                        wcast(blk + 1)
                        if blk + 3 < NB:
                            wgather(blk + 3)
                    if u + 1 < 2 * NB:
                        front(u + 1)
                    back(u)
                allys = [d.b for d in self.dYS]
                for t in range(NTL):
                    y1, y2 = stg[0][0], stg[0][1]
                    y1v, y2v = y1.v((A_, slice(0, D))), y2.v((A_, slice(0, D)))
                    self._ind("gather", self.YS, self.dsti.t[:, t, 0:1], y1v, reads=allys + [self.dsti.b], writes=[y1.b])
                    self._ind("gather", self.YS, self.dsti.t[:, t, 1:2], y2v, reads=allys + [self.dsti.b], writes=[y2.b])
                    DMA("sp", self.xa[:], self.XA[rowsl(t), :], reads=[self.dXA[t]], writes=[self.xa])
                    TS("dve", y1v, y1v, self.wk.v((A_, t, slice(0, 1))), None, ALU.mult)
                    STT("dve", y1v, y2v, self.wk.v((A_, t, slice(1, 2))), y1v, ALU.mult, ALU.add)
                    TT("dve", y1v, y1v, gfb, ALU.mult)
                    TT("dve", self.xa, self.xa, y1v, ALU.add)
                    emit(t, self.xa)

        def _ind(self, kind, dram, idx_ap, sb_, reads, writes):
            q = "pool"
            keys, i = kb.dma_pool[q]
            key = keys[i % len(keys)]
            kb.dma_pool[q][1] = i + 1
            if kb.cnt[key] > 0:
                kb._wait(q, (key, kb.cnt[key]))
            for b in reads:
                kb._wait(q, b.w)
            for b in writes:
                kb._wait(q, b.w)
                for k_, v_ in b.r.items():
                    kb._wait(q, (k_, v_))
            off = bass.IndirectOffsetOnAxis(ap=idx_ap, axis=0)
            if kind == "gather":
                inst = nc.gpsimd.indirect_dma_start(out=_ap(sb_), out_offset=None, in_=dram, in_offset=off)
            else:
                inst = nc.gpsimd.indirect_dma_start(out=dram, out_offset=off, in_=_ap(sb_), in_offset=None)
            kb.cnt[key] += 16
            inst.then_inc(kb.sems[key], 16)
            kb._mark((key, kb.cnt[key]), reads, writes)
            kb.n_inst += 1


    class FFN:
        def __init__(self, es_, tag=""):
            def sbx(name, shape, dt=F32):
                return T_(es_.enter_context(nc.sbuf_tensor(name + tag, list(shape), dt)), name)

            def psx(name, shape, dt=F32):
                return T_(es_.enter_context(nc.psum_tensor(name + tag, list(shape), dt)), name)
            self.acc = sbx("acc", [128, GT, D])
            self.hTb = sbx("hTb", [128, GT, 8, 128], BF16)
            self.gw = sbx("gw", [128, GT, 32])
            self.hTf = sbx("hTf", [128, 8, 128])
            self.xn = sbx("xn_f", [128, D])
            self.wr = sbx("wr", [128, 8, 36])
            self.sm = sbx("sm", [128, 96])
            self.stg = [sbx(f"stg{i}", [128, 4096]) for i in range(2)]
            self.Wg = [sbx(f"Wgb{i}", [128, 8, 512], BF16) for i in range(2)]
            self.Wu = [sbx(f"Wub{i}", [128, 8, 512], BF16) for i in range(2)]
            self.Wd = [sbx(f"Wdb{i}", [128, 4, D], BF16) for i in range(2)]
            self.sg = [sbx("sg0", [128, 512])] * 2
            self.hb = [sbx(f"hb{i}", [128, 512], BF16) for i in range(2)]
            self.hT2 = [sbx(f"hT2{i}", [128, 4, 128], BF16) for i in range(2)]
            self.PX = psx("PX", [128, D])
            self.PSg = [psx(f"PSg{i}", [128, 512]) for i in range(2)]
            self.PSu = [psx(f"PSu{i}", [128, 512]) for i in range(2)]
            self.PSh = psx("PSh", [128, 8, 128], BF16)
            self.PSr = psx("PSr", [128, 64])
            self.lcur = None

        def prep(self, l, t):
            if self.lcur != l:
                DMA("sp", self.wr[:], moe_wr[l].rearrange("(m p) n -> p m n", p=128), writes=[self.wr])
                self.lcur = l
            xa = self.acc.v((A_, t, A_))
            sm = self.sm
            c1 = lambda i, w=1: sm.v((A_, slice(i, i + w)))
            ACT(self.xn, xa, AF.Square, accum=c1(0))
            ACT(c1(1), c1(0), AF.Ln, bias=1024e-6)
            ACT(c1(1), c1(1), AF.Exp, scale=-0.5)
            TS("dve", self.xn, xa, c1(1), None, ALU.mult)
            for m in range(8):
                TR(self.PX.v((A_, cs(m))), self.xn.v((A_, cs(m))), ident)
            ai = 2 + 4 * l
            for m in range(8):
                ACT(self.hTf.v((A_, m, A_)), self.PX.v((A_, cs(m))), AF.Identity,
                    scale=AB.v((A_, ai, slice(m, m + 1))), bias=AB.v((A_, ai + 1, slice(m, m + 1))))
            CP("pool", self.hTb.v((A_, t, A_, A_)), self.hTf)
            for m in range(8):
                MM(self.PSr.v((A_, slice(0, 36))), self.hTf.v((A_, m, A_)), self.wr.v((A_, m, A_)), start=(m == 0), stop=(m == 7))
            lg = c1(8, 36)
            CP("act", lg, self.PSr.v((A_, slice(0, 36))))
            g4 = c1(8, 4)
            RED("dve", c1(2), g4, op=ALU.max)
            TS("dve", c1(3), c1(2), -1.0, None, ALU.mult)
            ACT(c1(44, 4), g4, AF.Exp, bias=c1(3), accum=c1(4))
            kb.op("dve", lambda e: e.reciprocal(out=_ap(c1(5)), in_=_ap(c1(4))), reads=[sm.b], writes=[sm.b])
            TS("dve", c1(48, 4), g4, c1(2), None, ALU.is_equal)
            sel = c1(52, 8)
            TS("dve", sel, c1(12, 8), c1(48), None, ALU.mult)
            for g in range(1, 4):
                STT("dve", sel, c1(12 + 8 * g, 8), c1(48 + g), sel, ALU.mult, ALU.add)
            RED("dve", c1(6), sel, op=ALU.max)
            TS("dve", c1(60, 8), sel, c1(6), None, ALU.is_equal)
            STT("dve", c1(68, 8), c1(60, 8), -1e30, sel, ALU.mult, ALU.add)
            RED("dve", c1(7), c1(68, 8), op=ALU.max)
            TS("dve", c1(76, 8), c1(68, 8), c1(7), None, ALU.is_equal)
            TT("dve", c1(84), c1(7), c1(6), ALU.subtract)
            ACT(c1(85), c1(84), AF.Exp)
            TS("dve", c1(86), c1(85), 1.0, None, ALU.add)
            kb.op("dve", lambda e: e.reciprocal(out=_ap(c1(87)), in_=_ap(c1(86))), reads=[sm.b], writes=[sm.b])
            TT("dve", c1(87), c1(87), c1(5), ALU.mult)
            TT("dve", c1(88), c1(87), c1(85), ALU.mult)
            TS("dve", c1(60, 8), c1(60, 8), c1(87), None, ALU.mult)
            STT("dve", c1(60, 8), c1(76, 8), c1(88), c1(60, 8), ALU.mult, ALU.add)
            for g in range(4):
                TS("dve", self.gw.v((A_, t, slice(8 * g, 8 * g + 8))), c1(60, 8), c1(48 + g), None, ALU.mult)

        def experts(self, l, ntile, gfb):
            engs = ["act", "dve", "pool"]
            n = 0
            for e in range(NEXP):
                i = e % 2
                for (dst, src) in ((self.Wg[i], moe_wg), (self.Wu[i], moe_wu)):
                    st = self.stg[n % 2]
                    DMA("sp" if n % 2 == 0 else "pool", st[:].rearrange("p (m f) -> p m f", f=512), src[l, e].rearrange("(m p) f -> p m f", p=128), writes=[st])
                    CP(engs[n % 3], dst, View(st.t[:].rearrange("p (m f) -> p m f", f=512), st.b))
                    n += 1
                st = self.stg[n % 2]
                DMA("sp" if n % 2 == 0 else "pool", st[:].rearrange("p (m f) -> p m f", f=D), moe_wd[l, e].rearrange("(m p) f -> p m f", p=128), writes=[st])
                for fk in range(4):
                    TT("pool" if fk % 2 else "dve", self.Wd[i].v((A_, fk, A_)), st.v((A_, slice(fk * D, (fk + 1) * D))), gfb, ALU.mult)
                n += 1
                for t in range(ntile):
                    j = t % 2
                    for mk in range(8):
                        MM(self.PSg[j], self.hTb.v((A_, t, mk, A_)), self.Wg[i].v((A_, mk, A_)), start=(mk == 0), stop=(mk == 7))
                    for mk in range(8):
                        MM(self.PSu[j], self.hTb.v((A_, t, mk, A_)), self.Wu[i].v((A_, mk, A_)), start=(mk == 0), stop=(mk == 7))
                    ACT(self.sg[j], self.PSg[j], AF.Silu)
                    STT("dve", self.hb[j], self.PSu[j], self.gw.v((A_, t, slice(e, e + 1))), self.sg[j], ALU.mult, ALU.mult)
                    for fk in range(4):
                        TR(self.PSh.v((A_, fk, A_)), self.hb[j].v((A_, cs(fk))), identb)
                    CP("act", self.hT2[j], self.PSh.v((A_, slice(0, 4), A_)))
                    for h in range(2):
                        for fk in range(4):
                            MM(self.PX.v((A_, hs(h))), self.hT2[j].v((A_, fk, A_)), self.Wd[i].v((A_, fk, hs(h))), start=(fk == 0), stop=(fk == 3))
                    TT("dve", self.acc.v((A_, t, A_)), self.acc.v((A_, t, A_)), self.PX, ALU.add)

    kb.barrier()
    if has("p3"):
        with ExitStack() as es3:
            ffn = FFN2(es3, "_l0", NT)
            with ExitStack() as es3a:
                def sb3(name, shape, dt=F32):
                    return T_(es3a.enter_context(nc.sbuf_tensor(name, list(shape), dt)), name)
                Wo = sb3("Wo", [128, 8, D], BF16)
                gng, gnb = sb3("gng", [128, D]), sb3("gnb", [128, D])
                G3 = [sb3(f"G3{i}", [128, D]) for i in range(3)] + [T_(_APWrap(ffn.hTf.t[:].rearrange("p m n -> p (m n)")), "hTf_flat"), ffn.xn]
                G3[3].b = ffn.hTf.b
                zb = sb3("zb", [128, D], BF16)
                zT = sb3("zT", [128, 8, 128], BF16)
                st16 = sb3("st16", [128, 48])
                PZ = ffn.PSh
                vh = lambda t: View(t.t[:].rearrange("p (h n) -> p h n", n=64), t.b)
                bch = lambda v_: View(v_.ap.unsqueeze(2).broadcast_to([128, 16, 64]), v_.b)
                s16 = lambda i: st16.v((A_, slice(16 * i, 16 * i + 16)))
                for m in range(8):
                    st = G3[m % 2]
                    DMA("sp", st[:], rw_wo[cs(m), :], writes=[st])
                    CP("act" if m % 2 else "dve", Wo.v((A_, m, A_)), st)
                bc_row(gng, vec_row[VR_GNG:VR_GNG + 1, :])
                bc_row(gnb, vec_row[VR_GNB:VR_GNB + 1, :])
                for tt in range(NT):
                    yv, gv, bv, xv, wk = G3
                    ydeps = [dY[2 * tt], dY[2 * tt + 1]] if has("p2") else []
                    DMA("sp", yv[:], YSC[rowsl(tt), :], reads=ydeps, writes=[yv])
                    DMA("sp", gv[:], GSC[rowsl(tt), :], reads=[dG[tt]], writes=[gv])
                    DMA("sp", bv[:], BON[rowsl(tt), :], reads=[dBON[tt]], writes=[bv])
                    DMA("sp", xv[:], x_in[rowsl(tt), :], writes=[xv])
                    RED("dve", s16(0), vh(yv))
                    TS("dve", s16(0), s16(0), -1.0 / 64, None, ALU.mult)
                    TT("dve", vh(yv), vh(yv), bch(s16(0)), ALU.add)
                    TT("pool", wk, yv, yv, ALU.mult)
                    RED("dve", s16(1), vh(wk))
                    ACT(s16(1), s16(1), AF.Ln, scale=1.0 / 64, bias=64e-5)
                    ACT(s16(1), s16(1), AF.Exp, scale=-0.5)
                    TT("dve", vh(yv), vh(yv), bch(s16(1)), ALU.mult)
                    TT("pool", yv, yv, gng, ALU.mult)
                    TT("pool", yv, yv, gnb, ALU.add)
                    TT("dve", yv, yv, bv, ALU.add)
                    TT("dve", zb, yv, gv, ALU.mult)
                    for m in range(8):
                        TR(PZ.v((A_, m, A_)), zb.v((A_, cs(m))), identb)
                    CP("act", zT, PZ)
                    for h in range(2):
                        for mk in range(8):
                            MM(ffn.PX.v((A_, hs(h))), zT.v((A_, mk, A_)), Wo.v((A_, mk, hs(h))), start=(mk == 0), stop=(mk == 7))
                    TT("dve", wk, ffn.PX, gmb[0], ALU.mult)
                    TT("dve", ffn.xa, wk, xv, ALU.add)
                    ffn.prep(0, tt)
            kb.barrier()

            def emit0(t, xa_):
                DMA("sp", X1[rowsl(t), :], xa_[:], reads=[xa_], writes=[dX1[t]])
            ffn.run(0, gmb[1], emit0)
    if dbg == 3:
        allb = [d.b for d in dX1]
        kb.finish("sp", allb)
        kb.finish("pool", allb)
        kb.close()
        es.close()
        return nc, in_names, dbg_outs

    NO = NT // 2
    KTS = dscr("kts", [16, 64, T], BF16)
    VS = dscr("vs", [T, D], BF16)
    QTS = dscr("qts", [16, 64, NO * 128], BF16)
    XO = dscr("xo", [NO * 128, D], F32)
    OS = dscr("os", [NO * 128, D], BF16)
    dKT = [DR(f"kt{t}") for t in range(NT)]
    dVS = [DR(f"vs{t}") for t in range(NT)]
    dQT = [DR(f"qt{t}") for t in range(NO)]
    dXO = [DR(f"xo{t}") for t in range(NO)]
    dOS = [DR(f"os{t}") for t in range(NO)]
    dOUT = [DR(f"out{t}") for t in range(NO)]
    LAM_INIT = 0.8 - 0.6 * math.exp(-0.3 * 1)
    kb.barrier()
    if has("p5"):
        with ExitStack() as es4:
            def sb4(name, shape, dt=F32):
                return T_(es4.enter_context(nc.sbuf_tensor(name, list(shape), dt)), name)

            def ps4(name, shape, dt=F32):
                return T_(es4.enter_context(nc.psum_tensor(name, list(shape), dt)), name)
            Wkv = sb4("Wkv", [128, 8, 2 * D], BF16)
            Wq = sb4("Wq", [128, 8, D], BF16)
            stg = [sb4(f"stg4{i}", [128, 2 * D]) for i in range(2)]
            xt4 = [sb4(f"xt4{i}", [128, D]) for i in range(3)]
            hk = sb4("hk", [128, 8, 128], BF16)
            kts = sb4("kts_sb", [64, 16, 128], BF16)
            vsb = sb4("vsb", [128, D], BF16)
            sm4 = sb4("sm4", [128, 32])
            selc = sb4("selc", [128, 4])
            cmk = sb4("cmk", [128, 2, 128], BF16)
            cmf = sb4("cmf", [128, 2, 128])
            lamt = sb4("lamt", [128, 8])
            subg = sb4("subg", [128, 128])
            KTh = sb4("KTh", [128, 2, T], BF16)
            Vh = sb4("Vh", [128, NT, 129], BF16)
            QTh = sb4("QTh", [128, 2, NO * 128], BF16)
            mxs = [sb4(f"mx{i}", [128, 16]) for i in range(4)]
            negm65 = sb4("negm65", [128, 4, 65])
            rowbuf = [Buf(f"row{i}") for i in range(4)]
            osq = sb4("osq", [128, 128], BF16)
            pk_slot = [Buf("pk0"), Buf("pk1")]
            psn_slot = [Buf("psn0"), Buf("psn1")]
            pst_slot = [Buf(f"pst{i}") for i in range(4)]
            Pt = [sb4(f"Pt{i}", [128, 2, 128], BF16) for i in range(3)]
            ot = sb4("ot", [128, 128])
            ob = sb4("ob", [128, 128], BF16)
            PXa = ps4("PXa", [128, D])
            PKs = ps4("PKs", [128, D])
            PSt = ps4("PSt4", [128, 4, 128])
            PSo = [ps4(f"PSo{i}", [128, 512]) for i in range(2)]
            PSn = ps4("PSn", [128, 512])
            c4 = lambda i, w=1: sm4.v((A_, slice(i, i + w)))
            PK = View(PKs.t[0:64, :].rearrange("p (s t) -> p s t", t=128), PKs.b)

            n = 0
            for m in range(8):
                st = stg[n % 2]
                DMA("sp" if n % 2 == 0 else "pool", st[:], w_kv[cs(m), :], writes=[st])
                CP(["act", "dve", "pool"][n % 3], Wkv.v((A_, m, A_)), st)
                n += 1
            for m in range(8):
                st = stg[n % 2]
                DMA("sp" if n % 2 == 0 else "pool", st[:, 0:D], df_wq[cs(m), :], writes=[st])
                CP(["act", "dve", "pool"][n % 3], Wq.v((A_, m, A_)), st.v((A_, slice(0, D))))
                n += 1
            DMA("sp", selc[:], sel_in[:, :], writes=[selc])
            DMA("sp", cmf[:], cmask_in.rearrange("c k q -> k c q"), writes=[cmf])
            CP("dve", cmk, cmf)
            DMA("sp", lamt[:, 0:4], lam_in[:, :], writes=[lamt])
            bc_row(subg, vec_row[VR_SUBLN:VR_SUBLN + 1, 0:128])
            TS("dve", subg, subg, 1.0 - LAM_INIT, None, ALU.mult)
            TT("dve", lamt.v((A_, slice(4, 5))), lamt.v((A_, slice(0, 1))), lamt.v((A_, slice(1, 2))), ALU.mult)
            TT("dve", lamt.v((A_, slice(5, 6))), lamt.v((A_, slice(2, 3))), lamt.v((A_, slice(3, 4))), ALU.mult)
            MM(PSn.v((A_, slice(0, 2))), ones, lamt.v((A_, slice(4, 6))))
            ACT(lamt.v((A_, slice(6, 8))), PSn.v((A_, slice(0, 2))), AF.Exp)
            TT("dve", lamt.v((A_, slice(4, 5))), lamt.v((A_, slice(7, 8))), lamt.v((A_, slice(6, 7))), ALU.subtract)
            TS("dve", lamt.v((A_, slice(4, 5))), lamt.v((A_, slice(4, 5))), -LAM_INIT, None, ALU.add)
            neglam = lamt.v((A_, slice(4, 5)))

            def norm_T(xv, ai, dst):
                ACT(xt4[2], xv, AF.Square, accum=c4(0))
                ACT(c4(1), c4(0), AF.Ln, bias=1024e-6)
                ACT(c4(1), c4(1), AF.Exp, scale=-0.5)
                TS("dve", xt4[2], xv, c4(1), None, ALU.mult)
                for m in range(8):
                    TR(PXa.v((A_, cs(m))), xt4[2].v((A_, cs(m))), ident)
                for m in range(8):
                    ACT(dst.v((A_, m, A_)), PXa.v((A_, cs(m))), AF.Identity,
                        scale=AB.v((A_, ai, slice(m, m + 1))), bias=AB.v((A_, ai + 1, slice(m, m + 1))))

            def proj64(W, coff, scale, dram, col0):
                for half in range(2):
                    for s8 in range(8):
                        sl = half * 8 + s8
                        for mk in range(8):
                            MM(View(PK.ap[:, s8, :], PK.b), W.v((A_, mk, slice(coff + sl * 64, coff + sl * 64 + 64))), hk.v((A_, mk, A_)),
                               start=(mk == 0), stop=(mk == 7))
                    ACT(kts.v((A_, slice(half * 8, half * 8 + 8), A_)), PK, AF.Copy, scale=scale)

            for tt in range(NT):
                DMA("sp", xt4[0][:], X1[rowsl(tt), :], reads=[dX1[tt]], writes=[xt4[0]])
                norm_T(xt4[0], 8, hk)
                proj64(Wkv, 0, 1.0, KTS, tt * 128)
                DMA("pool", KTS[:, :, tt * 128:(tt + 1) * 128].rearrange("s d t -> d s t"), kts[:], reads=[kts], writes=[dKT[tt]])
                for h in range(2):
                    for mk in range(8):
                        MM(PXa.v((A_, hs(h))), hk.v((A_, mk, A_)), Wkv.v((A_, mk, slice(D + h * 512, D + (h + 1) * 512))), start=(mk == 0), stop=(mk == 7))
                CP("dve", vsb, PXa)
                DMA("pool", VS[rowsl(tt), :], vsb[:], reads=[vsb], writes=[dVS[tt]])
            for i in range(NO):
                DMA("sp", xt4[0][:], X1[rowsl(2 * i), :], reads=[dX1[2 * i]], writes=[xt4[0]])
                DMA("sp", xt4[1][:], X1[rowsl(2 * i + 1), :], reads=[dX1[2 * i + 1]], writes=[xt4[1]])
                TS("dve", xt4[0], xt4[0], selc.v((A_, slice(0, 1))), None, ALU.mult)
                STT("dve", xt4[0], xt4[1], selc.v((A_, slice(1, 2))), xt4[0], ALU.mult, ALU.add)
                DMA("pool", XO[rowsl(i), :], xt4[0][:], reads=[xt4[0]], writes=[dXO[i]])
                norm_T(xt4[0], 4, hk)
                proj64(Wq, 0, 0.125, QTS, i * 128)
                DMA("pool", QTS[:, :, i * 128:(i + 1) * 128].rearrange("s d t -> d s t"), kts[:], reads=[kts], writes=[dQT[i]])
            kb.barrier()
            kb.op("pool", lambda e: e.memset(Vh[:, :, 128:129], 1.0), writes=[Vh.b])
            kb.op("pool", lambda e: e.memset(QTh[:], 0.0), writes=[QTh.b])
            kb.op("pool", lambda e: e.memset(KTh[:], 0.0), writes=[KTh.b])
            kb.op("pool", lambda e: e.memset(KTh[64:65, :, :], 1.0), writes=[KTh.b])
            kb.op("pool", lambda e: e.memset(negm65[:], 0.0), writes=[negm65.b])
            for hd in range(8):
                for c in range(2):
                    DMA("sp", KTh[0:64, c, :], KTS[2 * hd + c], reads=dKT, writes=[KTh])
                    DMA("sp", QTh[0:64, c, :], QTS[2 * hd + c], reads=dQT, writes=[QTh])
                DMA("sp", Vh[:, :, 0:128], VS[:, hd * 128:(hd + 1) * 128].rearrange("(n p) e -> p n e", p=128), reads=dVS, writes=[Vh])
                pst_views = [View(PSt.t[:, 0:2, :], pst_slot[0]), View(PXa.t[:, 0:256].rearrange("p (c q) -> p c q", c=2), pst_slot[1]),
                             View(PXa.t[:, 512:768].rearrange("p (c q) -> p c q", c=2), pst_slot[2])]

                def pass1(i):
                    par = i % 2
                    nk = 2 * i + 2
                    qs = slice(i * 128, (i + 1) * 128)
                    nch = (nk * 128 + 511) // 512
                    for c in range(2):
                        mxt = mxs[2 * par + c]
                        for kc in range(nch):
                            w = min(512, nk * 128 - kc * 512)
                            pss = View(PKs.t[:, (kc % 2) * 512:(kc % 2) * 512 + w], pk_slot[kc % 2])
                            MM(pss, QTh.v((slice(0, 64), c, qs)), KTh.v((slice(0, 64), c, slice(kc * 512, kc * 512 + w))))
                            RED("dve", mxt.v((A_, slice(kc, kc + 1))), pss, op=ALU.max)
                        a = 8 + 4 * par + 2 * c
                        RED("dve", c4(a), mxt.v((A_, slice(0, nch))), op=ALU.max)
                        TS("dve", negm65.v((A_, 2 * par + c, slice(64, 65))), c4(a), -1.0, None, ALU.mult)
                        psn = View(PSn.t[0:65, 0:128], PSn.b)
                        TR(psn, negm65.v((A_, 2 * par + c, A_)), ident)
                        rb = rowbuf[2 * par + c]
                        kb.op("act", lambda en, c=c, qs=qs: en.copy(out=QTh[64:65, c, qs], in_=PSn[64:65, 0:128]), reads=[PSn.b], writes=[rb])

                def pass2(i):
                    par = i % 2
                    nk = 2 * i + 2
                    qs = slice(i * 128, (i + 1) * 128)

                    def score(kt):
                        pst = pst_views[kt % 3]
                        for c in range(2):
                            kb.op("pe", lambda en, c=c, kt=kt, pst=pst: en.matmul(pst.ap[:, c, :], lhsT=KTh[:, c, kt * 128:(kt + 1) * 128],
                                                                               rhs=QTh[:, c, qs], start=True, stop=True),
                                  reads=[KTh.b, QTh.b, rowbuf[2 * par + c]], writes=[pst.b])
                    for kt in range(min(2, nk)):
                        score(kt)
                    for kt in range(nk):
                        if kt + 2 < nk:
                            score(kt + 2)
                        p_ = Pt[kt % 3]
                        ACT(p_, pst_views[kt % 3], AF.Exp)
                        if kt >= nk - 2:
                            kb.op("dve", lambda e, p_=p_, kt=kt: e.tensor_tensor(
                                out=p_[:], in0=p_[:], in1=cmk[:, kt - (nk - 2):kt - (nk - 2) + 1, :].broadcast_to([128, 2, 128]), op=ALU.mult),
                                reads=[p_.b, cmk.b], writes=[p_.b])
                        for c in range(2):
                            MM(PSo[c].v((A_, slice(0, 129))), p_.v((A_, c, A_)), Vh.v((A_, kt, A_)), start=(kt == 0), stop=(kt == nk - 1))
                    kb.op("dve", lambda e: e.reciprocal(out=_ap(c4(4)), in_=PSo[0][:, 128:129]), reads=[PSo[0].b], writes=[sm4.b])
                    kb.op("dve", lambda e: e.reciprocal(out=_ap(c4(5)), in_=PSo[1][:, 128:129]), reads=[PSo[1].b], writes=[sm4.b])
                    TT("dve", c4(5), c4(5), neglam, ALU.mult)
                    TS("dve", ot, PSo[0].v((A_, slice(0, 128))), c4(4), None, ALU.mult)
                    STT("dve", ot, PSo[1].v((A_, slice(0, 128))), c4(5), ot, ALU.mult, ALU.add)
                    ACT(osq, ot, AF.Square, accum=c4(6))
                    ACT(c4(7), c4(6), AF.Ln, scale=1.0 / 128, bias=1e-5)
                    ACT(c4(7), c4(7), AF.Exp, scale=-0.5)
                    STT("dve", ob, ot, c4(7), subg, ALU.mult, ALU.mult)
                    DMA("pool", OS[rowsl(i), hd * 128:(hd + 1) * 128], ob[:], reads=[ob], writes=[dOS[i]])
                pass1(0)
                for i in range(NO):
                    if i + 1 < NO:
                        pass1(i + 1)
                    pass2(i)
        kb.barrier()
        with ExitStack() as es5:
            def sb5(name, shape, dt=F32):
                return T_(es5.enter_context(nc.sbuf_tensor(name, list(shape), dt)), name)
            ffn = FFN2(es5, "_l1", NO)
            fg = sb5("fg", [128, D])
            s5 = sb5("s5", [128, 4])
            hflat = T_(_APWrap(ffn.hTf.t[:].rearrange("p m n -> p (m n)")), "hTf_flat5")
            hflat.b = ffn.hTf.b
            bc_row(fg, vec_row[VR_FING:VR_FING + 1, :])
            TS("dve", fg, fg, 32.0, None, ALU.mult)
            with ExitStack() as es5a:
                def sb5a(name, shape, dt=F32):
                    return T_(es5a.enter_context(nc.sbuf_tensor(name, list(shape), dt)), name)
                Wo1 = sb5a("Wo1", [128, 8, D], BF16)
                x5 = [sb5a(f"x5{i}", [128, D]) for i in range(2)]
                osb = sb5a("osb", [128, D], BF16)
                oT = sb5a("oT", [128, 8, 128], BF16)
                for m in range(8):
                    st = x5[m % 2]
                    DMA("sp", st[:], df_wo[cs(m), :], writes=[st])
                    CP("act" if m % 2 else "dve", Wo1.v((A_, m, A_)), st)
                for i in range(NO):
                    DMA("sp", osb[:], OS[rowsl(i), :], reads=[dOS[i]], writes=[osb])
                    DMA("sp", x5[0][:], XO[rowsl(i), :], reads=[dXO[i]], writes=[x5[0]])
                    for m in range(8):
                        TR(ffn.PSh.v((A_, m, A_)), osb.v((A_, cs(m))), identb)
                    CP("act", oT, ffn.PSh)
                    for h in range(2):
                        for mk in range(8):
                            MM(ffn.PX.v((A_, hs(h))), oT.v((A_, mk, A_)), Wo1.v((A_, mk, hs(h))), start=(mk == 0), stop=(mk == 7))
                    TT("dve", x5[1], ffn.PX, gmb[2], ALU.mult)
                    TT("dve", ffn.xa, x5[1], x5[0], ALU.add)
                    ffn.prep(1, i)
            kb.barrier()

            def emit1(i, xa_):
                ACT(ffn.xn, xa_, AF.Square, accum=s5.v((A_, slice(0, 1))))
                ACT(s5.v((A_, slice(1, 2))), s5.v((A_, slice(0, 1))), AF.Ln, bias=1024e-6)
                ACT(s5.v((A_, slice(1, 2))), s5.v((A_, slice(1, 2))), AF.Exp, scale=-0.5)
                STT("dve", hflat, xa_, s5.v((A_, slice(1, 2))), fg, ALU.mult, ALU.mult)
                DMA("sp", out[rowsl(i), :], hflat[:], reads=[hflat], writes=[dOUT[i]])
            ffn.run(1, gmb[3], emit1)
    allb = [d.b for d in dOUT]
    kb.finish("sp", allb)
    kb.finish("pool", allb)
    kb.close()
    es.close()
    return nc, in_names, dbg_outs


def _col(v):
    return np.ascontiguousarray(np.asarray(v, np.float32).reshape(8, 128).T)


def make_in_maps(inputs, T=None, n_cores=8):
    p = {k: np.asarray(v) for k, v in inputs.items()}
    Tfull = p["x"].shape[1]
    T = T or Tfull
    shared = {
        "ada_w": np.ascontiguousarray(p["ada_w"]),
        "ada_kv_w": np.ascontiguousarray(p["ada_kv_w"]),
        "rw_w_rkv": np.ascontiguousarray(p["rw_w_rkv"][0]),
        "rw_w1a": np.ascontiguousarray(np.concatenate([p["rw_w1"][0], p["rw_a1"][0]], axis=1)),
        "rw_g1": np.ascontiguousarray(p["rw_g1"][0]),
        "rw_w2a2": np.ascontiguousarray(np.concatenate([p["rw_w2"][0], p["rw_a2"][0]], axis=0)),
        "rw_g2": np.ascontiguousarray(p["rw_g2"][0]),
        "rw_w_o": np.ascontiguousarray(p["rw_w_o"][0]),
        "w_kv": np.ascontiguousarray(p["w_kv"]),
        "df_w_q": np.ascontiguousarray(p["df_w_q"][0]),
        "df_w_o": np.ascontiguousarray(p["df_w_o"][0]),
        "moe_wr": np.ascontiguousarray(np.concatenate([p["moe_w_rg"], p["moe_w_re"]], axis=2)),
        "moe_w_gate": np.ascontiguousarray(p["moe_w_gate"].reshape(2, NEXP, 8, 128, EFF).transpose(0, 1, 3, 2, 4).reshape(2, NEXP * 128, 4096)),
        "moe_w_up": np.ascontiguousarray(p["moe_w_up"].reshape(2, NEXP, 8, 128, EFF).transpose(0, 1, 3, 2, 4).reshape(2, NEXP * 128, 4096)),
        "moe_w_down": np.ascontiguousarray(p["moe_w_down"].reshape(2, NEXP, 4, 128, D).transpose(0, 1, 3, 2, 4).reshape(2, NEXP * 128, 4096)),
    }
    vr = np.zeros((N_VR, D), np.float32)
    vr[VR_KK] = p["rw_k_k"][0]
    vr[VR_KA] = p["rw_k_a"][0]
    vr[VR_RK] = p["rw_r_k"][0].reshape(-1)
    vr[VR_GNG] = p["rw_gn_g"][0]
    vr[VR_GNB] = p["rw_gn_b"][0]
    vr[VR_W0] = p["rw_w0"][0]
    vr[VR_A0] = p["rw_a0"][0]
    vr[VR_FING] = p["final_g"]
    vr[VR_GM0] = p["ada_b"][0, 2 * D:3 * D]
    vr[VR_GF0] = p["ada_b"][0, 5 * D:6 * D]
    vr[VR_GM1] = p["ada_b"][1, 2 * D:3 * D]
    vr[VR_GF1] = p["ada_b"][1, 5 * D:6 * D]
    vr[VR_SUBLN] = np.tile(p["df_subln_g"][0], 8)
    shared["vec_row"] = vr
    lam = np.zeros((128, 4), np.float32)
    lam[:64, 0] = p["df_lq1"][0]
    lam[:64, 1] = p["df_lk1"][0]
    lam[:64, 2] = p["df_lq2"][0]
    lam[:64, 3] = p["df_lk2"][0]
    shared["lam"] = lam
    tri = (np.arange(128)[:, None] <= np.arange(128)[None, :]).astype(np.float32)
    maps = []
    for core in range(n_cores):
        b, s = core // 2, core % 2
        vc = np.zeros((128, N_VC, 8), np.float32)
        vc[:, VC_C] = _col(p["c"][b])
        vc[:, VC_NMIX0] = _col(p["norm_mix_g"][0])
        vc[:, VC_NFFN0] = _col(p["norm_ffn_g"][0])
        vc[:, VC_NMIX1] = _col(p["norm_mix_g"][1])
        vc[:, VC_NFFN1] = _col(p["norm_ffn_g"][1])
        vc[:, VC_NKV] = _col(p["norm_kv_g"])
        for i in range(6):
            vc[:, VC_MU + i] = _col(p["rw_mu"][0, i])
            vc[:, VC_ADAB0 + i] = _col(p["ada_b"][0, i * D:(i + 1) * D])
            vc[:, VC_ADAB1 + i] = _col(p["ada_b"][1, i * D:(i + 1) * D])
        vc[:, VC_KVB] = _col(p["ada_kv_b"][:D])
        vc[:, VC_KVB + 1] = _col(p["ada_kv_b"][D:])
        sel = np.zeros((128, 4), np.float32)
        sel[:, 0] = 1.0 if s == 0 else 0.0
        sel[:, 1] = 1.0 if s == 1 else 0.0
        cm = np.stack([tri if s == 0 else np.ones_like(tri), np.zeros_like(tri) if s == 0 else tri]).astype(np.float32)
        m = dict(shared)
        m.update({"x": np.ascontiguousarray(p["x"][b, :T]), "vec_col": vc, "sel": sel, "cmask": cm})
        maps.append(m)
    return maps


_CACHE = {}


def kernel(**inputs):
    T = int(np.asarray(inputs["x"]).shape[1])
    if T not in _CACHE:
        _CACHE[T] = build_program(T)[0]
    nc = _CACHE[T]
    maps = make_in_maps(inputs)
    res = run_bass_kernel_spmd(nc, maps, core_ids=list(range(8)))
    B = np.asarray(inputs["x"]).shape[0]
    outp = np.zeros((B, T, D), np.float32)
    for core in range(8):
        b, s = core // 2, core % 2
        o = np.asarray(res.results[core]["out"]).reshape(T // 256, 128, D)
        outp[b].reshape(T // 128, 128, D)[s::2] = o
    return outp
```
